# Optimizing a Trainium2 kernel written in Bass

```python
import math
import jax, jax.numpy as jnp
from jax import lax
import numpy as np

D_MODEL = 2048
BATCH = 2
SEQ = 4096
DEPTH = 1

D_MIX = D_MODEL
MLSTM_WIDTH = D_MIX // 2
MLSTM_HEADS = 4
MLSTM_HEAD_DIM = MLSTM_WIDTH // MLSTM_HEADS
MLSTM_CHUNK = 128
CONV_WIDTH = 4
S5_WIDTH = D_MIX - MLSTM_WIDTH
S5_GROUP = 16
S5_GROUPS = S5_WIDTH // S5_GROUP
S5_STATE = 64
S5_DT_MIN = 1e-3
S5_DT_MAX = 1e-1
PEER_HEADS = 8
PEER_NKEYS = 128
PEER_EXPERTS = PEER_NKEYS * PEER_NKEYS
PEER_TOPK = 16
PEER_QDIM = 256
PEER_BLOCK = 128
RMS_EPS = 1e-6
N_IN = 4 * MLSTM_WIDTH + 2 * MLSTM_HEADS + S5_WIDTH

kernel_name = 'hybrid_mlstm_s5_peer_block'


def rmsnorm(x, g):
    xf = x.astype(jnp.float32)
    y = xf * lax.rsqrt(jnp.mean(xf * xf, axis=-1, keepdims=True) + RMS_EPS)
    return (y * g.astype(jnp.float32)).astype(x.dtype)


def causal_dwconv(x, w):
    K, C = w.shape
    return lax.conv_general_dilated(
        x, w[:, None, :].astype(x.dtype), window_strides=(1,), padding=((K - 1, 0),),
        dimension_numbers=('NWC', 'WIO', 'NWC'), feature_group_count=C)


def mlstm_chunkwise(q, k, v, li, lf):
    B, H, S, Dh = q.shape
    L = MLSTM_CHUNK
    NC = S // L
    q = q.reshape(B, H, NC, L, Dh) * (Dh ** -0.5)
    k = k.reshape(B, H, NC, L, Dh)
    v = v.reshape(B, H, NC, L, Dh)
    li = li.reshape(B, H, NC, L)
    lf = lf.reshape(B, H, NC, L)
    a = jnp.cumsum(lf, axis=-1)
    A = a[..., -1]
    g = A[..., None] - a + li
    m_loc = jnp.max(g, axis=-1)
    w = jnp.exp(g - m_loc[..., None])
    dC = jnp.einsum('bhcl,bhcld,bhcle->bhcde', w, k, v)
    dn = jnp.einsum('bhcl,bhcld->bhcd', w, k)

    def step(carry, inp):
        C, n, m = carry
        dC_c, dn_c, m_c, A_c = inp
        m_new = jnp.maximum(A_c + m, m_c)
        s_old = jnp.exp(A_c + m - m_new)
        s_new = jnp.exp(m_c - m_new)
        C_new = s_old[..., None, None] * C + s_new[..., None, None] * dC_c
        n_new = s_old[..., None] * n + s_new[..., None] * dn_c
        return (C_new, n_new, m_new), (C, n, m)

    init = (jnp.zeros((B, H, Dh, Dh), jnp.float32), jnp.zeros((B, H, Dh), jnp.float32),
            jnp.zeros((B, H), jnp.float32))
    xs = (jnp.moveaxis(dC, 2, 0), jnp.moveaxis(dn, 2, 0), jnp.moveaxis(m_loc, 2, 0), jnp.moveaxis(A, 2, 0))
    _, (C0, n0, m0) = lax.scan(step, init, xs)
    C0 = jnp.moveaxis(C0, 0, 2)
    n0 = jnp.moveaxis(n0, 0, 2)
    m0 = jnp.moveaxis(m0, 0, 2)

    Dmat = a[..., :, None] - a[..., None, :] + li[..., None, :]
    causal = jnp.tril(jnp.ones((L, L), dtype=bool))
    Dmat = jnp.where(causal, Dmat, -jnp.inf)
    m_inter = a + m0[..., None]
    m_t = jnp.maximum(m_inter, jnp.max(Dmat, axis=-1))
    Wm = jnp.exp(Dmat - m_t[..., None])
    s_inter = jnp.exp(m_inter - m_t)
    qk = jnp.einsum('bhcjd,bhcsd->bhcjs', q, k) * Wm
    num = jnp.einsum('bhcjs,bhcse->bhcje', qk, v) + s_inter[..., None] * jnp.einsum('bhcjd,bhcde->bhcje', q, C0)
    den = jnp.sum(qk, axis=-1) + s_inter * jnp.einsum('bhcjd,bhcd->bhcj', q, n0)
    h = num / jnp.maximum(jnp.abs(den), jnp.exp(-m_t))[..., None]
    return h.reshape(B, H, S, Dh)


def s5_groups(u, lam_re, lam_im, log_dt, b_re, b_im, c_re, c_im, d, glu_w):
    S = u.shape[1]
    f32 = jnp.float32
    lam_re = lam_re.astype(f32); lam_im = lam_im.astype(f32)
    dt = jnp.exp(log_dt.astype(f32))[:, None]
    mag = jnp.exp(lam_re * dt)
    ar = mag * jnp.cos(lam_im * dt)
    ai = mag * jnp.sin(lam_im * dt)
    den = lam_re * lam_re + lam_im * lam_im
    fr = ((ar - 1.0) * lam_re + ai * lam_im) / den
    fi = (ai * lam_re - (ar - 1.0) * lam_im) / den
    b_re = b_re.astype(f32); b_im = b_im.astype(f32)
    bbr = fr[..., None] * b_re - fi[..., None] * b_im
    bbi = fr[..., None] * b_im + fi[..., None] * b_re
    xr = jnp.einsum('bsgh,gph->bsgp', u, bbr)
    xi = jnp.einsum('bsgh,gph->bsgp', u, bbi)
    G, P = ar.shape
    a_r = jnp.broadcast_to(ar[None, None], (1, S, G, P))
    a_i = jnp.broadcast_to(ai[None, None], (1, S, G, P))

    def combine(e1, e2):
        a1r, a1i, b1r, b1i = e1
        a2r, a2i, b2r, b2i = e2
        return (a1r * a2r - a1i * a2i, a1r * a2i + a1i * a2r,
                a2r * b1r - a2i * b1i + b2r, a2r * b1i + a2i * b1r + b2i)

    _, _, sr, si = lax.associative_scan(combine, (a_r, a_i, xr, xi), axis=1)
    y = (jnp.einsum('bsgp,ghp->bsgh', sr, c_re.astype(f32))
         - jnp.einsum('bsgp,ghp->bsgh', si, c_im.astype(f32))
         + d.astype(f32) * u)
    ab = jnp.einsum('bsgh,ghk->bsgk', jax.nn.gelu(y, approximate=False), glu_w.astype(f32))
    return ab[..., :S5_GROUP] * jax.nn.sigmoid(ab[..., S5_GROUP:])


def peer(x, wq, subkeys, u_tab, v_tab):
    B, S, D = x.shape
    f32 = jnp.float32
    half = PEER_QDIM // 2
    q = (x @ wq).astype(f32).reshape(B, S, PEER_HEADS, 2, half)
    s = jnp.einsum('bshpd,pkd->bshpk', q, subkeys.astype(f32))
    s1, i1 = lax.top_k(s[..., 0, :], PEER_TOPK)
    s2, i2 = lax.top_k(s[..., 1, :], PEER_TOPK)
    cand_s = (s1[..., :, None] + s2[..., None, :]).reshape(B, S, PEER_HEADS, PEER_TOPK * PEER_TOPK)
    cand_i = (i1[..., :, None] * PEER_NKEYS + i2[..., None, :]).reshape(B, S, PEER_HEADS, PEER_TOPK * PEER_TOPK)
    top_s, pos = lax.top_k(cand_s, PEER_TOPK)
    idx = jnp.take_along_axis(cand_i, pos, axis=-1)
    gate = jax.nn.softmax(top_s, axis=-1)
    T = B * S
    HK = PEER_HEADS * PEER_TOPK
    xb = x.reshape(T // PEER_BLOCK, PEER_BLOCK, D)
    ib = idx.reshape(T // PEER_BLOCK, PEER_BLOCK, HK)
    gb = gate.reshape(T // PEER_BLOCK, PEER_BLOCK, HK)

    def block(args):
        xt, it, gt = args
        ue = jnp.take(u_tab, it, axis=0)
        ve = jnp.take(v_tab, it, axis=0)
        act = jax.nn.gelu(jnp.einsum('td,ted->te', xt, ue).astype(f32), approximate=False)
        return jnp.einsum('te,ted->td', (gt * act).astype(ve.dtype), ve)

    out = lax.map(block, (xb, ib, gb))
    return out.reshape(B, S, D)


def setup_inputs(seed: int = 0) -> dict:
    key = jax.random.key(seed)
    ks = jax.random.split(key, 24)
    f32 = jnp.float32
    nrm = lambda k, shape, s: jax.random.normal(k, shape, f32) * s
    L = DEPTH
    G, P, Hg = S5_GROUPS, S5_STATE, S5_GROUP
    f_bias = jnp.broadcast_to(jnp.linspace(3.0, 6.0, MLSTM_HEADS, dtype=f32), (L, MLSTM_HEADS))
    b_gates = jnp.concatenate([nrm(ks[3], (L, MLSTM_HEADS), 0.1),
                               f_bias + nrm(ks[4], (L, MLSTM_HEADS), 0.1)], axis=-1)
    lam_im0 = jnp.pi * jnp.arange(P, dtype=f32)
    log_dt = jax.random.uniform(ks[8], (L, G), f32, math.log(S5_DT_MIN), math.log(S5_DT_MAX))
    return dict(
        x=nrm(ks[0], (BATCH, SEQ, D_MODEL), 1.0),
        norm1_g=1.0 + nrm(ks[1], (L, D_MODEL), 0.1),
        w_in=nrm(ks[2], (L, D_MODEL, N_IN), D_MODEL ** -0.5),
        b_gates=b_gates,
        conv_qk_w=nrm(ks[5], (L, CONV_WIDTH, 2 * MLSTM_WIDTH), CONV_WIDTH ** -0.5),
        mlstm_norm_g=1.0 + nrm(ks[6], (L, MLSTM_WIDTH), 0.1),
        s5_lambda_re=-0.5 + nrm(ks[7], (L, G, P), 0.01),
        s5_lambda_im=lam_im0 + nrm(ks[9], (L, G, P), 0.01),
        s5_log_dt=log_dt,
        s5_b_re=nrm(ks[10], (L, G, P, Hg), (2 * Hg) ** -0.5),
        s5_b_im=nrm(ks[11], (L, G, P, Hg), (2 * Hg) ** -0.5),
        s5_c_re=nrm(ks[12], (L, G, Hg, P), P ** -0.5),
        s5_c_im=nrm(ks[13], (L, G, Hg, P), P ** -0.5),
        s5_d=nrm(ks[14], (L, G, Hg), 0.5),
        s5_glu_w=nrm(ks[15], (L, G, Hg, 2 * Hg), Hg ** -0.5),
        w_out=nrm(ks[16], (L, D_MIX, D_MODEL), D_MIX ** -0.5),
        norm2_g=1.0 + nrm(ks[17], (L, D_MODEL), 0.1),
        peer_wq=nrm(ks[18], (L, D_MODEL, PEER_HEADS * PEER_QDIM), D_MODEL ** -0.5),
        peer_subkeys=nrm(ks[19], (L, 2, PEER_NKEYS, PEER_QDIM // 2), (PEER_QDIM // 2) ** -0.5),
        peer_u=nrm(ks[20], (L, PEER_EXPERTS, D_MODEL), D_MODEL ** -0.5),
        peer_v=nrm(ks[21], (L, PEER_EXPERTS, D_MODEL), PEER_HEADS ** -0.5),
        final_g=1.0 + nrm(ks[22], (D_MODEL,), 0.1),
    )


def reference(x, norm1_g, w_in, b_gates, conv_qk_w, mlstm_norm_g, s5_lambda_re, s5_lambda_im,
              s5_log_dt, s5_b_re, s5_b_im, s5_c_re, s5_c_im, s5_d, s5_glu_w, w_out, norm2_g,
              peer_wq, peer_subkeys, peer_u, peer_v, final_g):
    B, S, _ = x.shape
    f32 = jnp.float32
    W, H, Dh = MLSTM_WIDTH, MLSTM_HEADS, MLSTM_HEAD_DIM
    h = x
    for l in range(DEPTH):
        xn = rmsnorm(h, norm1_g[l])
        z = xn @ w_in[l]
        q, k, v, o, gates, u = jnp.split(z, [W, 2 * W, 3 * W, 4 * W, 4 * W + 2 * H], axis=-1)
        qk = jax.nn.silu(causal_dwconv(jnp.concatenate([q, k], axis=-1), conv_qk_w[l]))
        q, k = qk[..., :W], qk[..., W:]
        to_heads = lambda t: t.astype(f32).reshape(B, S, H, Dh).transpose(0, 2, 1, 3)
        gates = gates.astype(f32) + b_gates[l].astype(f32)
        li = gates[..., :H].transpose(0, 2, 1)
        lf = jax.nn.log_sigmoid(gates[..., H:]).transpose(0, 2, 1)
        hm = mlstm_chunkwise(to_heads(q), to_heads(k), to_heads(v), li, lf)
        hm = hm.transpose(0, 2, 1, 3)
        hm = hm * lax.rsqrt(jnp.mean(hm * hm, axis=-1, keepdims=True) + RMS_EPS)
        hm = hm.reshape(B, S, W) * mlstm_norm_g[l].astype(f32) * jax.nn.sigmoid(o.astype(f32))
        ys = s5_groups(u.astype(f32).reshape(B, S, S5_GROUPS, S5_GROUP),
                       s5_lambda_re[l], s5_lambda_im[l], s5_log_dt[l], s5_b_re[l], s5_b_im[l],
                       s5_c_re[l], s5_c_im[l], s5_d[l], s5_glu_w[l]).reshape(B, S, S5_WIDTH)
        mix = jnp.concatenate([hm, ys], axis=-1).astype(h.dtype) @ w_out[l]
        h = h + mix.astype(h.dtype)
        h = h + peer(rmsnorm(h, norm2_g[l]), peer_wq[l], peer_subkeys[l], peer_u[l], peer_v[l]).astype(h.dtype)
    return rmsnorm(h, final_g)
```

```python
import os
import numpy as np
import ml_dtypes
from contextlib import ExitStack
import concourse.bass as bass
import concourse.mybir as mybir
from concourse.bass_utils import run_bass_kernel_spmd

F32 = mybir.dt.float32
BF16 = mybir.dt.bfloat16
ALU = mybir.AluOpType
AF = mybir.ActivationFunctionType
AX = mybir.AxisListType

EPS = 1e-6
NT = 4096
DM = 2048
KC = 16


class Prog:
    ENG = ['pe', 'dve', 'act', 'pool', 'sp']

    def __init__(self, nc):
        self.nc = nc
        self.ops = {e: [] for e in self.ENG}
        self.cnt = {e: 0 for e in self.ENG}
        self.dcnt = {}
        self.lastw = {}
        self.rds = {}
        self.floor = {}

    def _tok_add(self, d, tok):
        s, v, e = tok
        if s not in d or d[s][0] < v:
            d[s] = (v, e)

    def _mk(self, reads, writes):
        deps = dict(self.floor)
        for k in reads:
            if k in self.lastw:
                self._tok_add(deps, self.lastw[k])
        for k in writes:
            if k in self.lastw:
                self._tok_add(deps, self.lastw[k])
            for s, (v, e) in self.rds.get(k, {}).items():
                self._tok_add(deps, (s, v, e))
        return deps

    def _commit(self, tok, reads, writes):
        for k in reads:
            self._tok_add(self.rds.setdefault(k, {}), tok)
        for k in writes:
            self.lastw[k] = tok
            self.rds[k] = {}

    def _skip(self, force):
        self.total = getattr(self, 'total', 0) + 1
        cut = int(os.environ.get('KCUT', '0'))
        return bool(cut) and self.total > cut and not force

    def op(self, eng, fn, reads=(), writes=(), force=False):
        if self._skip(force):
            return
        deps = self._mk(reads, writes)
        self.cnt[eng] += 1
        tok = (eng, self.cnt[eng], eng)
        self.ops[eng].append((deps, fn, eng, 1))
        self._commit(tok, reads, writes)

    def dma(self, queue, fn, stream, reads=(), writes=(), inc=16, force=False):
        if self._skip(force):
            return
        deps = self._mk(reads, writes)
        if stream == 'c':
            self.nuniq = getattr(self, 'nuniq', 0) + 1
            stream = f'c{self.nuniq}'
        s = 'd_' + stream
        self.dcnt[s] = self.dcnt.get(s, 0) + inc
        tok = (s, self.dcnt[s], 'dma')
        self.ops[queue].append((deps, fn, s, inc))
        self._commit(tok, reads, writes)

    def barrier(self):
        for e in self.ENG:
            if self.cnt[e]:
                self.floor[e] = (self.cnt[e], e)
        for s, v in self.dcnt.items():
            self.floor[s] = (v, 'dma')

    def emit(self, stack, final_waits=True):
        nc = self.nc
        sems = {}
        for e in self.ENG:
            sems[e] = stack.enter_context(nc.semaphore('sem_' + e))
        for s in self.dcnt:
            sems[s] = stack.enter_context(nc.semaphore('sem_' + s))
        block = stack.enter_context(nc.Block())
        total = dict((e, (self.cnt[e], e)) for e in self.ENG if self.cnt[e])
        for s, v in self.dcnt.items():
            total[s] = (v, 'dma')

        def run(ename):
            def body(eng):
                seen = {}
                for deps, fn, sname, inc in self.ops[ename]:
                    for s, (v, de) in deps.items():
                        if de == ename and ename == 'pe':
                            continue
                        if seen.get(s, 0) < v:
                            eng.wait_ge(sems[s], v)
                            seen[s] = v
                    ins = fn(eng)
                    ins.then_inc(sems[sname], inc)
                if ename == 'sp':
                    for s, (v, de) in total.items():
                        if seen.get(s, 0) < v:
                            eng.wait_ge(sems[s], v)
            return body
        block.tensor(run('pe'))
        block.vector(run('dve'))
        block.scalar(run('act'))
        block.gpsimd(run('pool'))
        block.sync(run('sp'))


def _ident():
    return np.eye(128, dtype=np.float32)


def build(stage='full'):
    nc = bass.Bass("TRN2", target_bir_lowering=False)
    P = Prog(nc)
    D = {}

    def din(name, shape, dt=F32):
        D[name] = nc.dram_tensor(name, list(shape), dt, kind="ExternalInput").ap()
        return D[name]

    x = din('x', [NT, DM])
    w_a = din('w_a', [128, KC, 1282])
    g1 = din('g1', [128, KC])
    bg = din('bg', [128, 2])
    convw = din('convw', [128, 4, 4])
    mg = din('mg', [128, 256])
    c_ident = din('c_ident', [128, 128])
    c_triu = din('c_triu', [128, 128])
    c_ones = din('c_ones', [128, 128])

    s5c = din('s5c', [128, 3, 8])
    s5r = din('s5r', [128, 3, 1024])
    s5b = din('s5b', [128, 2, 1024])
    s5cm = din('s5cm', [128, 2, 1024])
    s5d = din('s5d', [128, 2])
    s5gw = din('s5gw', [128, 4, 128])
    x_own = din('x_own', [1024, DM])
    w_o = din('w_o', [128, KC, DM])
    w_q = din('w_q', [128, KC, DM])
    g2c = din('g2c', [128, KC])
    g2r = din('g2r', [128, DM])
    gfr = din('gfr', [128, DM])
    skT = din('skT', [128, 2, 128])
    pu = din('pu', [16384, DM])
    pv_ = din('pv', [16384, DM])
    selin = din('sel', [128, 4])
    if stage == 'simB':
        mx_test = din('mx_test', [2048, NT], BF16)
    if stage in ('full', 'simB'):
        yout = nc.dram_tensor('y', [1024, DM], F32, kind="ExternalOutput").ap()
    mxi = [nc.dram_tensor(f'mxi{q}', [512, 1024], BF16) for q in range(4)]
    mxa = [nc.dram_tensor(f'mxa{q}', [2048, 1024], BF16) for q in range(4)]

    if stage in ('A1', 'A2', 'A3'):
        dbg = nc.dram_tensor('dbg', [512, NT], BF16, kind="ExternalOutput").ap()

    top = ExitStack()
    with top:
        def sb(stack, name, shape, dt=F32):
            return stack.enter_context(nc.sbuf_tensor(name, list(shape), dt))

        def ps(stack, name, shape, dt=F32):
            return stack.enter_context(nc.psum_tensor(name, list(shape), dt))

        ident_f = sb(top, 'ident_f', [128, 128])
        ident_b = sb(top, 'ident_b', [128, 128], BF16)
        triu = sb(top, 'triu', [128, 128])
        ones = sb(top, 'ones', [128, 128])
        P.dma('sp', lambda e: e.dma_start(out=ident_f[:, :], in_=c_ident[:, :]), 'c', writes=['ident_f'])
        P.dma('sp', lambda e: e.dma_start(out=triu[:, :], in_=c_triu[:, :]), 'c', writes=['triu'])
        P.dma('sp', lambda e: e.dma_start(out=ones[:, :], in_=c_ones[:, :]), 'c', writes=['ones'])
        P.op('dve', lambda e: e.tensor_copy(out=ident_b[:, :], in_=ident_f[:, :]), reads=['ident_f'], writes=['ident_b'])

        sA = ExitStack()
        uT = sb(sA, 'uT', [128, 2, NT], BF16)
        sA2 = ExitStack()
        qT = sb(sA2, 'qT', [128, 2, NT], BF16)
        kT = sb(sA2, 'kT', [128, 2, NT], BF16)
        v_aug = sb(sA2, 'v_aug', [128, 32, 257], BF16)
        gso = sb(sA2, 'gso', [128, 32, 256], BF16)
        g_tm = sb(sA2, 'g_tm', [128, 32, 2])
        bgt = sb(sA2, 'bgt', [128, 2])
        P.op('pool', lambda e: e.memset(v_aug[:, :, 256:257], 1.0), writes=['v_aug'])

        s1 = ExitStack()
        W = sb(s1, 'W', [128, KC, 1282], BF16)
        wst = [sb(s1, f'wst{i}', [128, 1282]) for i in range(2)]
        g1t = sb(s1, 'g1t', [128, KC])
        cw = sb(s1, 'cw', [128, 4, 4])
        mgt = sb(s1, 'mgt', [128, 256])
        xt = [sb(s1, f'xt{i}', [128, DM]) for i in range(2)]
        xs = [sb(s1, f'xs{i}', [128, DM], BF16) for i in range(2)]
        junk = sb(s1, 'junk', [128, DM], BF16)
        ssq = [sb(s1, f'ssq{i}', [128, 1]) for i in range(2)]
        rstd = [sb(s1, f'rstd{i}', [128, 1]) for i in range(2)]
        xnT = sb(s1, 'xnT', [128, KC, 512], BF16)
        pre = sb(s1, 'pre', [128, 4, 515])
        cacc = [sb(s1, f'cacc{i}', [128, 512]) for i in range(2)]
        sgo = [sb(s1, f'sgo{i}', [128, 256]) for i in range(2)]
        ptr = [ps(s1, f'ptr{i}', [128, 8, 128], BF16) for i in range(2)]
        pf = [ps(s1, f'pf{i}', [128, 512]) for i in range(2)]
        pv = [ps(s1, f'pv{i}', [128, 258]) for i in range(2)]
        po = [ps(s1, f'po{i}', [128, 256]) for i in range(2)]

        P.dma('sp', lambda e: e.dma_start(out=g1t[:, :], in_=g1[:, :]), 'c', writes=['g1t'])
        P.dma('sp', lambda e: e.dma_start(out=bgt[:, :], in_=bg[:, :]), 'c', writes=['bgt'])
        P.dma('sp', lambda e: e.dma_start(out=cw[:, :, :], in_=convw[:, :, :]), 'c', writes=['cw'])
        P.dma('sp', lambda e: e.dma_start(out=mgt[:, :], in_=mg[:, :]), 'c', writes=['mgt'])
        for kc in range(KC):
            b = kc % 2
            P.dma('sp', lambda e, kc=kc, b=b: e.dma_start(out=wst[b][:, :], in_=w_a[:, kc, :]), f'w{b}', writes=[f'wst{b}'])
            P.op('dve' if kc % 2 == 0 else 'pool',
                 lambda e, kc=kc, b=b: e.tensor_scalar(out=W[:, kc, :], in0=wst[b][:, :], scalar1=g1t[:, kc:kc + 1], scalar2=None, op0=ALU.mult),
                 reads=[f'wst{b}', 'g1t'], writes=[('W', kc)])
        Wkeys = [('W', kc) for kc in range(KC)]
        for m in range(4):
            P.op('pool', lambda e, m=m: e.memset(pre[:, m, 0:3], 0.0), writes=[('pre', m)])

        print('ops before token loop', getattr(P, 'total', 0))
        for tb in range(8):
            print('tb', tb, getattr(P, 'total', 0))
            for t4 in range(4):
                c = tb * 4 + t4
                b = c % 2
                P.dma('sp', lambda e, c=c, b=b: e.dma_start(out=xt[b][:, :], in_=x[c * 128:(c + 1) * 128, :]), f'x{b}', writes=[f'xt{b}'])
                P.op('act', lambda e, b=b: e.activation(out=junk[:, :], in_=xt[b][:, :], func=AF.Square, accum_out=ssq[b][:, :]),
                     reads=[f'xt{b}'], writes=['junk', f'ssq{b}'])
                P.op('dve', lambda e, b=b: e.tensor_scalar(out=rstd[b][:, :], in0=ssq[b][:, :], scalar1=1.0 / DM, scalar2=EPS, op0=ALU.mult, op1=ALU.add),
                     reads=[f'ssq{b}'], writes=[f'rstd{b}'])
                P.op('act', lambda e, b=b: e.activation(out=rstd[b][:, :], in_=rstd[b][:, :], func=AF.Sqrt),
                     reads=[f'rstd{b}'], writes=[f'rstd{b}'])
                P.op('dve', lambda e, b=b: e.reciprocal(out=rstd[b][:, :], in_=rstd[b][:, :]),
                     reads=[f'rstd{b}'], writes=[f'rstd{b}'])
                P.op('dve', lambda e, b=b: e.tensor_scalar(out=xs[b][:, :], in0=xt[b][:, :], scalar1=rstd[b][:, 0:1], scalar2=None, op0=ALU.mult),
                     reads=[f'xt{b}', f'rstd{b}'], writes=[f'xs{b}'])
                for half in range(2):
                    for j in range(8):
                        kc = half * 8 + j
                        P.op('pe', lambda e, b=b, kc=kc, half=half, j=j: e.transpose(out=ptr[half][:, j, :], in_=xs[b][:, kc * 128:(kc + 1) * 128], identity=ident_b[:, :]),
                             reads=[f'xs{b}', 'ident_b'], writes=[f'ptr{half}'])
                    P.op('act' if half == 0 else 'dve',
                         (lambda e, half=half, t4=t4: e.activation(out=xnT[:, half * 8:half * 8 + 8, t4 * 128:(t4 + 1) * 128], in_=ptr[half][:, :, :], func=AF.Copy)) if half == 0 else
                         (lambda e, half=half, t4=t4: e.tensor_copy(out=xnT[:, half * 8:half * 8 + 8, t4 * 128:(t4 + 1) * 128], in_=ptr[half][:, :, :])),
                         reads=[f'ptr{half}'], writes=[('xnT', t4)])
            xk = [('xnT', t) for t in range(4)]
            for m in range(6):
                pb = m % 2
                for kc in range(KC):
                    P.op('pe', lambda e, m=m, kc=kc, pb=pb: e.matmul(pf[pb][:, :], lhsT=W[:, kc, m * 128:(m + 1) * 128], rhs=xnT[:, kc, :], start=(kc == 0), stop=(kc == KC - 1)),
                         reads=xk + Wkeys, writes=[f'pf{pb}'])
                if m < 4:
                    P.op('act', lambda e, m=m, pb=pb: e.activation(out=pre[:, m, 3:515], in_=pf[pb][:, :], func=AF.Copy),
                         reads=[f'pf{pb}'], writes=[('pre', m)])
                    cb = m % 2
                    P.op('dve', lambda e, m=m, cb=cb: e.tensor_scalar(out=cacc[cb][:, :], in0=pre[:, m, 0:512], scalar1=cw[:, m, 0:1], scalar2=None, op0=ALU.mult),
                         reads=[('pre', m), 'cw'], writes=[f'cacc{cb}'])
                    for j in range(1, 4):
                        P.op('dve', lambda e, m=m, cb=cb, j=j: e.scalar_tensor_tensor(out=cacc[cb][:, :], in0=pre[:, m, j:j + 512], scalar=cw[:, m, j:j + 1], in1=cacc[cb][:, :], op0=ALU.mult, op1=ALU.add),
                             reads=[('pre', m), 'cw', f'cacc{cb}'], writes=[f'cacc{cb}'])
                    dst = qT if m < 2 else kT
                    P.op('act', lambda e, m=m, cb=cb, dst=dst, tb=tb: e.activation(out=dst[:, m % 2, tb * 512:(tb + 1) * 512], in_=cacc[cb][:, :], func=AF.Silu),
                         reads=[f'cacc{cb}'], writes=[('qk', m, tb)])
                    P.op('pool', lambda e, m=m: e.tensor_copy(out=pre[:, m, 0:3], in_=pre[:, m, 512:515]),
                         reads=[('pre', m)], writes=[('pre', m)])
                else:
                    P.op('dve', lambda e, m=m, pb=pb, tb=tb: e.tensor_copy(out=uT[:, m - 4, tb * 512:(tb + 1) * 512], in_=pf[pb][:, :]),
                         reads=[f'pf{pb}'], writes=[('uT', m - 4, tb)])
            for t4 in range(4):
                c = tb * 4 + t4
                pb = c % 2
                for kc in range(KC):
                    P.op('pe', lambda e, kc=kc, pb=pb, t4=t4: e.matmul(pv[pb][:, :], lhsT=xnT[:, kc, t4 * 128:(t4 + 1) * 128], rhs=W[:, kc, 768:1026], start=(kc == 0), stop=(kc == KC - 1)),
                         reads=xk + Wkeys, writes=[f'pv{pb}'])
                for kc in range(KC):
                    P.op('pe', lambda e, kc=kc, pb=pb, t4=t4: e.matmul(po[pb][:, :], lhsT=xnT[:, kc, t4 * 128:(t4 + 1) * 128], rhs=W[:, kc, 1026:1282], start=(kc == 0), stop=(kc == KC - 1)),
                         reads=xk + Wkeys, writes=[f'po{pb}'])
                P.op('dve', lambda e, c=c, pb=pb: e.tensor_copy(out=v_aug[:, c, 0:256], in_=pv[pb][:, 0:256]),
                     reads=[f'pv{pb}'], writes=[('v', c)])
                P.op('dve', lambda e, c=c, pb=pb: e.tensor_copy(out=g_tm[:, c, :], in_=pv[pb][:, 256:258]),
                     reads=[f'pv{pb}'], writes=[('g_tm', c)])
                P.op('act', lambda e, pb=pb: e.activation(out=sgo[pb][:, :], in_=po[pb][:, :], func=AF.Sigmoid),
                     reads=[f'po{pb}'], writes=[f'sgo{pb}'])
                P.op('pool', lambda e, c=c, pb=pb: e.tensor_tensor(out=gso[:, c, :], in0=sgo[pb][:, :], in1=mgt[:, :], op=ALU.mult),
                     reads=[f'sgo{pb}', 'mgt'], writes=[('gso', c)])
        P.barrier()
        s1.close()
        if stage == 'A1':
            P.dma('sp', lambda e: e.dma_start(out=dbg[0:128, :], in_=qT[:, 0, :]), 'out', writes=['dbg'], force=True)
            P.dma('sp', lambda e: e.dma_start(out=dbg[128:256, :], in_=kT[:, 1, :]), 'out', writes=['dbg'], force=True)
            P.dma('sp', lambda e: e.dma_start(out=dbg[256:384, :], in_=uT[:, 0, :]), 'out', writes=['dbg'], force=True)
            sA2.close()
            sA.close()
            P.emit(top)
            return nc

        print('ops before A2', getattr(P, 'total', 0))
        s2 = ExitStack()
        li = sb(s2, 'li', [128, 32])
        nlf = sb(s2, 'nlf', [128, 32])
        nega = sb(s2, 'nega', [128, 32])
        tmp = sb(s2, 'tmp', [128, 32])
        tmp2 = sb(s2, 'tmp2', [128, 32])
        es = sb(s2, 'es', [128, 32])
        wk = sb(s2, 'wk', [128, 32])
        eA = sb(s2, 'eA', [128, 32])
        nlfrep = sb(s2, 'nlfrep', [128, 32, 128])
        ea_bc = sb(s2, 'ea_bc', [128, NT])
        lnc = sb(s2, 'lnc', [128, 1])
        Cst = sb(s2, 'Cst', [128, 2, 257])
        Cb = sb(s2, 'Cb', [128, 2, 257], BF16)
        Pm = [sb(s2, f'Pm{i}', [128, 128], BF16) for i in range(2)]
        kw = [sb(s2, f'kw{i}', [128, 256], BF16) for i in range(2)]
        hmn = [sb(s2, f'hmn{i}', [128, 256], BF16) for i in range(2)]
        hT = [sb(s2, f'hT{i}', [128, 2, 128], BF16) for i in range(2)]
        sm = [sb(s2, f'sm{i}', [128, 8]) for i in range(2)]
        junk2 = sb(s2, 'junk2', [128, 256], BF16)
        pa = ps(s2, 'pa', [128, 64])
        pbc = ps(s2, 'pbc', [128, 512])
        ps1 = ps(s2, 'ps1', [128, 128])
        ptk = ps(s2, 'ptk', [128, 2, 128], BF16)
        psO = ps(s2, 'psO', [128, 257])
        pth = ps(s2, 'pth', [128, 2, 128], BF16)
        psC = [ps(s2, f'psC{i}', [128, 257]) for i in range(2)]

        gk = [('g_tm', c) for c in range(32)]
        P.op('dve', lambda e: e.tensor_scalar(out=li[:, :], in0=g_tm[:, :, 0], scalar1=bgt[:, 0:1], scalar2=None, op0=ALU.add), reads=gk + ['bgt'], writes=['li'])
        P.op('dve', lambda e: e.tensor_scalar(out=tmp[:, :], in0=g_tm[:, :, 1], scalar1=bgt[:, 1:2], scalar2=None, op0=ALU.add), reads=gk + ['bgt'], writes=['tmp'])
        P.op('act', lambda e: e.activation(out=tmp2[:, :], in_=tmp[:, :], func=AF.Exp, scale=-1.0), reads=['tmp'], writes=['tmp2'])
        P.op('dve', lambda e: e.tensor_scalar(out=tmp2[:, :], in0=tmp2[:, :], scalar1=1.0, scalar2=None, op0=ALU.add), reads=['tmp2'], writes=['tmp2'])
        P.op('act', lambda e: e.activation(out=nlf[:, :], in_=tmp2[:, :], func=AF.Ln), reads=['tmp2'], writes=['nlf'])
        P.op('dve', lambda e: e.memset(lnc[:, :], float(np.log(1.0 / 16.0))), writes=['lnc'])
        P.op('pe', lambda e: e.matmul(pa[:, 0:32], lhsT=triu[:, :], rhs=nlf[:, :], start=True, stop=True), reads=['triu', 'nlf'], writes=['pa'])
        P.op('pe', lambda e: e.matmul(pa[:, 32:64], lhsT=ones[:, :], rhs=nlf[:, :], start=True, stop=True), reads=['ones', 'nlf'], writes=['pa'])
        P.op('dve', lambda e: e.tensor_copy(out=nega[:, :], in_=pa[:, 0:32]), reads=['pa'], writes=['nega'])
        P.op('dve', lambda e: e.tensor_tensor(out=tmp[:, :], in0=li[:, :], in1=nega[:, :], op=ALU.add), reads=['li', 'nega'], writes=['tmp'])
        P.op('act', lambda e: e.activation(out=es[:, :], in_=tmp[:, :], func=AF.Exp), reads=['tmp'], writes=['es'])
        P.op('dve', lambda e: e.tensor_tensor(out=tmp2[:, :], in0=tmp[:, :], in1=pa[:, 32:64], op=ALU.subtract), reads=['tmp', 'pa'], writes=['tmp2'])
        P.op('act', lambda e: e.activation(out=wk[:, :], in_=tmp2[:, :], func=AF.Exp), reads=['tmp2'], writes=['wk'])
        P.op('act', lambda e: e.activation(out=eA[:, :], in_=pa[:, 32:64], func=AF.Exp, scale=-1.0), reads=['pa'], writes=['eA'])
        P.op('dve', lambda e: e.tensor_copy(out=nlfrep[:, :, :], in_=nlf[:, :].to_broadcast([128, 32, 128]) if False else nlf[:, :, None].to_broadcast([128, 32, 128])),
             reads=['nlf'], writes=['nlfrep'])
        for k8 in range(8):
            for j in range(4):
                c = k8 * 4 + j
                P.op('pe', lambda e, c=c, j=j: e.matmul(pbc[:, j * 128:(j + 1) * 128], lhsT=nlfrep[:, c, :], rhs=triu[:, :], start=True, stop=True),
                     reads=['nlfrep', 'triu'], writes=['pbc'])
            P.op('act', lambda e, k8=k8: e.activation(out=ea_bc[:, k8 * 512:(k8 + 1) * 512], in_=pbc[:, :], func=AF.Exp, scale=-1.0, bias=lnc[:, 0:1]),
                 reads=['pbc', 'lnc'], writes=[('ea_bc', k8)])
            for dc in range(2):
                P.op('dve' if dc == 0 else 'pool', lambda e, k8=k8, dc=dc: e.tensor_tensor(out=qT[:, dc, k8 * 512:(k8 + 1) * 512], in0=qT[:, dc, k8 * 512:(k8 + 1) * 512], in1=ea_bc[:, k8 * 512:(k8 + 1) * 512], op=ALU.mult),
                     reads=[('ea_bc', k8), ('qk', dc, k8)], writes=[('qk', dc, k8)])
        P.op('dve', lambda e: e.memset(Cst[:, :, :], 0.0), writes=['Cst'])
        P.op('pool', lambda e: e.memset(Cb[:, :, :], 0.0), writes=['Cb'])

        for c in range(32):
            b = c % 2
            tb = c // 4
            cs = slice(c * 128, (c + 1) * 128)
            qkk = [('qk', m, tb) for m in range(4)]
            for dc in range(2):
                P.op('pe', lambda e, dc=dc, cs=cs: e.matmul(ps1[:, :], lhsT=kT[:, dc, cs], rhs=qT[:, dc, cs], start=(dc == 0), stop=(dc == 1)),
                     reads=qkk, writes=['ps1'])
            P.op('dve', lambda e, b=b, c=c: e.scalar_tensor_tensor(out=Pm[b][:, :], in0=ps1[:, :], scalar=es[:, c:c + 1], in1=triu[:, :], op0=ALU.mult, op1=ALU.mult),
                 reads=['ps1', 'es', 'triu'], writes=[f'Pm{b}'])
            for dc in range(2):
                P.op('pe', lambda e, dc=dc, cs=cs: e.transpose(out=ptk[:, dc, :], in_=kT[:, dc, cs], identity=ident_b[:, :]),
                     reads=qkk + ['ident_b'], writes=['ptk'])
            P.op('act', lambda e, b=b, c=c: e.activation(out=kw[b][:, :], in_=ptk[:, :, :], func=AF.Copy, scale=wk[:, c:c + 1]),
                 reads=['ptk', 'wk'], writes=[f'kw{b}'])
            P.op('pe', lambda e, b=b, c=c: e.matmul(psO[:, :], lhsT=Pm[b][:, :], rhs=v_aug[:, c, :], start=True, stop=False),
                 reads=[f'Pm{b}', ('v', c), 'v_aug'], writes=['psO'])
            for dc in range(2):
                P.op('pe', lambda e, dc=dc, cs=cs: e.matmul(psO[:, :], lhsT=qT[:, dc, cs], rhs=Cb[:, dc, :], start=False, stop=(dc == 1)),
                     reads=qkk + ['Cb'], writes=['psO'])
            P.op('act', lambda e, b=b: e.activation(out=sm[b][:, 0:1], in_=psO[:, 256:257], func=AF.Abs), reads=['psO'], writes=[f'sm{b}'])
            P.op('dve', lambda e, b=b: e.tensor_scalar(out=sm[b][:, 0:1], in0=sm[b][:, 0:1], scalar1=1.0, scalar2=None, op0=ALU.max), reads=[f'sm{b}'], writes=[f'sm{b}'])
            P.op('dve', lambda e, b=b: e.reciprocal(out=sm[b][:, 1:2], in_=sm[b][:, 0:1]), reads=[f'sm{b}'], writes=[f'sm{b}'])
            P.op('act', lambda e, b=b: e.activation(out=junk2[:, :], in_=psO[:, 0:256], func=AF.Square, accum_out=sm[b][:, 2:3]), reads=['psO', f'sm{b}'], writes=['junk2', f'sm{b}'])
            P.op('dve', lambda e, b=b: e.tensor_scalar(out=sm[b][:, 3:4], in0=sm[b][:, 2:3], scalar1=sm[b][:, 1:2], scalar2=sm[b][:, 1:2], op0=ALU.mult, op1=ALU.mult), reads=[f'sm{b}'], writes=[f'sm{b}'])
            P.op('dve', lambda e, b=b: e.tensor_scalar(out=sm[b][:, 4:5], in0=sm[b][:, 3:4], scalar1=1.0 / 256.0, scalar2=EPS, op0=ALU.mult, op1=ALU.add), reads=[f'sm{b}'], writes=[f'sm{b}'])
            P.op('act', lambda e, b=b: e.activation(out=sm[b][:, 6:7], in_=sm[b][:, 4:5], func=AF.Sqrt), reads=[f'sm{b}'], writes=[f'sm{b}'])
            P.op('dve', lambda e, b=b: e.reciprocal(out=sm[b][:, 7:8], in_=sm[b][:, 6:7]), reads=[f'sm{b}'], writes=[f'sm{b}'])
            P.op('dve', lambda e, b=b: e.tensor_tensor(out=sm[b][:, 5:6], in0=sm[b][:, 7:8], in1=sm[b][:, 1:2], op=ALU.mult), reads=[f'sm{b}'], writes=[f'sm{b}'])
            P.op('dve', lambda e, b=b, c=c: e.scalar_tensor_tensor(out=hmn[b][:, :], in0=psO[:, 0:256], scalar=sm[b][:, 5:6], in1=gso[:, c, :], op0=ALU.mult, op1=ALU.mult),
                 reads=['psO', f'sm{b}', ('gso', c)], writes=[f'hmn{b}'])
            for ec in range(2):
                P.op('pe', lambda e, b=b, ec=ec: e.transpose(out=pth[:, ec, :], in_=hmn[b][:, ec * 128:(ec + 1) * 128], identity=ident_b[:, :]),
                     reads=[f'hmn{b}', 'ident_b'], writes=['pth'])
            P.op('act', lambda e, b=b: e.activation(out=hT[b][:, :, :], in_=pth[:, :, :], func=AF.Copy), reads=['pth'], writes=[f'hT{b}'])
            P.dma('sp', lambda e, b=b, c=c: e.dma_start(out=mxi[c // 8][0:256, (c % 8) * 128:(c % 8 + 1) * 128].rearrange("(ec p) j -> p ec j", p=128), in_=hT[b][:, :, :]), f'h{b}',
                  reads=[f'hT{b}'], writes=[('mx_in', 'h', c)])
            for dc in range(2):
                P.op('pe', lambda e, b=b, c=c, dc=dc: e.matmul(psC[dc][:, :], lhsT=kw[b][:, dc * 128:(dc + 1) * 128], rhs=v_aug[:, c, :], start=True, stop=True),
                     reads=[f'kw{b}', ('v', c), 'v_aug'], writes=[f'psC{dc}'])
                P.op('dve', lambda e, c=c, dc=dc: e.scalar_tensor_tensor(out=Cst[:, dc, :], in0=Cst[:, dc, :], scalar=eA[:, c:c + 1], in1=psC[dc][:, :], op0=ALU.mult, op1=ALU.add),
                     reads=['Cst', 'eA', f'psC{dc}'], writes=['Cst'])
            P.op('act', lambda e: e.activation(out=Cb[:, :, :], in_=Cst[:, :, :], func=AF.Copy), reads=['Cst'], writes=['Cb'])
        P.barrier()
        s2.close()
        sA2.close()

        if stage == 'A2':
            hk = [('mx_in', 'h', c) for c in range(32)]
            for q_ in range(4):
                P.dma('sp', lambda e, q_=q_: e.dma_start(out=dbg[0:256, q_ * 1024:(q_ + 1) * 1024], in_=mxi[q_][0:256, :]), 'out', reads=hk, writes=['dbg'])
            sA2.close()
            sA.close()
            P.emit(top)
            return nc


        print('ops before A3', getattr(P, 'total', 0))
        s3 = ExitStack()
        colp = sb(s3, 'colp', [128, 3, 8])
        rowp = sb(s3, 'rowp', [128, 3, 1024])
        bl = sb(s3, 'bl', [128, 2, 1024])
        cml = sb(s3, 'cml', [128, 2, 1024])
        dcol = sb(s3, 'dcol', [128, 2])
        gwf = sb(s3, 'gwf', [128, 4, 128])
        gwb = sb(s3, 'gwb', [128, 4, 128], BF16)
        hpi = sb(s3, 'hpi', [128, 1])
        Bre = sb(s3, 'Bre', [128, 1024], BF16)
        Bim = sb(s3, 'Bim', [128, 1024], BF16)
        pwr = sb(s3, 'pwr', [128, 8, 12])
        pwi = sb(s3, 'pwi', [128, 8, 12])
        npwi = sb(s3, 'npwi', [128, 8, 12])
        yacc = sb(s3, 'yacc', [128, NT])
        gy = sb(s3, 'gy', [128, NT], BF16)
        sgb = [sb(s3, f'sgb{i}', [128, 512]) for i in range(2)]
        yso = [sb(s3, f'yso{i}', [128, 512], BF16) for i in range(2)]
        pB = [ps(s3, f'pB{i}', [128, 512]) for i in range(4)]
        pY = [ps(s3, f'pY{i}', [128, 512]) for i in range(2)]
        pGa = ps(s3, 'pGa', [128, 512])
        pGb = ps(s3, 'pGb', [128, 512])
        Ur = sb(s3, 'Ur', [128, 8, 64])
        Ui = sb(s3, 'Ui', [128, 8, 64])
        Vr = sb(s3, 'Vr', [128, 8, 64])
        Vi = sb(s3, 'Vi', [128, 8, 64])
        t1s = sb(s3, 't1s', [128, 8, 64])
        t2s = sb(s3, 't2s', [128, 8, 64])
        lamz = sb(s3, 'lamz', [128, 8, 4])
        cS = [sb(s3, f'cS{i}', [128, 2, 64]) for i in range(2)]
        cZ = sb(s3, 'cZ', [128, 2, 65])
        s3t = ExitStack()
        rt = [sb(s3t, f'rt{i}', [128, 1024]) for i in range(7)]
        ct = [sb(s3t, f'ct{i}', [128, 8]) for i in range(8)]

        P.dma('sp', lambda e: e.dma_start(out=colp[:, :, :], in_=s5c[:, :, :]), 'c', writes=['colp'])
        P.dma('sp', lambda e: e.dma_start(out=rowp[:, :, :], in_=s5r[:, :, :]), 'c', writes=['rowp'])
        P.dma('sp', lambda e: e.dma_start(out=bl[:, :, :], in_=s5b[:, :, :]), 'c', writes=['bl'])
        P.dma('sp', lambda e: e.dma_start(out=cml[:, :, :], in_=s5cm[:, :, :]), 'c', writes=['cml'])
        P.dma('sp', lambda e: e.dma_start(out=dcol[:, :], in_=s5d[:, :]), 'c', writes=['dcol'])
        P.dma('sp', lambda e: e.dma_start(out=gwf[:, :, :], in_=s5gw[:, :, :]), 'c', writes=['gwf'])
        P.op('dve', lambda e: e.tensor_copy(out=gwb[:, :, :], in_=gwf[:, :, :]), reads=['gwf'], writes=['gwb'])
        P.op('dve', lambda e: e.memset(hpi[:, :], float(np.pi / 2)), writes=['hpi'])

        def lam_bar(src, T, n, tag, srckey):
            dt_, lrd, th, sn, cs, t1, t2 = T[:7]
            k = [tag]
            P.op('act', lambda e: e.activation(out=dt_[:, 0:n], in_=src[:, 2, :], func=AF.Exp), reads=k + [srckey], writes=k)
            P.op('dve', lambda e: e.tensor_tensor(out=lrd[:, 0:n], in0=src[:, 0, :], in1=dt_[:, 0:n], op=ALU.mult), reads=k + [srckey], writes=k)
            P.op('dve', lambda e: e.tensor_tensor(out=th[:, 0:n], in0=src[:, 1, :], in1=dt_[:, 0:n], op=ALU.mult), reads=k + [srckey], writes=k)
            P.op('act', lambda e: e.activation(out=sn[:, 0:n], in_=th[:, 0:n], func=AF.Sin, scale=1.0 / 16.0), reads=k, writes=k)
            P.op('act', lambda e: e.activation(out=cs[:, 0:n], in_=th[:, 0:n], func=AF.Sin, scale=1.0 / 16.0, bias=hpi[:, 0:1]), reads=k + ['hpi'], writes=k)
            for _ in range(4):
                P.op('dve', lambda e: e.tensor_tensor(out=t1[:, 0:n], in0=cs[:, 0:n], in1=cs[:, 0:n], op=ALU.mult), reads=k, writes=k)
                P.op('dve', lambda e: e.tensor_tensor(out=t2[:, 0:n], in0=sn[:, 0:n], in1=sn[:, 0:n], op=ALU.mult), reads=k, writes=k)
                P.op('dve', lambda e: e.scalar_tensor_tensor(out=sn[:, 0:n], in0=sn[:, 0:n], scalar=2.0, in1=cs[:, 0:n], op0=ALU.mult, op1=ALU.mult), reads=k, writes=k)
                P.op('dve', lambda e: e.tensor_tensor(out=cs[:, 0:n], in0=t1[:, 0:n], in1=t2[:, 0:n], op=ALU.subtract), reads=k, writes=k)
            P.op('act', lambda e: e.activation(out=t1[:, 0:n], in_=lrd[:, 0:n], func=AF.Exp), reads=k, writes=k)
            P.op('dve', lambda e: e.tensor_tensor(out=cs[:, 0:n], in0=cs[:, 0:n], in1=t1[:, 0:n], op=ALU.mult), reads=k, writes=k)
            P.op('dve', lambda e: e.tensor_tensor(out=sn[:, 0:n], in0=sn[:, 0:n], in1=t1[:, 0:n], op=ALU.mult), reads=k, writes=k)
            return cs, sn

        car, cai = lam_bar(colp, ct, 8, 'c', 'colp')
        P.op('dve', lambda e: e.tensor_copy(out=pwr[:, :, 0], in_=car[:, 0:8]), reads=['c'], writes=['pw'])
        P.op('dve', lambda e: e.tensor_copy(out=pwi[:, :, 0], in_=cai[:, 0:8]), reads=['c'], writes=['pw'])
        for k_ in range(1, 12):
            P.op('dve', lambda e, k_=k_: e.tensor_tensor(out=ct[0][:, :], in0=pwr[:, :, k_ - 1], in1=pwr[:, :, k_ - 1], op=ALU.mult), reads=['pw', 'c'], writes=['c'])
            P.op('dve', lambda e, k_=k_: e.tensor_tensor(out=ct[1][:, :], in0=pwi[:, :, k_ - 1], in1=pwi[:, :, k_ - 1], op=ALU.mult), reads=['pw', 'c'], writes=['c'])
            P.op('dve', lambda e, k_=k_: e.tensor_tensor(out=pwr[:, :, k_], in0=ct[0][:, :], in1=ct[1][:, :], op=ALU.subtract), reads=['c', 'pw'], writes=['pw'])
            P.op('dve', lambda e, k_=k_: e.scalar_tensor_tensor(out=pwi[:, :, k_], in0=pwr[:, :, k_ - 1], scalar=2.0, in1=pwi[:, :, k_ - 1], op0=ALU.mult, op1=ALU.mult), reads=['pw'], writes=['pw'])
        P.op('dve', lambda e: e.tensor_scalar(out=npwi[:, :, :], in0=pwi[:, :, :], scalar1=-1.0, scalar2=None, op0=ALU.mult), reads=['pw'], writes=['npw'])

        rar, rai = lam_bar(rowp, rt, 1024, 'r', 'rowp')
        R = ['r']
        P.op('dve', lambda e: e.tensor_scalar(out=rar[:, :], in0=rar[:, :], scalar1=-1.0, scalar2=None, op0=ALU.add), reads=R, writes=R)
        P.op('dve', lambda e: e.tensor_tensor(out=rt[0][:, :], in0=rowp[:, 0, :], in1=rowp[:, 0, :], op=ALU.mult), reads=R + ['rowp'], writes=R)
        P.op('dve', lambda e: e.tensor_tensor(out=rt[1][:, :], in0=rowp[:, 1, :], in1=rowp[:, 1, :], op=ALU.mult), reads=R + ['rowp'], writes=R)
        P.op('dve', lambda e: e.tensor_tensor(out=rt[0][:, :], in0=rt[0][:, :], in1=rt[1][:, :], op=ALU.add), reads=R, writes=R)
        P.op('dve', lambda e: e.reciprocal(out=rt[0][:, :], in_=rt[0][:, :]), reads=R, writes=R)
        P.op('dve', lambda e: e.tensor_tensor(out=rt[1][:, :], in0=rar[:, :], in1=rowp[:, 0, :], op=ALU.mult), reads=R + ['rowp'], writes=R)
        P.op('dve', lambda e: e.tensor_tensor(out=rt[2][:, :], in0=rai[:, :], in1=rowp[:, 1, :], op=ALU.mult), reads=R + ['rowp'], writes=R)
        P.op('dve', lambda e: e.tensor_tensor(out=rt[1][:, :], in0=rt[1][:, :], in1=rt[2][:, :], op=ALU.add), reads=R, writes=R)
        P.op('dve', lambda e: e.tensor_tensor(out=rt[1][:, :], in0=rt[1][:, :], in1=rt[0][:, :], op=ALU.mult), reads=R, writes=R)
        P.op('dve', lambda e: e.tensor_tensor(out=rt[2][:, :], in0=rai[:, :], in1=rowp[:, 0, :], op=ALU.mult), reads=R + ['rowp'], writes=R)
        P.op('dve', lambda e: e.tensor_tensor(out=rt[5][:, :], in0=rar[:, :], in1=rowp[:, 1, :], op=ALU.mult), reads=R + ['rowp'], writes=R)
        P.op('dve', lambda e: e.tensor_tensor(out=rt[2][:, :], in0=rt[2][:, :], in1=rt[5][:, :], op=ALU.subtract), reads=R, writes=R)
        P.op('dve', lambda e: e.tensor_tensor(out=rt[2][:, :], in0=rt[2][:, :], in1=rt[0][:, :], op=ALU.mult), reads=R, writes=R)
        P.op('dve', lambda e: e.tensor_tensor(out=rt[5][:, :], in0=rt[1][:, :], in1=bl[:, 0, :], op=ALU.mult), reads=R + ['bl'], writes=R)
        P.op('dve', lambda e: e.tensor_tensor(out=rt[6][:, :], in0=rt[2][:, :], in1=bl[:, 1, :], op=ALU.mult), reads=R + ['bl'], writes=R)
        P.op('dve', lambda e: e.tensor_tensor(out=Bre[:, :], in0=rt[5][:, :], in1=rt[6][:, :], op=ALU.subtract), reads=R, writes=['Bre'])
        P.op('dve', lambda e: e.tensor_tensor(out=rt[5][:, :], in0=rt[1][:, :], in1=bl[:, 1, :], op=ALU.mult), reads=R + ['bl', 'Bre'], writes=R)
        P.op('dve', lambda e: e.tensor_tensor(out=rt[6][:, :], in0=rt[2][:, :], in1=bl[:, 0, :], op=ALU.mult), reads=R + ['bl'], writes=R)
        P.op('dve', lambda e: e.tensor_tensor(out=Bim[:, :], in0=rt[5][:, :], in1=rt[6][:, :], op=ALU.add), reads=R, writes=['Bim'])
        P.op('dve', lambda e: e.tensor_scalar(out=cml[:, 1, :], in0=cml[:, 1, :], scalar1=-1.0, scalar2=None, op0=ALU.mult), reads=['cml'], writes=['cml'])

        P.op('dve', lambda e: e.memset(Ur[:, :, 0:1], 1.0), writes=['U'])
        P.op('dve', lambda e: e.memset(Ui[:, :, 0:1], 0.0), reads=['U'], writes=['U'])
        for k_ in range(6):
            s_ = 1 << k_
            Lr = lambda s_=s_, k_=k_: pwr[:, :, k_:k_ + 1].to_broadcast([128, 8, s_])
            Li = lambda s_=s_, k_=k_: pwi[:, :, k_:k_ + 1].to_broadcast([128, 8, s_])
            P.op('dve', lambda e, s_=s_, Lr=Lr: e.tensor_tensor(out=t1s[:, :, 0:s_], in0=Ur[:, :, 0:s_], in1=Lr(), op=ALU.mult), reads=['U', 'pw'], writes=['t1s'])
            P.op('dve', lambda e, s_=s_, Li=Li: e.tensor_tensor(out=t2s[:, :, 0:s_], in0=Ui[:, :, 0:s_], in1=Li(), op=ALU.mult), reads=['U', 'pw'], writes=['t2s'])
            P.op('dve', lambda e, s_=s_: e.tensor_tensor(out=Ur[:, :, s_:2 * s_], in0=t1s[:, :, 0:s_], in1=t2s[:, :, 0:s_], op=ALU.subtract), reads=['t1s', 't2s', 'U'], writes=['U2'])
            P.op('dve', lambda e, s_=s_, Li=Li: e.tensor_tensor(out=t1s[:, :, 0:s_], in0=Ur[:, :, 0:s_], in1=Li(), op=ALU.mult), reads=['U', 'U2', 'pw'], writes=['t1s'])
            P.op('dve', lambda e, s_=s_, Lr=Lr: e.tensor_tensor(out=t2s[:, :, 0:s_], in0=Ui[:, :, 0:s_], in1=Lr(), op=ALU.mult), reads=['U', 'U2', 'pw'], writes=['t2s'])
            P.op('dve', lambda e, s_=s_: e.tensor_tensor(out=Ui[:, :, s_:2 * s_], in0=t1s[:, :, 0:s_], in1=t2s[:, :, 0:s_], op=ALU.add), reads=['t1s', 't2s', 'U2'], writes=['U'])
        P.op('dve', lambda e: e.tensor_tensor(out=t1s[:, :, :], in0=Ur[:, :, :], in1=Ur[:, :, :], op=ALU.mult), reads=['U'], writes=['t1s'])
        P.op('dve', lambda e: e.tensor_tensor(out=t2s[:, :, :], in0=Ui[:, :, :], in1=Ui[:, :, :], op=ALU.mult), reads=['U'], writes=['t2s'])
        P.op('dve', lambda e: e.tensor_tensor(out=t1s[:, :, :], in0=t1s[:, :, :], in1=t2s[:, :, :], op=ALU.add), reads=['t1s', 't2s'], writes=['t1s'])
        P.op('dve', lambda e: e.reciprocal(out=t1s[:, :, :], in_=t1s[:, :, :]), reads=['t1s'], writes=['t1s'])
        P.op('dve', lambda e: e.tensor_tensor(out=Vr[:, :, :], in0=Ur[:, :, :], in1=t1s[:, :, :], op=ALU.mult), reads=['U', 't1s'], writes=['V'])
        P.op('dve', lambda e: e.scalar_tensor_tensor(out=Vi[:, :, :], in0=Ui[:, :, :], scalar=-1.0, in1=t1s[:, :, :], op0=ALU.mult, op1=ALU.mult), reads=['U', 't1s', 'V'], writes=['V'])
        P.op('dve', lambda e: e.tensor_copy(out=lamz[:, :, 0:1], in_=Ur[:, :, 63:64]), reads=['U'], writes=['lamz'])
        P.op('dve', lambda e: e.tensor_copy(out=lamz[:, :, 1:2], in_=Ui[:, :, 63:64]), reads=['U', 'lamz'], writes=['lamz'])
        P.op('dve', lambda e: e.tensor_scalar(out=lamz[:, :, 2:3], in0=Ui[:, :, 63:64], scalar1=-1.0, scalar2=None, op0=ALU.mult), reads=['U', 'lamz'], writes=['lamz'])
        P.barrier()
        s3t.close()
        BT = [sb(s3, f'BT{i}', [128, NT]) for i in range(5)]
        msk = sb(s3, 'msk', [128, NT])
        P.op('pool', lambda e: e.memset(msk[:, :], 1.0), writes=['msk'])
        P.op('pool', lambda e: e.memset(msk[:, 0:NT:64], 0.0), reads=['msk'], writes=['msk'])
        P.op('dve', lambda e: e.memset(cZ[:, :, 0:1], 0.0), writes=['cZ0'])

        def v3(t):
            return t[:, :].rearrange("p (c t) -> p c t", t=64)
        for q in range(8):
            cc = q // 4
            ukeys = [('uT', cc, tb) for tb in range(8)]
            for nb in range(8):
                ns = slice(nb * 512, (nb + 1) * 512)
                pr, pi_ = pB[(nb % 2) * 2], pB[(nb % 2) * 2 + 1]
                P.op('pe', lambda e, q=q, cc=cc, ns=ns, pr=pr: e.matmul(pr[:, :], lhsT=Bre[:, q * 128:(q + 1) * 128], rhs=uT[:, cc, ns], start=True, stop=True),
                     reads=ukeys + ['Bre'], writes=[('pB', (nb % 2) * 2)])
                P.op('pe', lambda e, q=q, cc=cc, ns=ns, pi_=pi_: e.matmul(pi_[:, :], lhsT=Bim[:, q * 128:(q + 1) * 128], rhs=uT[:, cc, ns], start=True, stop=True),
                     reads=ukeys + ['Bim'], writes=[('pB', (nb % 2) * 2 + 1)])
                P.op('act', lambda e, ns=ns, pr=pr: e.activation(out=BT[0][:, ns], in_=pr[:, :], func=AF.Copy), reads=[('pB', (nb % 2) * 2)], writes=['BT0'])
                P.op('dve', lambda e, ns=ns, pi_=pi_: e.tensor_copy(out=BT[1][:, ns], in_=pi_[:, :]), reads=[('pB', (nb % 2) * 2 + 1)], writes=['BT1'])
            A_, B_, C_, D_, E_ = BT
            F_ = D_
            tb_ = lambda T, q=q: T[:, q, None, :].to_broadcast([128, 64, 64])
            P.op('pool', lambda e, tb_=tb_: e.tensor_tensor(out=v3(C_), in0=v3(A_), in1=tb_(Vr), op=ALU.mult), reads=['BT0', 'V'], writes=['BT2'])
            P.op('pool', lambda e, tb_=tb_: e.tensor_tensor(out=v3(D_), in0=v3(B_), in1=tb_(Vi), op=ALU.mult), reads=['BT1', 'V'], writes=['BT3'])
            P.op('dve', lambda e: e.tensor_tensor(out=C_[:, :], in0=C_[:, :], in1=D_[:, :], op=ALU.subtract), reads=['BT2', 'BT3'], writes=['BT2'])
            P.op('pool', lambda e, tb_=tb_: e.tensor_tensor(out=v3(D_), in0=v3(A_), in1=tb_(Vi), op=ALU.mult), reads=['BT0', 'V', 'BT2'], writes=['BT3'])
            P.op('pool', lambda e, tb_=tb_: e.tensor_tensor(out=v3(E_), in0=v3(B_), in1=tb_(Vr), op=ALU.mult), reads=['BT1', 'V'], writes=['BT4'])
            P.op('dve', lambda e: e.tensor_tensor(out=D_[:, :], in0=D_[:, :], in1=E_[:, :], op=ALU.add), reads=['BT3', 'BT4'], writes=['BT3'])
            P.op('dve', lambda e: e.tensor_tensor_scan(out=A_[:, :], data0=msk[:, :], data1=C_[:, :], initial=0.0, op0=ALU.mult, op1=ALU.add), reads=['msk', 'BT2'], writes=['BT0'])
            P.op('dve', lambda e: e.tensor_tensor_scan(out=B_[:, :], data0=msk[:, :], data1=D_[:, :], initial=0.0, op0=ALU.mult, op1=ALU.add), reads=['msk', 'BT3'], writes=['BT1'])
            P.op('dve', lambda e, q=q: e.tensor_scalar(out=cS[0][:, 0, :], in0=v3(A_)[:, :, 63], scalar1=lamz[:, q, 0:1], scalar2=None, op0=ALU.mult), reads=['BT0', 'lamz'], writes=['cS0'])
            P.op('dve', lambda e, q=q: e.scalar_tensor_tensor(out=cS[0][:, 0, :], in0=v3(B_)[:, :, 63], scalar=lamz[:, q, 2:3], in1=cS[0][:, 0, :], op0=ALU.mult, op1=ALU.add), reads=['BT1', 'lamz', 'cS0'], writes=['cS0'])
            P.op('dve', lambda e, q=q: e.tensor_scalar(out=cS[0][:, 1, :], in0=v3(B_)[:, :, 63], scalar1=lamz[:, q, 0:1], scalar2=None, op0=ALU.mult), reads=['BT1', 'lamz', 'cS0'], writes=['cS0'])
            P.op('dve', lambda e, q=q: e.scalar_tensor_tensor(out=cS[0][:, 1, :], in0=v3(A_)[:, :, 63], scalar=lamz[:, q, 1:2], in1=cS[0][:, 1, :], op0=ALU.mult, op1=ALU.add), reads=['BT0', 'lamz', 'cS0'], writes=['cS0'])
            cur = 0
            for j_ in range(6):
                sft = 1 << j_
                k_ = 6 + j_
                nxt = 1 - cur
                s_, d_ = cS[cur], cS[nxt]
                ks, kd = f'cS{cur}', f'cS{nxt}'
                P.op('dve', lambda e, s_=s_, d_=d_, sft=sft, q=q, k_=k_: e.scalar_tensor_tensor(out=d_[:, 0, sft:], in0=s_[:, 0, 0:64 - sft], scalar=pwr[:, q, k_:k_ + 1], in1=s_[:, 0, sft:], op0=ALU.mult, op1=ALU.add), reads=[ks, 'pw'], writes=[kd])
                P.op('dve', lambda e, s_=s_, d_=d_, sft=sft, q=q, k_=k_: e.scalar_tensor_tensor(out=d_[:, 0, sft:], in0=s_[:, 1, 0:64 - sft], scalar=npwi[:, q, k_:k_ + 1], in1=d_[:, 0, sft:], op0=ALU.mult, op1=ALU.add), reads=[ks, 'npw', kd], writes=[kd])
                P.op('dve', lambda e, s_=s_, d_=d_, sft=sft, q=q, k_=k_: e.scalar_tensor_tensor(out=d_[:, 1, sft:], in0=s_[:, 1, 0:64 - sft], scalar=pwr[:, q, k_:k_ + 1], in1=s_[:, 1, sft:], op0=ALU.mult, op1=ALU.add), reads=[ks, 'pw', kd], writes=[kd])
                P.op('dve', lambda e, s_=s_, d_=d_, sft=sft, q=q, k_=k_: e.scalar_tensor_tensor(out=d_[:, 1, sft:], in0=s_[:, 0, 0:64 - sft], scalar=pwi[:, q, k_:k_ + 1], in1=d_[:, 1, sft:], op0=ALU.mult, op1=ALU.add), reads=[ks, 'pw', kd], writes=[kd])
                P.op('dve', lambda e, s_=s_, d_=d_, sft=sft: e.tensor_copy(out=d_[:, :, 0:sft], in_=s_[:, :, 0:sft]), reads=[ks, kd], writes=[kd])
                cur = nxt
            Xc = cS[cur]
            kx = f'cS{cur}'
            P.op('dve', lambda e, q=q, Xc=Xc: e.tensor_scalar(out=cZ[:, 0, 1:65], in0=Xc[:, 0, :], scalar1=pwr[:, q, 0:1], scalar2=None, op0=ALU.mult), reads=[kx, 'pw', 'cZ0'], writes=['cZ'])
            P.op('dve', lambda e, q=q, Xc=Xc: e.scalar_tensor_tensor(out=cZ[:, 0, 1:65], in0=Xc[:, 1, :], scalar=npwi[:, q, 0:1], in1=cZ[:, 0, 1:65], op0=ALU.mult, op1=ALU.add), reads=[kx, 'npw', 'cZ'], writes=['cZ'])
            P.op('dve', lambda e, q=q, Xc=Xc: e.tensor_scalar(out=cZ[:, 1, 1:65], in0=Xc[:, 1, :], scalar1=pwr[:, q, 0:1], scalar2=None, op0=ALU.mult), reads=[kx, 'pw', 'cZ'], writes=['cZ'])
            P.op('dve', lambda e, q=q, Xc=Xc: e.scalar_tensor_tensor(out=cZ[:, 1, 1:65], in0=Xc[:, 0, :], scalar=pwi[:, q, 0:1], in1=cZ[:, 1, 1:65], op0=ALU.mult, op1=ALU.add), reads=[kx, 'pw', 'cZ'], writes=['cZ'])
            P.op('dve', lambda e, tb_=tb_: e.tensor_tensor(out=v3(A_), in0=v3(A_), in1=cZ[:, 0, 0:64, None].to_broadcast([128, 64, 64]), op=ALU.add), reads=['BT0', 'cZ'], writes=['BT0'])
            P.op('dve', lambda e, tb_=tb_: e.tensor_tensor(out=v3(B_), in0=v3(B_), in1=cZ[:, 1, 0:64, None].to_broadcast([128, 64, 64]), op=ALU.add), reads=['BT1', 'cZ'], writes=['BT1'])
            P.op('pool', lambda e, tb_=tb_: e.tensor_tensor(out=v3(C_), in0=v3(A_), in1=tb_(Ur), op=ALU.mult), reads=['BT0', 'U'], writes=['BT2'])
            P.op('pool', lambda e, tb_=tb_: e.tensor_tensor(out=v3(D_), in0=v3(B_), in1=tb_(Ui), op=ALU.mult), reads=['BT1', 'U'], writes=['BT3'])
            P.op('dve', lambda e: e.tensor_tensor(out=C_[:, :], in0=C_[:, :], in1=D_[:, :], op=ALU.subtract), reads=['BT2', 'BT3'], writes=['BT2'])
            P.op('dve', lambda e, tb_=tb_: e.tensor_tensor(out=v3(E_), in0=v3(A_), in1=tb_(Ui), op=ALU.mult), reads=['BT0', 'U'], writes=['BT4'])
            P.op('pool', lambda e, tb_=tb_: e.tensor_tensor(out=v3(F_), in0=v3(B_), in1=tb_(Ur), op=ALU.mult), reads=['BT1', 'U'], writes=['BT3'])
            P.op('dve', lambda e: e.tensor_tensor(out=E_[:, :], in0=E_[:, :], in1=F_[:, :], op=ALU.add), reads=['BT4', 'BT3'], writes=['BT4'])
            fr_, fi_ = C_, E_
            for nb in range(8):
                ns = slice(nb * 512, (nb + 1) * 512)
                py = pY[nb % 2]
                P.op('pe', lambda e, q=q, ns=ns, py=py, fr_=fr_: e.matmul(py[:, :], lhsT=cml[:, 0, q * 128:(q + 1) * 128], rhs=fr_[:, ns], start=True, stop=False),
                     reads=['cml', 'BT2'], writes=[('pY', nb % 2)])
                P.op('pe', lambda e, q=q, ns=ns, py=py, fi_=fi_: e.matmul(py[:, :], lhsT=cml[:, 1, q * 128:(q + 1) * 128], rhs=fi_[:, ns], start=False, stop=True),
                     reads=['cml', 'BT4'], writes=[('pY', nb % 2)])
                if q % 4 == 0:
                    P.op('dve', lambda e, cc=cc, ns=ns, py=py: e.scalar_tensor_tensor(out=yacc[:, ns], in0=uT[:, cc, ns], scalar=dcol[:, cc:cc + 1], in1=py[:, :], op0=ALU.mult, op1=ALU.add),
                         reads=ukeys + ['dcol', ('pY', nb % 2)], writes=[('yacc', nb)])
                else:
                    P.op('dve', lambda e, ns=ns, py=py: e.tensor_tensor(out=yacc[:, ns], in0=yacc[:, ns], in1=py[:, :], op=ALU.add),
                         reads=[('yacc', nb), ('pY', nb % 2)], writes=[('yacc', nb)])
            if q % 4 == 3:
                for nb in range(8):
                    ns = slice(nb * 512, (nb + 1) * 512)
                    b = nb % 2
                    P.op('act', lambda e, ns=ns: e.activation(out=gy[:, ns], in_=yacc[:, ns], func=AF.Gelu), reads=[('yacc', nb)], writes=[('gy', nb)])
                    P.op('pe', lambda e, cc=cc, ns=ns: e.matmul(pGa[:, :], lhsT=gwb[:, cc * 2, :], rhs=gy[:, ns], start=True, stop=True), reads=['gwb', ('gy', nb)], writes=['pGa'])
                    P.op('pe', lambda e, cc=cc, ns=ns: e.matmul(pGb[:, :], lhsT=gwb[:, cc * 2 + 1, :], rhs=gy[:, ns], start=True, stop=True), reads=['gwb', ('gy', nb)], writes=['pGb'])
                    P.op('act', lambda e, b=b: e.activation(out=sgb[b][:, :], in_=pGb[:, :], func=AF.Sigmoid), reads=['pGb'], writes=[f'sgb{b}'])
                    P.op('dve', lambda e, b=b: e.tensor_tensor(out=yso[b][:, :], in0=pGa[:, :], in1=sgb[b][:, :], op=ALU.mult), reads=['pGa', f'sgb{b}'], writes=[f'yso{b}'])
                    P.dma('sp', lambda e, cc=cc, nb=nb, b=b: e.dma_start(out=mxi[nb // 2][256 + cc * 128:256 + (cc + 1) * 128, (nb % 2) * 512:(nb % 2 + 1) * 512], in_=yso[b][:, :]), f'ys{b}',
                          reads=[f'yso{b}'], writes=[('mx_in', 'y', cc, nb)])
        P.barrier()
        s3.close()
        if stage == 'A3':
            for q_ in range(4):
                P.dma('sp', lambda e, q_=q_: e.dma_start(out=dbg[:, q_ * 1024:(q_ + 1) * 1024], in_=mxi[q_][:, :]), 'out', writes=['dbg'], force=True)
            sA2.close()
            sA.close()
            P.emit(top)
            return nc

        sA.close()
        print('ops before exchange', getattr(P, 'total', 0))
        NEI = int(os.environ.get('KNE', '128'))
        if stage == 'simB':
            mxsrc = [mx_test[:, q_ * 1024:(q_ + 1) * 1024] for q_ in range(4)]
        else:
            allk = [('mx_in', 'h', c) for c in range(32)] + [('mx_in', 'y', cc, nb) for cc in range(2) for nb in range(8)]
            for q_ in range(4):
                P.dma('pool', lambda e, q_=q_: e.collective_compute("AllGather", ALU.bypass, replica_groups=[[0, 1, 2, 3], [4, 5, 6, 7]],
                                                                   ins=[mxi[q_].ap().opt()], outs=[mxa[q_].ap().opt()]), f'cc{q_}', reads=allk, writes=[('mx_all', q_)], inc=1)
            mxsrc = [mxa[q_].ap() for q_ in range(4)]
        sB = ExitStack()
        h1 = sb(sB, 'h1', [128, 8, DM])
        xn2T = sb(sB, 'xn2T', [128, KC, 1024], BF16)
        em = sb(sB, 'em', [128, 8, 16, 128], BF16)
        sc = sb(sB, 'sc', [128, 8, 8, 2])
        selt = sb(sB, 'selt', [128, 4])
        g2t = sb(sB, 'g2t', [128, KC])
        ssq2 = [sb(sB, f'ssq2{i}', [128, 1]) for i in range(2)]
        rstd2 = [sb(sB, f'rstd2{i}', [128, 1]) for i in range(2)]
        P.dma('sp', lambda e: e.dma_start(out=selt[:, :], in_=selin[:, :]), 'c', writes=['selt'])
        P.dma('sp', lambda e: e.dma_start(out=g2t[:, :], in_=g2c[:, :]), 'c', writes=['g2t'])
        P.dma('sp', lambda e: e.dma_start(out=h1[:, :, :], in_=x_own.rearrange("(tt p) d -> p tt d", p=128)), 'c', writes=['h1'])

        print('ops before B1', getattr(P, 'total', 0))
        sb1 = ExitStack()
        mixo = sb(sb1, 'mixo', [128, KC, 1024], BF16)
        wb = sb(sb1, 'wb', [128, KC, 512], BF16)
        mst = [sb(sb1, f'mst{i}', [128, 1024], BF16) for i in range(2)]
        wst2 = [sb(sb1, f'wst2{i}', [128, 512]) for i in range(2)]
        xs2 = [sb(sb1, f'xs2{i}', [128, DM], BF16) for i in range(2)]
        pM = [ps(sb1, f'pM{i}', [128, 512]) for i in range(2)]
        ptr2 = [ps(sb1, f'ptr2{i}', [128, 8, 128], BF16) for i in range(2)]

        for kc in range(KC):
            for q_ in range(4):
                b = (kc * 4 + q_) % 2
                P.dma('sp', lambda e, kc=kc, q_=q_, b=b: e.dma_start(out=mst[b][:, :], in_=mxsrc[q_][kc * 128:(kc + 1) * 128, :]), f'ms{b}',
                      reads=[('mx_all', q_)], writes=[f'mst{b}'])
                if q_ == 0:
                    P.op('dve', lambda e, kc=kc, b=b: e.tensor_scalar(out=mixo[:, kc, :], in0=mst[b][:, :], scalar1=selt[:, 0:1], scalar2=None, op0=ALU.mult),
                         reads=[f'mst{b}', 'selt'], writes=['mixo'])
                else:
                    P.op('dve', lambda e, kc=kc, b=b, q_=q_: e.scalar_tensor_tensor(out=mixo[:, kc, :], in0=mst[b][:, :], scalar=selt[:, q_:q_ + 1], in1=mixo[:, kc, :], op0=ALU.mult, op1=ALU.add),
                         reads=[f'mst{b}', 'selt', 'mixo'], writes=['mixo'])
        for nblk in range(4):
            for kc in range(KC):
                b = kc % 2
                P.dma('sp', lambda e, kc=kc, b=b, nblk=nblk: e.dma_start(out=wst2[b][:, :], in_=w_o[:, kc, nblk * 512:(nblk + 1) * 512]), f'wo{b}', writes=[f'wst2{b}'])
                P.op('act', lambda e, kc=kc, b=b: e.activation(out=wb[:, kc, :], in_=wst2[b][:, :], func=AF.Copy), reads=[f'wst2{b}'], writes=['wb'])
            for tt in range(8):
                pb_ = tt % 2
                for kc in range(KC):
                    P.op('pe', lambda e, kc=kc, tt=tt, pb_=pb_: e.matmul(pM[pb_][:, :], lhsT=mixo[:, kc, tt * 128:(tt + 1) * 128], rhs=wb[:, kc, :], start=(kc == 0), stop=(kc == KC - 1)),
                         reads=['mixo', 'wb'], writes=[f'pM{pb_}'])
                P.op('dve', lambda e, tt=tt, pb_=pb_, nblk=nblk: e.tensor_tensor(out=h1[:, tt, nblk * 512:(nblk + 1) * 512], in0=h1[:, tt, nblk * 512:(nblk + 1) * 512], in1=pM[pb_][:, :], op=ALU.add),
                     reads=[f'pM{pb_}', 'h1'], writes=['h1'])

        def rms(tt, b, jt, jk):
            P.op('act', lambda e: e.activation(out=jt[:, :], in_=h1[:, tt, :], func=AF.Square, accum_out=ssq2[b][:, :]), reads=['h1'], writes=[jk, f'ssq2{b}'])
            P.op('dve', lambda e: e.tensor_scalar(out=rstd2[b][:, :], in0=ssq2[b][:, :], scalar1=1.0 / DM, scalar2=EPS, op0=ALU.mult, op1=ALU.add), reads=[f'ssq2{b}'], writes=[f'rstd2{b}'])
            P.op('act', lambda e: e.activation(out=rstd2[b][:, :], in_=rstd2[b][:, :], func=AF.Sqrt), reads=[f'rstd2{b}'], writes=[f'rstd2{b}'])
            P.op('dve', lambda e: e.reciprocal(out=rstd2[b][:, :], in_=rstd2[b][:, :]), reads=[f'rstd2{b}'], writes=[f'rstd2{b}'])
        for tt in range(8):
            b = tt % 2
            rms(tt, b, xs2[b], f'xs2{b}')
            P.op('dve', lambda e, tt=tt, b=b: e.tensor_scalar(out=xs2[b][:, :], in0=h1[:, tt, :], scalar1=rstd2[b][:, 0:1], scalar2=None, op0=ALU.mult), reads=['h1', f'rstd2{b}'], writes=[f'xs2{b}'])
            for half in range(2):
                for j in range(8):
                    kc = half * 8 + j
                    P.op('pe', lambda e, b=b, kc=kc, half=half, j=j: e.transpose(out=ptr2[half][:, j, :], in_=xs2[b][:, kc * 128:(kc + 1) * 128], identity=ident_b[:, :]),
                         reads=[f'xs2{b}', 'ident_b'], writes=[f'ptr2{half}'])
                P.op('dve', lambda e, half=half, tt=tt: e.tensor_tensor(out=xn2T[:, half * 8:half * 8 + 8, tt * 128:(tt + 1) * 128], in0=ptr2[half][:, :, :],
                                                                         in1=g2t[:, half * 8:half * 8 + 8, None].to_broadcast([128, 8, 128]), op=ALU.mult),
                     reads=[f'ptr2{half}', 'g2t'], writes=['xn2T'])
        P.barrier()
        sb1.close()

        print('ops before B2', getattr(P, 'total', 0))
        sb2 = ExitStack()
        v16 = sb(sb2, 'v16', [128, 8, 16, 16])
        skf = sb(sb2, 'skf', [128, 2, 128])
        skb = sb(sb2, 'skb', [128, 2, 128], BF16)
        wqst = [sb(sb2, f'wqst{i}', [128, KC, 128]) for i in range(2)]
        wqb = [sb(sb2, f'wqb{i}', [128, KC, 128], BF16) for i in range(2)]
        qj = [sb(sb2, f'qj{i}', [128, 1024], BF16) for i in range(2)]
        ebf = [sb(sb2, f'ebf{i}', [128, 128], BF16) for i in range(2)]
        ef = [sb(sb2, f'ef{i}', [128, 128]) for i in range(2)]
        ef2 = [sb(sb2, f'ef2{i}', [128, 128]) for i in range(2)]
        cand = [sb(sb2, f'cand{i}', [128, 256]) for i in range(2)]
        cand2 = [sb(sb2, f'cand2{i}', [128, 256]) for i in range(2)]
        c16 = [sb(sb2, f'c16{i}', [128, 16]) for i in range(2)]
        pQ = [ps(sb2, f'pQ{i}', [128, 512]) for i in range(2)]
        pS = [ps(sb2, f'pS{i}', [128, 128]) for i in range(2)]
        P.dma('sp', lambda e: e.dma_start(out=skf[:, :, :], in_=skT[:, :, :]), 'c', writes=['skf'])
        P.op('dve', lambda e: e.tensor_copy(out=skb[:, :, :], in_=skf[:, :, :]), reads=['skf'], writes=['skb'])
        for j in range(16):
            b = j % 2
            half = j % 2
            P.dma('sp', lambda e, j=j, b=b: e.dma_start(out=wqst[b][:, :, :], in_=w_q[:, :, j * 128:(j + 1) * 128]), f'wq{b}', writes=[f'wqst{b}'])
            P.op('dve', lambda e, b=b: e.tensor_copy(out=wqb[b][:, :, :], in_=wqst[b][:, :, :]),
                 reads=[f'wqst{b}'], writes=[f'wqb{b}'])
            for th in range(2):
                for kc in range(KC):
                    P.op('pe', lambda e, b=b, kc=kc, th=th: e.matmul(pQ[th][:, :], lhsT=wqb[b][:, kc, :], rhs=xn2T[:, kc, th * 512:(th + 1) * 512], start=(kc == 0), stop=(kc == KC - 1)),
                         reads=[f'wqb{b}', 'xn2T'], writes=[f'pQ{th}'])
                P.op('act', lambda e, b=b, th=th: e.activation(out=qj[b][:, th * 512:(th + 1) * 512], in_=pQ[th][:, :], func=AF.Copy), reads=[f'pQ{th}'], writes=[f'qj{b}'])
            for tt in range(8):
                b2 = tt % 2
                P.op('pe', lambda e, b=b, tt=tt, b2=b2, half=half: e.matmul(pS[b2][:, :], lhsT=qj[b][:, tt * 128:(tt + 1) * 128], rhs=skb[:, half, :], start=True, stop=True),
                     reads=[f'qj{b}', 'skb'], writes=[f'pS{b2}'])
                P.op('act', lambda e, b2=b2: e.activation(out=ebf[b2][:, :], in_=pS[b2][:, :], func=AF.Exp), reads=[f'pS{b2}'], writes=[f'ebf{b2}'])
                P.op('dve', lambda e, b2=b2: e.tensor_copy(out=ef[b2][:, :], in_=ebf[b2][:, :]), reads=[f'ebf{b2}'], writes=[f'ef{b2}'])
                P.op('dve', lambda e, b2=b2, tt=tt, j=j: e.max(out=v16[:, tt, j, 0:8], in_=ef[b2][:, :]), reads=[f'ef{b2}'], writes=['v16'])
                P.op('dve', lambda e, b2=b2, tt=tt, j=j: e.match_replace(out=ef2[b2][:, :], in_to_replace=v16[:, tt, j, 0:8], in_values=ef[b2][:, :], imm_value=-1.0), reads=[f'ef{b2}', 'v16'], writes=[f'ef2{b2}'])
                P.op('dve', lambda e, b2=b2, tt=tt, j=j: e.max(out=v16[:, tt, j, 8:16], in_=ef2[b2][:, :]), reads=[f'ef2{b2}'], writes=['v16'])
                P.op('dve', lambda e, b2=b2, tt=tt, j=j: e.scalar_tensor_tensor(out=em[:, tt, j, :], in0=ef[b2][:, :], scalar=v16[:, tt, j, 15:16], in1=ef[b2][:, :], op0=ALU.is_ge, op1=ALU.mult),
                     reads=[f'ef{b2}', 'v16'], writes=['em'])
        for tt in range(8):
            for hh in range(8):
                b = hh % 2
                P.op('dve', lambda e, tt=tt, hh=hh, b=b: e.tensor_tensor(out=cand[b][:, :].rearrange("p (a c) -> p a c", a=16), in0=v16[:, tt, 2 * hh, :, None].to_broadcast([128, 16, 16]),
                                                                          in1=v16[:, tt, 2 * hh + 1, None, :].to_broadcast([128, 16, 16]), op=ALU.mult), reads=['v16'], writes=[f'cand{b}'])
                P.op('dve', lambda e, b=b: e.max(out=c16[b][:, 0:8], in_=cand[b][:, :]), reads=[f'cand{b}'], writes=[f'c16{b}'])
                P.op('dve', lambda e, b=b: e.match_replace(out=cand2[b][:, :], in_to_replace=c16[b][:, 0:8], in_values=cand[b][:, :], imm_value=-1.0), reads=[f'cand{b}', f'c16{b}'], writes=[f'cand2{b}'])
                P.op('dve', lambda e, b=b: e.max(out=c16[b][:, 8:16], in_=cand2[b][:, :]), reads=[f'cand2{b}'], writes=[f'c16{b}'])
                P.op('dve', lambda e, b=b, tt=tt, hh=hh: e.tensor_scalar(out=sc[:, tt, hh, 0:1], in0=c16[b][:, 15:16], scalar1=0.999996, scalar2=None, op0=ALU.mult), reads=[f'c16{b}'], writes=['sc'])
                P.op('dve', lambda e, b=b, tt=tt, hh=hh: e.reduce_sum(out=sc[:, tt, hh, 1:2], in_=c16[b][:, :], axis=AX.X), reads=[f'c16{b}'], writes=['sc'])
        P.op('dve', lambda e: e.reciprocal(out=sc[:, :, :, 1], in_=sc[:, :, :, 1]), reads=['sc'], writes=['sc'])
        P.barrier()
        sb2.close()

        print('ops before B3', getattr(P, 'total', 0))
        EB = 4
        sb3 = ExitStack()
        Ust = [sb(sb3, f'Ust{i}', [128, 1024]) for i in range(2)]
        Vst = [sb(sb3, f'Vst{i}', [128, 1024]) for i in range(2)]
        Ub = sb(sb3, 'Ub', [128, DM], BF16)
        Vb = [sb(sb3, f'Vb{i}', [128, DM], BF16) for i in range(EB)]
        UT = sb(sb3, 'UT', [128, KC, 128], BF16)
        gelT = [sb(sb3, f'gelT{i}', [128, 1024], BF16) for i in range(2)]
        thw = [sb(sb3, f'thw{i}', [128, 3, 8, 8]) for i in range(2)]
        Yb = [sb(sb3, f'Yb{i}', [128, 8, 128], BF16) for i in range(3)]
        Dg = [sb(sb3, f'Dg{i}', [128, 8, 128], BF16) for i in range(3)]
        Mk = [sb(sb3, f'Mk{i}', [128, 8, 128], BF16) for i in range(3)]
        GAT = sb(sb3, 'GAT', [128, EB, 1024], BF16)
        ptr3 = ps(sb3, 'ptr3', [128, 8, 128], BF16)
        pA2 = [ps(sb3, f'pA2{i}', [128, 512]) for i in range(2)]
        pG = [ps(sb3, f'pG{i}', [128, 128]) for i in range(3)]
        pO = [ps(sb3, f'pO{i}', [128, 512]) for i in range(2)]
        LAG = 2
        pend = []

        def flush_one():
            il_, ib_, tt_, b_ = pend.pop(0)
            P.op('dve', lambda e: e.tensor_tensor(out=GAT[:, il_, tt_ * 128:(tt_ + 1) * 128], in0=pG[b_][:, :], in1=gelT[ib_][:, tt_ * 128:(tt_ + 1) * 128], op=ALU.mult),
                 reads=[('pG', b_), f'gelT{ib_}'], writes=[('GAT', il_, tt_)])
        def prepA_pieces(i):
            ib = i % 2
            tk = f'thw{ib}'
            pieces = []

            def p_load():
                for dh in range(2):
                    ds_ = slice(dh * 1024, (dh + 1) * 1024)
                    P.dma('sp', lambda e, dh=dh, ds_=ds_: e.dma_start(out=Ust[dh][:, :], in_=pu[i * 128:(i + 1) * 128, ds_]), f'pu{dh}', writes=[f'Ust{dh}'])
                    P.op('act', lambda e, dh=dh, ds_=ds_: e.activation(out=Ub[:, ds_], in_=Ust[dh][:, :], func=AF.Copy), reads=[f'Ust{dh}'], writes=[('Ub', dh)])
                P.op('dve', lambda e: e.tensor_scalar(out=thw[ib][:, 0, :, :], in0=em[:, :, 0:16:2, i], scalar1=1e-30, scalar2=None, op0=ALU.max), reads=['em'], writes=[tk])
                P.op('dve', lambda e: e.reciprocal(out=thw[ib][:, 0, :, :], in_=thw[ib][:, 0, :, :]), reads=[tk], writes=[tk])
                P.op('dve', lambda e: e.tensor_tensor(out=thw[ib][:, 1, :, :], in0=thw[ib][:, 0, :, :], in1=sc[:, :, :, 0], op=ALU.mult), reads=[tk, 'sc'], writes=[tk])
                P.op('dve', lambda e: e.tensor_tensor(out=thw[ib][:, 2, :, :], in0=em[:, :, 0:16:2, i], in1=sc[:, :, :, 1], op=ALU.mult), reads=['em', 'sc', tk], writes=[tk])
            pieces.append(p_load)

            def p_tr(half):
                def f():
                    for j in range(8):
                        kc = half * 8 + j
                        P.op('pe', lambda e, kc=kc, j=j: e.transpose(out=ptr3[:, j, :], in_=Ub[:, kc * 128:(kc + 1) * 128], identity=ident_b[:, :]),
                             reads=[('Ub', half), 'ident_b'], writes=['ptr3'])
                    P.op('act', lambda e: e.activation(out=UT[:, half * 8:half * 8 + 8, :], in_=ptr3[:, :, :], func=AF.Copy), reads=['ptr3'], writes=['UT'])
                return f
            pieces.append(p_tr(0))
            pieces.append(p_tr(1))

            def p_mm(th, k0, k1):
                def f():
                    for kc in range(k0, k1):
                        P.op('pe', lambda e, kc=kc: e.matmul(pA2[th][:, :], lhsT=UT[:, kc, :], rhs=xn2T[:, kc, th * 512:(th + 1) * 512], start=(kc == 0), stop=(kc == KC - 1)),
                             reads=['xn2T', 'UT'], writes=[f'pA2{th}'])
                    if k1 == KC:
                        P.op('act', lambda e: e.activation(out=gelT[ib][:, th * 512:(th + 1) * 512], in_=pA2[th][:, :], func=AF.Gelu), reads=[f'pA2{th}'], writes=[f'gelT{ib}'])
                return f
            for th in range(2):
                for k0 in (0, 6, 11):
                    pieces.append(p_mm(th, k0, {0: 6, 6: 11, 11: 16}[k0]))
            return pieces

        for pc in prepA_pieces(0):
            pc()
        for blk in range(NEI // EB):
            for il in range(EB):
                i = blk * EB + il
                ib = i % 2
                tk = f'thw{ib}'
                for dh in range(2):
                    ds_ = slice(dh * 1024, (dh + 1) * 1024)
                    P.dma('sp', lambda e, i=i, dh=dh, ds_=ds_: e.dma_start(out=Vst[dh][:, :], in_=pv_[i * 128:(i + 1) * 128, ds_]), f'pv{dh}', writes=[f'Vst{dh}'])
                    P.op('act', lambda e, dh=dh, ds_=ds_, il=il: e.activation(out=Vb[il][:, ds_], in_=Vst[dh][:, :], func=AF.Copy), reads=[f'Vst{dh}'], writes=[('Vb', il)])
                nxt = prepA_pieces(i + 1) if i + 1 < NEI else []
                if nxt:
                    nxt.pop(0)()
                for tt in range(8):
                    n = il * 8 + tt
                    b = n % 3
                    gb = n % 3
                    mb = n % 3
                    P.op('pool', lambda e, b=b, ib=ib, tt=tt: e.tensor_tensor(out=Dg[b][:, 0:4, :], in0=ident_f[:, None, :].to_broadcast([128, 4, 128]),
                                                                         in1=thw[ib][:, 2, tt, 0:4, None].to_broadcast([128, 4, 128]), op=ALU.mult),
                         reads=['ident_f', tk], writes=[(f'Dg{b}', 0)])
                    for hh in range(4, 8):
                        P.op('act', lambda e, b=b, ib=ib, tt=tt, hh=hh: e.activation(out=Dg[b][:, hh, :], in_=ident_f[:, :], func=AF.Copy, scale=thw[ib][:, 2, tt, hh:hh + 1]),
                             reads=['ident_f', tk], writes=[(f'Dg{b}', hh)])
                    P.op('dve', lambda e, mb=mb, ib=ib, tt=tt: e.tensor_tensor(out=Mk[mb][:, :, :], in0=em[:, tt, 1:16:2, :], in1=thw[ib][:, 1, tt, :, None].to_broadcast([128, 8, 128]), op=ALU.is_ge),
                         reads=['em', tk], writes=[f'Mk{mb}'])
                    P.op('dve', lambda e, b=b, mb=mb, tt=tt: e.tensor_tensor(out=Yb[b][:, 0:4, :], in0=Mk[mb][:, 0:4, :], in1=em[:, tt, 1:8:2, :], op=ALU.mult),
                         reads=['em', f'Mk{mb}'], writes=[(f'Yb{b}', 0)])
                    P.op('pool', lambda e, b=b, mb=mb, tt=tt: e.tensor_tensor(out=Yb[b][:, 4:8, :], in0=Mk[mb][:, 4:8, :], in1=em[:, tt, 9:16:2, :], op=ALU.mult),
                         reads=['em', f'Mk{mb}'], writes=[(f'Yb{b}', 1)])
                    for hh in range(8):
                        P.op('pe', lambda e, b=b, hh=hh, gb=gb: e.matmul(pG[gb][:, :], lhsT=Yb[b][:, hh, :], rhs=Dg[b][:, hh, :], start=(hh == 0), stop=(hh == 7)),
                             reads=[(f'Yb{b}', 0 if hh < 4 else 1), (f'Dg{b}', 0 if hh < 4 else hh)], writes=[('pG', gb)])
                    pend.append((il, ib, tt, gb))
                    if len(pend) > LAG:
                        flush_one()
                    if nxt:
                        nxt.pop(0)()
                while nxt:
                    nxt.pop(0)()
            while pend:
                flush_one()
            for tt in range(8):
                for nblk in range(4):
                    ob = nblk % 2
                    for il in range(EB):
                        P.op('pe', lambda e, il=il, tt=tt, nblk=nblk, ob=ob: e.matmul(pO[ob][:, :], lhsT=GAT[:, il, tt * 128:(tt + 1) * 128], rhs=Vb[il][:, nblk * 512:(nblk + 1) * 512], start=(il == 0), stop=(il == EB - 1)),
                             reads=[('GAT', il, tt), ('Vb', il)], writes=[('pO', ob)])
                    P.op('dve', lambda e, tt=tt, nblk=nblk, ob=ob: e.tensor_tensor(out=h1[:, tt, nblk * 512:(nblk + 1) * 512], in0=h1[:, tt, nblk * 512:(nblk + 1) * 512], in1=pO[ob][:, :], op=ALU.add),
                         reads=[('pO', ob), ('h1', tt, nblk)], writes=[('h1', tt, nblk)])
        P.barrier()
        for nh in range(2):
            P.dma('sp', lambda e, nh=nh: e.dma_start(out=Vst[nh][:, :], in_=gfr[:, nh * 1024:(nh + 1) * 1024]), f'pv{nh}', writes=[f'Vst{nh}'])
        for tt in range(8):
            b = tt % 2
            rms(tt, b, Ub, ('Ub', 0))
            for nh in range(2):
                P.op('dve', lambda e, tt=tt, b=b, nh=nh: e.scalar_tensor_tensor(out=Ust[nh][:, :], in0=h1[:, tt, nh * 1024:(nh + 1) * 1024], scalar=rstd2[b][:, 0:1], in1=Vst[nh][:, :], op0=ALU.mult, op1=ALU.mult),
                     reads=['h1', f'rstd2{b}', f'Vst{nh}'], writes=[f'Ust{nh}'])
                P.dma('sp', lambda e, tt=tt, nh=nh: e.dma_start(out=yout[tt * 128:(tt + 1) * 128, nh * 1024:(nh + 1) * 1024], in_=Ust[nh][:, :]), f'yo{nh}', reads=[f'Ust{nh}'], writes=[('y', tt, nh)])
        P.barrier()
        sb3.close()
        sB.close()
        P.emit(top)
    return nc


def host_inputs(inp, r):
    b, h = r // 4, r % 4
    w_in = inp['w_in'][0]
    cols = np.concatenate([
        np.arange(h * 256, (h + 1) * 256),
        1024 + np.arange(h * 256, (h + 1) * 256),
        4104 + np.arange(h * 256, (h + 1) * 256),
        2048 + np.arange(h * 256, (h + 1) * 256),
        np.array([4096 + h, 4100 + h]),
        3072 + np.arange(h * 256, (h + 1) * 256),
    ])
    w_a = np.ascontiguousarray(w_in[:, cols].reshape(KC, 128, 1282).transpose(1, 0, 2))
    g1 = np.ascontiguousarray(inp['norm1_g'][0].reshape(KC, 128).T)
    bgv = inp['b_gates'][0]
    bg = np.ascontiguousarray(np.broadcast_to(np.array([bgv[h], bgv[4 + h]], np.float32)[None, :], (128, 2)))
    cwf = inp['conv_qk_w'][0]
    chans = np.concatenate([np.arange(h * 256, (h + 1) * 256), 1024 + np.arange(h * 256, (h + 1) * 256)])
    convw = np.ascontiguousarray(cwf[:, chans].T.reshape(4, 128, 4).transpose(1, 0, 2))
    mg = np.ascontiguousarray(np.broadcast_to(inp['mlstm_norm_g'][0][h * 256:(h + 1) * 256][None, :], (128, 256)))

    G0 = 16 * h
    lre = inp['s5_lambda_re'][0][G0:G0 + 16]
    lim = inp['s5_lambda_im'][0][G0:G0 + 16]
    ldt = np.broadcast_to(inp['s5_log_dt'][0][G0:G0 + 16][:, None], (16, 64))
    def colrow(a):
        col = a.reshape(8, 128).T
        row = a.reshape(1024)
        return col, row
    cols_, rows_ = zip(*[colrow(np.asarray(a, np.float32)) for a in (lre, lim, ldt)])
    s5c = np.ascontiguousarray(np.stack(cols_, axis=1))
    s5r = np.ascontiguousarray(np.broadcast_to(np.stack(rows_, axis=0)[None], (128, 3, 1024)))
    s5b = np.zeros((128, 2, 8, 128), np.float32)
    s5cm = np.zeros((128, 2, 8, 128), np.float32)
    for ri, (bsrc, csrc) in enumerate(((inp['s5_b_re'][0], inp['s5_c_re'][0]), (inp['s5_b_im'][0], inp['s5_c_im'][0]))):
        for gl in range(16):
            q, g2 = gl // 2, gl % 2
            r0 = (gl % 8) * 16
            s5b[r0:r0 + 16, ri, q, g2 * 64:(g2 + 1) * 64] = bsrc[G0 + gl].T
            s5cm[g2 * 64:(g2 + 1) * 64, ri, q, r0:r0 + 16] = csrc[G0 + gl].T
    s5d = np.ascontiguousarray(inp['s5_d'][0][G0:G0 + 16].reshape(2, 128).T)
    s5gw = np.zeros((128, 4, 128), np.float32)
    gw = inp['s5_glu_w'][0]
    for gl in range(16):
        cc, r0 = gl // 8, (gl % 8) * 16
        s5gw[r0:r0 + 16, cc * 2, r0:r0 + 16] = gw[G0 + gl][:, :16]
        s5gw[r0:r0 + 16, cc * 2 + 1, r0:r0 + 16] = gw[G0 + gl][:, 16:]

    perm = np.concatenate([np.concatenate([256 * hh + np.arange(256), 1024 + 256 * hh + np.arange(256)]) for hh in range(4)])
    w_o = np.ascontiguousarray(inp['w_out'][0][perm].reshape(KC, 128, DM).transpose(1, 0, 2))
    w_q = np.ascontiguousarray(inp['peer_wq'][0].reshape(KC, 128, DM).transpose(1, 0, 2))
    g2 = inp['norm2_g'][0]
    g2c = np.ascontiguousarray(g2.reshape(KC, 128).T)
    g2r = np.ascontiguousarray(np.broadcast_to(g2[None, :], (128, DM)))
    gfr = np.ascontiguousarray(np.broadcast_to(inp['final_g'][None, :], (128, DM)))
    skT = np.ascontiguousarray(inp['peer_subkeys'][0].transpose(2, 0, 1))
    d = dict(
        x_own=np.ascontiguousarray(inp['x'][b][h * 1024:(h + 1) * 1024]), w_o=w_o, w_q=w_q, g2c=g2c, g2r=g2r, gfr=gfr, skT=skT,
        pu=inp['peer_u'][0], pv=inp['peer_v'][0], sel=np.ascontiguousarray(np.broadcast_to(np.eye(4, dtype=np.float32)[h][None, :], (128, 4))),
        s5c=s5c, s5r=s5r, s5b=s5b.reshape(128, 2, 1024), s5cm=s5cm.reshape(128, 2, 1024), s5d=s5d, s5gw=s5gw,
        x=np.ascontiguousarray(inp['x'][b]),
        w_a=w_a, g1=g1, bg=bg, convw=convw, mg=mg,
        c_ident=_ident(),
        c_triu=np.triu(np.ones((128, 128), np.float32)),
        c_ones=np.ones((128, 128), np.float32),
    )
    return d


def kernel(**inputs):
    inp = {k: np.asarray(v) for k, v in inputs.items()}
    nc = build('full')
    in_maps = [host_inputs(inp, r) for r in range(8)]
    res = run_bass_kernel_spmd(nc, in_maps, core_ids=list(range(8)))
    out = np.zeros((2, NT, DM), np.float32)
    for r in range(8):
        b, h = r // 4, r % 4
        out[b, h * 1024:(h + 1) * 1024] = res.results[r]['y']
    return out
```

```python
import os
import numpy as np
import ml_dtypes
from contextlib import ExitStack
import concourse.bass as bass
import concourse.mybir as mybir
from concourse.bass_utils import run_bass_kernel_spmd

F32 = mybir.dt.float32
BF16 = mybir.dt.bfloat16
ALU = mybir.AluOpType
AF = mybir.ActivationFunctionType
AX = mybir.AxisListType

EPS = 1e-6
NT = 4096
DM = 2048
KC = 16


class Prog:
    ENG = ['pe', 'dve', 'act', 'pool', 'sp']

    def __init__(self, nc):
        self.nc = nc
        self.ops = {e: [] for e in self.ENG}
        self.cnt = {e: 0 for e in self.ENG}
        self.dcnt = {}
        self.lastw = {}
        self.rds = {}
        self.floor = {}

    def _tok_add(self, d, tok):
        s, v, e = tok
        if s not in d or d[s][0] < v:
            d[s] = (v, e)

    def _mk(self, reads, writes):
        deps = dict(self.floor)
        for k in reads:
            if k in self.lastw:
                self._tok_add(deps, self.lastw[k])
        for k in writes:
            if k in self.lastw:
                self._tok_add(deps, self.lastw[k])
            for s, (v, e) in self.rds.get(k, {}).items():
                self._tok_add(deps, (s, v, e))
        return deps

    def _commit(self, tok, reads, writes):
        for k in reads:
            self._tok_add(self.rds.setdefault(k, {}), tok)
        for k in writes:
            self.lastw[k] = tok
            self.rds[k] = {}

    def _skip(self, force):
        self.total = getattr(self, 'total', 0) + 1
        cut = int(os.environ.get('KCUT', '0'))
        return bool(cut) and self.total > cut and not force

    def op(self, eng, fn, reads=(), writes=(), force=False):
        if self._skip(force):
            return
        deps = self._mk(reads, writes)
        self.cnt[eng] += 1
        tok = (eng, self.cnt[eng], eng)
        self.ops[eng].append((deps, fn, eng, 1))
        self._commit(tok, reads, writes)

    def dma(self, queue, fn, stream, reads=(), writes=(), inc=16, force=False):
        if self._skip(force):
            return
        deps = self._mk(reads, writes)
        if stream == 'c':
            self.nuniq = getattr(self, 'nuniq', 0) + 1
            stream = f'c{self.nuniq}'
        s = 'd_' + stream
        self.dcnt[s] = self.dcnt.get(s, 0) + inc
        tok = (s, self.dcnt[s], 'dma')
        self.ops[queue].append((deps, fn, s, inc))
        self._commit(tok, reads, writes)

    def barrier(self):
        for e in self.ENG:
            if self.cnt[e]:
                self.floor[e] = (self.cnt[e], e)
        for s, v in self.dcnt.items():
            self.floor[s] = (v, 'dma')

    def emit(self, stack, final_waits=True):
        nc = self.nc
        sems = {}
        for e in self.ENG:
            sems[e] = stack.enter_context(nc.semaphore('sem_' + e))
        for s in self.dcnt:
            sems[s] = stack.enter_context(nc.semaphore('sem_' + s))
        block = stack.enter_context(nc.Block())
        total = dict((e, (self.cnt[e], e)) for e in self.ENG if self.cnt[e])
        for s, v in self.dcnt.items():
            total[s] = (v, 'dma')

        def run(ename):
            def body(eng):
                seen = {}
                for deps, fn, sname, inc in self.ops[ename]:
                    for s, (v, de) in deps.items():
                        if de == ename and ename == 'pe':
                            continue
                        if seen.get(s, 0) < v:
                            eng.wait_ge(sems[s], v)
                            seen[s] = v
                    ins = fn(eng)
                    ins.then_inc(sems[sname], inc)
                if ename == 'sp':
                    for s, (v, de) in total.items():
                        if seen.get(s, 0) < v:
                            eng.wait_ge(sems[s], v)
            return body
        block.tensor(run('pe'))
        block.vector(run('dve'))
        block.scalar(run('act'))
        block.gpsimd(run('pool'))
        block.sync(run('sp'))


def _ident():
    return np.eye(128, dtype=np.float32)


def build(stage='full'):
    nc = bass.Bass("TRN2", target_bir_lowering=False)
    P = Prog(nc)
    D = {}

    def din(name, shape, dt=F32):
        D[name] = nc.dram_tensor(name, list(shape), dt, kind="ExternalInput").ap()
        return D[name]

    x = din('x', [NT, DM])
    w_a = din('w_a', [128, KC, 1282])
    g1 = din('g1', [128, KC])
    bg = din('bg', [128, 2])
    convw = din('convw', [128, 4, 4])
    mg = din('mg', [128, 256])
    c_ident = din('c_ident', [128, 128])
    c_triu = din('c_triu', [128, 128])
    c_ones = din('c_ones', [128, 128])

    s5c = din('s5c', [128, 3, 8])
    s5r = din('s5r', [128, 3, 1024])
    s5b = din('s5b', [128, 2, 1024])
    s5cm = din('s5cm', [128, 2, 1024])
    s5d = din('s5d', [128, 2])
    s5gw = din('s5gw', [128, 4, 128])
    x_own = din('x_own', [1024, DM])
    w_o = din('w_o', [128, KC, DM])
    w_q = din('w_q', [128, KC, DM])
    g2c = din('g2c', [128, KC])
    g2r = din('g2r', [128, DM])
    gfr = din('gfr', [128, DM])
    skT = din('skT', [128, 2, 128])
    pu = din('pu', [16384, DM])
    pv_ = din('pv', [16384, DM])
    selin = din('sel', [128, 4])
    if stage == 'simB':
        mx_test = din('mx_test', [2048, NT], BF16)
    if stage in ('full', 'simB'):
        yout = nc.dram_tensor('y', [1024, DM], F32, kind="ExternalOutput").ap()
    mxi = [nc.dram_tensor(f'mxi{q}', [512, 1024], BF16) for q in range(4)]
    mxa = [nc.dram_tensor(f'mxa{q}', [2048, 1024], BF16) for q in range(4)]

    if stage in ('A1', 'A2', 'A3'):
        dbg = nc.dram_tensor('dbg', [512, NT], BF16, kind="ExternalOutput").ap()

    top = ExitStack()
    with top:
        def sb(stack, name, shape, dt=F32):
            return stack.enter_context(nc.sbuf_tensor(name, list(shape), dt))

        def ps(stack, name, shape, dt=F32):
            return stack.enter_context(nc.psum_tensor(name, list(shape), dt))

        ident_f = sb(top, 'ident_f', [128, 128])
        ident_b = sb(top, 'ident_b', [128, 128], BF16)
        triu = sb(top, 'triu', [128, 128])
        ones = sb(top, 'ones', [128, 128])
        P.dma('sp', lambda e: e.dma_start(out=ident_f[:, :], in_=c_ident[:, :]), 'c', writes=['ident_f'])
        P.dma('sp', lambda e: e.dma_start(out=triu[:, :], in_=c_triu[:, :]), 'c', writes=['triu'])
        P.dma('sp', lambda e: e.dma_start(out=ones[:, :], in_=c_ones[:, :]), 'c', writes=['ones'])
        P.op('dve', lambda e: e.tensor_copy(out=ident_b[:, :], in_=ident_f[:, :]), reads=['ident_f'], writes=['ident_b'])

        sA = ExitStack()
        uT = sb(sA, 'uT', [128, 2, NT], BF16)
        sA2 = ExitStack()
        qT = sb(sA2, 'qT', [128, 2, NT], BF16)
        kT = sb(sA2, 'kT', [128, 2, NT], BF16)
        v_aug = sb(sA2, 'v_aug', [128, 32, 257], BF16)
        gso = sb(sA2, 'gso', [128, 32, 256], BF16)
        g_tm = sb(sA2, 'g_tm', [128, 32, 2])
        bgt = sb(sA2, 'bgt', [128, 2])
        P.op('pool', lambda e: e.memset(v_aug[:, :, 256:257], 1.0), writes=['v_aug'])

        s1 = ExitStack()
        W = sb(s1, 'W', [128, KC, 1282], BF16)
        wst = [sb(s1, f'wst{i}', [128, 1282]) for i in range(2)]
        g1t = sb(s1, 'g1t', [128, KC])
        cw = sb(s1, 'cw', [128, 4, 4])
        mgt = sb(s1, 'mgt', [128, 256])
        xt = [sb(s1, f'xt{i}', [128, DM]) for i in range(2)]
        xs = [sb(s1, f'xs{i}', [128, DM], BF16) for i in range(2)]
        junk = sb(s1, 'junk', [128, DM], BF16)
        ssq = [sb(s1, f'ssq{i}', [128, 1]) for i in range(2)]
        rstd = [sb(s1, f'rstd{i}', [128, 1]) for i in range(2)]
        xnT = sb(s1, 'xnT', [128, KC, 512], BF16)
        pre = sb(s1, 'pre', [128, 4, 515])
        cacc = [sb(s1, f'cacc{i}', [128, 512]) for i in range(2)]
        sgo = [sb(s1, f'sgo{i}', [128, 256]) for i in range(2)]
        ptr = [ps(s1, f'ptr{i}', [128, 8, 128], BF16) for i in range(2)]
        pf = [ps(s1, f'pf{i}', [128, 512]) for i in range(2)]
        pv = [ps(s1, f'pv{i}', [128, 258]) for i in range(2)]
        po = [ps(s1, f'po{i}', [128, 256]) for i in range(2)]

        P.dma('sp', lambda e: e.dma_start(out=g1t[:, :], in_=g1[:, :]), 'c', writes=['g1t'])
        P.dma('sp', lambda e: e.dma_start(out=bgt[:, :], in_=bg[:, :]), 'c', writes=['bgt'])
        P.dma('sp', lambda e: e.dma_start(out=cw[:, :, :], in_=convw[:, :, :]), 'c', writes=['cw'])
        P.dma('sp', lambda e: e.dma_start(out=mgt[:, :], in_=mg[:, :]), 'c', writes=['mgt'])
        for kc in range(KC):
            b = kc % 2
            P.dma('sp', lambda e, kc=kc, b=b: e.dma_start(out=wst[b][:, :], in_=w_a[:, kc, :]), f'w{b}', writes=[f'wst{b}'])
            P.op('dve' if kc % 2 == 0 else 'pool',
                 lambda e, kc=kc, b=b: e.tensor_scalar(out=W[:, kc, :], in0=wst[b][:, :], scalar1=g1t[:, kc:kc + 1], scalar2=None, op0=ALU.mult),
                 reads=[f'wst{b}', 'g1t'], writes=[('W', kc)])
        Wkeys = [('W', kc) for kc in range(KC)]
        for m in range(4):
            P.op('pool', lambda e, m=m: e.memset(pre[:, m, 0:3], 0.0), writes=[('pre', m)])

        print('ops before token loop', getattr(P, 'total', 0))
        for tb in range(8):
            print('tb', tb, getattr(P, 'total', 0))
            for t4 in range(4):
                c = tb * 4 + t4
                b = c % 2
                P.dma('sp', lambda e, c=c, b=b: e.dma_start(out=xt[b][:, 0:1024], in_=x[c * 128:(c + 1) * 128, 0:1024]), f'x{b}', writes=[f'xt{b}'])
                P.dma('pool', lambda e, c=c, b=b: e.dma_start(out=xt[b][:, 1024:2048], in_=x[c * 128:(c + 1) * 128, 1024:2048]), f'xq{b}', writes=[f'xtq{b}'])
                P.op('act', lambda e, b=b: e.activation(out=junk[:, :], in_=xt[b][:, :], func=AF.Square, accum_out=ssq[b][:, :]),
                     reads=[f'xt{b}', f'xtq{b}'], writes=['junk', f'ssq{b}'])
                P.op('dve', lambda e, b=b: e.tensor_scalar(out=rstd[b][:, :], in0=ssq[b][:, :], scalar1=1.0 / DM, scalar2=EPS, op0=ALU.mult, op1=ALU.add),
                     reads=[f'ssq{b}'], writes=[f'rstd{b}'])
                P.op('act', lambda e, b=b: e.activation(out=rstd[b][:, :], in_=rstd[b][:, :], func=AF.Sqrt),
                     reads=[f'rstd{b}'], writes=[f'rstd{b}'])
                P.op('dve', lambda e, b=b: e.reciprocal(out=rstd[b][:, :], in_=rstd[b][:, :]),
                     reads=[f'rstd{b}'], writes=[f'rstd{b}'])
                P.op('dve', lambda e, b=b: e.tensor_scalar(out=xs[b][:, :], in0=xt[b][:, :], scalar1=rstd[b][:, 0:1], scalar2=None, op0=ALU.mult),
                     reads=[f'xt{b}', f'xtq{b}', f'rstd{b}'], writes=[f'xs{b}'])
                for half in range(2):
                    for j in range(8):
                        kc = half * 8 + j
                        P.op('pe', lambda e, b=b, kc=kc, half=half, j=j: e.transpose(out=ptr[half][:, j, :], in_=xs[b][:, kc * 128:(kc + 1) * 128], identity=ident_b[:, :]),
                             reads=[f'xs{b}', 'ident_b'], writes=[f'ptr{half}'])
                    P.op('act' if half == 0 else 'dve',
                         (lambda e, half=half, t4=t4: e.activation(out=xnT[:, half * 8:half * 8 + 8, t4 * 128:(t4 + 1) * 128], in_=ptr[half][:, :, :], func=AF.Copy)) if half == 0 else
                         (lambda e, half=half, t4=t4: e.tensor_copy(out=xnT[:, half * 8:half * 8 + 8, t4 * 128:(t4 + 1) * 128], in_=ptr[half][:, :, :])),
                         reads=[f'ptr{half}'], writes=[('xnT', t4)])
            xk = [('xnT', t) for t in range(4)]
            for m in range(6):
                pb = m % 2
                for kc in range(KC):
                    P.op('pe', lambda e, m=m, kc=kc, pb=pb: e.matmul(pf[pb][:, :], lhsT=W[:, kc, m * 128:(m + 1) * 128], rhs=xnT[:, kc, :], start=(kc == 0), stop=(kc == KC - 1)),
                         reads=xk + Wkeys, writes=[f'pf{pb}'])
                if m < 4:
                    P.op('act', lambda e, m=m, pb=pb: e.activation(out=pre[:, m, 3:515], in_=pf[pb][:, :], func=AF.Copy),
                         reads=[f'pf{pb}'], writes=[('pre', m)])
                    cb = m % 2
                    P.op('dve', lambda e, m=m, cb=cb: e.tensor_scalar(out=cacc[cb][:, :], in0=pre[:, m, 0:512], scalar1=cw[:, m, 0:1], scalar2=None, op0=ALU.mult),
                         reads=[('pre', m), 'cw'], writes=[f'cacc{cb}'])
                    for j in range(1, 4):
                        P.op('dve', lambda e, m=m, cb=cb, j=j: e.scalar_tensor_tensor(out=cacc[cb][:, :], in0=pre[:, m, j:j + 512], scalar=cw[:, m, j:j + 1], in1=cacc[cb][:, :], op0=ALU.mult, op1=ALU.add),
                             reads=[('pre', m), 'cw', f'cacc{cb}'], writes=[f'cacc{cb}'])
                    dst = qT if m < 2 else kT
                    P.op('act', lambda e, m=m, cb=cb, dst=dst, tb=tb: e.activation(out=dst[:, m % 2, tb * 512:(tb + 1) * 512], in_=cacc[cb][:, :], func=AF.Silu),
                         reads=[f'cacc{cb}'], writes=[('qk', m, tb)])
                    P.op('pool', lambda e, m=m: e.tensor_copy(out=pre[:, m, 0:3], in_=pre[:, m, 512:515]),
                         reads=[('pre', m)], writes=[('pre', m)])
                else:
                    P.op('dve', lambda e, m=m, pb=pb, tb=tb: e.tensor_copy(out=uT[:, m - 4, tb * 512:(tb + 1) * 512], in_=pf[pb][:, :]),
                         reads=[f'pf{pb}'], writes=[('uT', m - 4, tb)])
            for t4 in range(4):
                c = tb * 4 + t4
                pb = c % 2
                for kc in range(KC):
                    P.op('pe', lambda e, kc=kc, pb=pb, t4=t4: e.matmul(pv[pb][:, :], lhsT=xnT[:, kc, t4 * 128:(t4 + 1) * 128], rhs=W[:, kc, 768:1026], start=(kc == 0), stop=(kc == KC - 1)),
                         reads=xk + Wkeys, writes=[f'pv{pb}'])
                for kc in range(KC):
                    P.op('pe', lambda e, kc=kc, pb=pb, t4=t4: e.matmul(po[pb][:, :], lhsT=xnT[:, kc, t4 * 128:(t4 + 1) * 128], rhs=W[:, kc, 1026:1282], start=(kc == 0), stop=(kc == KC - 1)),
                         reads=xk + Wkeys, writes=[f'po{pb}'])
                P.op('dve', lambda e, c=c, pb=pb: e.tensor_copy(out=v_aug[:, c, 0:256], in_=pv[pb][:, 0:256]),
                     reads=[f'pv{pb}'], writes=[('v', c)])
                P.op('dve', lambda e, c=c, pb=pb: e.tensor_copy(out=g_tm[:, c, :], in_=pv[pb][:, 256:258]),
                     reads=[f'pv{pb}'], writes=[('g_tm', c)])
                P.op('act', lambda e, pb=pb: e.activation(out=sgo[pb][:, :], in_=po[pb][:, :], func=AF.Sigmoid),
                     reads=[f'po{pb}'], writes=[f'sgo{pb}'])
                P.op('pool', lambda e, c=c, pb=pb: e.tensor_tensor(out=gso[:, c, :], in0=sgo[pb][:, :], in1=mgt[:, :], op=ALU.mult),
                     reads=[f'sgo{pb}', 'mgt'], writes=[('gso', c)])
        P.barrier()
        s1.close()
        if stage == 'A1':
            P.dma('sp', lambda e: e.dma_start(out=dbg[0:128, :], in_=qT[:, 0, :]), 'out', writes=['dbg'], force=True)
            P.dma('sp', lambda e: e.dma_start(out=dbg[128:256, :], in_=kT[:, 1, :]), 'out', writes=['dbg'], force=True)
            P.dma('sp', lambda e: e.dma_start(out=dbg[256:384, :], in_=uT[:, 0, :]), 'out', writes=['dbg'], force=True)
            sA2.close()
            sA.close()
            P.emit(top)
            return nc

        print('ops before A2', getattr(P, 'total', 0))
        s2 = ExitStack()
        li = sb(s2, 'li', [128, 32])
        nlf = sb(s2, 'nlf', [128, 32])
        nega = sb(s2, 'nega', [128, 32])
        tmp = sb(s2, 'tmp', [128, 32])
        tmp2 = sb(s2, 'tmp2', [128, 32])
        es = sb(s2, 'es', [128, 32])
        wk = sb(s2, 'wk', [128, 32])
        eA = sb(s2, 'eA', [128, 32])
        nlfrep = sb(s2, 'nlfrep', [128, 32, 128])
        ea_bc = sb(s2, 'ea_bc', [128, NT])
        lnc = sb(s2, 'lnc', [128, 1])
        Cst = sb(s2, 'Cst', [128, 2, 257])
        Cb = sb(s2, 'Cb', [128, 2, 257], BF16)
        Pm = [sb(s2, f'Pm{i}', [128, 128], BF16) for i in range(2)]
        kw = [sb(s2, f'kw{i}', [128, 256], BF16) for i in range(2)]
        hmn = [sb(s2, f'hmn{i}', [128, 256], BF16) for i in range(2)]
        hT = [sb(s2, f'hT{i}', [128, 2, 128], BF16) for i in range(2)]
        sm = [sb(s2, f'sm{i}', [128, 8]) for i in range(2)]
        junk2 = sb(s2, 'junk2', [128, 256], BF16)
        pa = ps(s2, 'pa', [128, 64])
        pbc = ps(s2, 'pbc', [128, 512])
        ps1 = ps(s2, 'ps1', [128, 128])
        ptk = ps(s2, 'ptk', [128, 2, 128], BF16)
        psO = ps(s2, 'psO', [128, 257])
        pth = ps(s2, 'pth', [128, 2, 128], BF16)
        psC = [ps(s2, f'psC{i}', [128, 257]) for i in range(2)]

        gk = [('g_tm', c) for c in range(32)]
        P.op('dve', lambda e: e.tensor_scalar(out=li[:, :], in0=g_tm[:, :, 0], scalar1=bgt[:, 0:1], scalar2=None, op0=ALU.add), reads=gk + ['bgt'], writes=['li'])
        P.op('dve', lambda e: e.tensor_scalar(out=tmp[:, :], in0=g_tm[:, :, 1], scalar1=bgt[:, 1:2], scalar2=None, op0=ALU.add), reads=gk + ['bgt'], writes=['tmp'])
        P.op('act', lambda e: e.activation(out=tmp2[:, :], in_=tmp[:, :], func=AF.Exp, scale=-1.0), reads=['tmp'], writes=['tmp2'])
        P.op('dve', lambda e: e.tensor_scalar(out=tmp2[:, :], in0=tmp2[:, :], scalar1=1.0, scalar2=None, op0=ALU.add), reads=['tmp2'], writes=['tmp2'])
        P.op('act', lambda e: e.activation(out=nlf[:, :], in_=tmp2[:, :], func=AF.Ln), reads=['tmp2'], writes=['nlf'])
        P.op('dve', lambda e: e.memset(lnc[:, :], float(np.log(1.0 / 16.0))), writes=['lnc'])
        P.op('pe', lambda e: e.matmul(pa[:, 0:32], lhsT=triu[:, :], rhs=nlf[:, :], start=True, stop=True), reads=['triu', 'nlf'], writes=['pa'])
        P.op('pe', lambda e: e.matmul(pa[:, 32:64], lhsT=ones[:, :], rhs=nlf[:, :], start=True, stop=True), reads=['ones', 'nlf'], writes=['pa'])
        P.op('dve', lambda e: e.tensor_copy(out=nega[:, :], in_=pa[:, 0:32]), reads=['pa'], writes=['nega'])
        P.op('dve', lambda e: e.tensor_tensor(out=tmp[:, :], in0=li[:, :], in1=nega[:, :], op=ALU.add), reads=['li', 'nega'], writes=['tmp'])
        P.op('act', lambda e: e.activation(out=es[:, :], in_=tmp[:, :], func=AF.Exp), reads=['tmp'], writes=['es'])
        P.op('dve', lambda e: e.tensor_tensor(out=tmp2[:, :], in0=tmp[:, :], in1=pa[:, 32:64], op=ALU.subtract), reads=['tmp', 'pa'], writes=['tmp2'])
        P.op('act', lambda e: e.activation(out=wk[:, :], in_=tmp2[:, :], func=AF.Exp), reads=['tmp2'], writes=['wk'])
        P.op('act', lambda e: e.activation(out=eA[:, :], in_=pa[:, 32:64], func=AF.Exp, scale=-1.0), reads=['pa'], writes=['eA'])
        P.op('dve', lambda e: e.tensor_copy(out=nlfrep[:, :, :], in_=nlf[:, :].to_broadcast([128, 32, 128]) if False else nlf[:, :, None].to_broadcast([128, 32, 128])),
             reads=['nlf'], writes=['nlfrep'])
        for k8 in range(8):
            for j in range(4):
                c = k8 * 4 + j
                P.op('pe', lambda e, c=c, j=j: e.matmul(pbc[:, j * 128:(j + 1) * 128], lhsT=nlfrep[:, c, :], rhs=triu[:, :], start=True, stop=True),
                     reads=['nlfrep', 'triu'], writes=['pbc'])
            P.op('act', lambda e, k8=k8: e.activation(out=ea_bc[:, k8 * 512:(k8 + 1) * 512], in_=pbc[:, :], func=AF.Exp, scale=-1.0, bias=lnc[:, 0:1]),
                 reads=['pbc', 'lnc'], writes=[('ea_bc', k8)])
            for dc in range(2):
                P.op('dve' if dc == 0 else 'pool', lambda e, k8=k8, dc=dc: e.tensor_tensor(out=qT[:, dc, k8 * 512:(k8 + 1) * 512], in0=qT[:, dc, k8 * 512:(k8 + 1) * 512], in1=ea_bc[:, k8 * 512:(k8 + 1) * 512], op=ALU.mult),
                     reads=[('ea_bc', k8), ('qk', dc, k8)], writes=[('qk', dc, k8)])
        P.op('dve', lambda e: e.memset(Cst[:, :, :], 0.0), writes=['Cst'])
        P.op('pool', lambda e: e.memset(Cb[:, :, :], 0.0), writes=['Cb'])

        for c in range(32):
            b = c % 2
            tb = c // 4
            cs = slice(c * 128, (c + 1) * 128)
            qkk = [('qk', m, tb) for m in range(4)]
            for dc in range(2):
                P.op('pe', lambda e, dc=dc, cs=cs: e.matmul(ps1[:, :], lhsT=kT[:, dc, cs], rhs=qT[:, dc, cs], start=(dc == 0), stop=(dc == 1)),
                     reads=qkk, writes=['ps1'])
            P.op('dve', lambda e, b=b, c=c: e.scalar_tensor_tensor(out=Pm[b][:, :], in0=ps1[:, :], scalar=es[:, c:c + 1], in1=triu[:, :], op0=ALU.mult, op1=ALU.mult),
                 reads=['ps1', 'es', 'triu'], writes=[f'Pm{b}'])
            for dc in range(2):
                P.op('pe', lambda e, dc=dc, cs=cs: e.transpose(out=ptk[:, dc, :], in_=kT[:, dc, cs], identity=ident_b[:, :]),
                     reads=qkk + ['ident_b'], writes=['ptk'])
            P.op('act', lambda e, b=b, c=c: e.activation(out=kw[b][:, :], in_=ptk[:, :, :], func=AF.Copy, scale=wk[:, c:c + 1]),
                 reads=['ptk', 'wk'], writes=[f'kw{b}'])
            P.op('pe', lambda e, b=b, c=c: e.matmul(psO[:, :], lhsT=Pm[b][:, :], rhs=v_aug[:, c, :], start=True, stop=False),
                 reads=[f'Pm{b}', ('v', c), 'v_aug'], writes=['psO'])
            for dc in range(2):
                P.op('pe', lambda e, dc=dc, cs=cs: e.matmul(psO[:, :], lhsT=qT[:, dc, cs], rhs=Cb[:, dc, :], start=False, stop=(dc == 1)),
                     reads=qkk + ['Cb'], writes=['psO'])
            P.op('act', lambda e, b=b: e.activation(out=sm[b][:, 0:1], in_=psO[:, 256:257], func=AF.Abs), reads=['psO'], writes=[f'sm{b}'])
            P.op('dve', lambda e, b=b: e.tensor_scalar(out=sm[b][:, 0:1], in0=sm[b][:, 0:1], scalar1=1.0, scalar2=None, op0=ALU.max), reads=[f'sm{b}'], writes=[f'sm{b}'])
            P.op('dve', lambda e, b=b: e.reciprocal(out=sm[b][:, 1:2], in_=sm[b][:, 0:1]), reads=[f'sm{b}'], writes=[f'sm{b}'])
            P.op('act', lambda e, b=b: e.activation(out=junk2[:, :], in_=psO[:, 0:256], func=AF.Square, accum_out=sm[b][:, 2:3]), reads=['psO', f'sm{b}'], writes=['junk2', f'sm{b}'])
            P.op('dve', lambda e, b=b: e.tensor_scalar(out=sm[b][:, 3:4], in0=sm[b][:, 2:3], scalar1=sm[b][:, 1:2], scalar2=sm[b][:, 1:2], op0=ALU.mult, op1=ALU.mult), reads=[f'sm{b}'], writes=[f'sm{b}'])
            P.op('dve', lambda e, b=b: e.tensor_scalar(out=sm[b][:, 4:5], in0=sm[b][:, 3:4], scalar1=1.0 / 256.0, scalar2=EPS, op0=ALU.mult, op1=ALU.add), reads=[f'sm{b}'], writes=[f'sm{b}'])
            P.op('act', lambda e, b=b: e.activation(out=sm[b][:, 6:7], in_=sm[b][:, 4:5], func=AF.Sqrt), reads=[f'sm{b}'], writes=[f'sm{b}'])
            P.op('dve', lambda e, b=b: e.reciprocal(out=sm[b][:, 7:8], in_=sm[b][:, 6:7]), reads=[f'sm{b}'], writes=[f'sm{b}'])
            P.op('dve', lambda e, b=b: e.tensor_tensor(out=sm[b][:, 5:6], in0=sm[b][:, 7:8], in1=sm[b][:, 1:2], op=ALU.mult), reads=[f'sm{b}'], writes=[f'sm{b}'])
            P.op('dve', lambda e, b=b, c=c: e.scalar_tensor_tensor(out=hmn[b][:, :], in0=psO[:, 0:256], scalar=sm[b][:, 5:6], in1=gso[:, c, :], op0=ALU.mult, op1=ALU.mult),
                 reads=['psO', f'sm{b}', ('gso', c)], writes=[f'hmn{b}'])
            for ec in range(2):
                P.op('pe', lambda e, b=b, ec=ec: e.transpose(out=pth[:, ec, :], in_=hmn[b][:, ec * 128:(ec + 1) * 128], identity=ident_b[:, :]),
                     reads=[f'hmn{b}', 'ident_b'], writes=['pth'])
            P.op('act', lambda e, b=b: e.activation(out=hT[b][:, :, :], in_=pth[:, :, :], func=AF.Copy), reads=['pth'], writes=[f'hT{b}'])
            P.dma('sp', lambda e, b=b, c=c: e.dma_start(out=mxi[c // 8][0:256, (c % 8) * 128:(c % 8 + 1) * 128].rearrange("(ec p) j -> p ec j", p=128), in_=hT[b][:, :, :]), f'h{b}',
                  reads=[f'hT{b}'], writes=[('mx_in', 'h', c)])
            for dc in range(2):
                P.op('pe', lambda e, b=b, c=c, dc=dc: e.matmul(psC[dc][:, :], lhsT=kw[b][:, dc * 128:(dc + 1) * 128], rhs=v_aug[:, c, :], start=True, stop=True),
                     reads=[f'kw{b}', ('v', c), 'v_aug'], writes=[f'psC{dc}'])
                P.op('dve', lambda e, c=c, dc=dc: e.scalar_tensor_tensor(out=Cst[:, dc, :], in0=Cst[:, dc, :], scalar=eA[:, c:c + 1], in1=psC[dc][:, :], op0=ALU.mult, op1=ALU.add),
                     reads=['Cst', 'eA', f'psC{dc}'], writes=['Cst'])
            P.op('act', lambda e: e.activation(out=Cb[:, :, :], in_=Cst[:, :, :], func=AF.Copy), reads=['Cst'], writes=['Cb'])
        P.barrier()
        s2.close()
        sA2.close()

        if stage == 'A2':
            hk = [('mx_in', 'h', c) for c in range(32)]
            for q_ in range(4):
                P.dma('sp', lambda e, q_=q_: e.dma_start(out=dbg[0:256, q_ * 1024:(q_ + 1) * 1024], in_=mxi[q_][0:256, :]), 'out', reads=hk, writes=['dbg'])
            sA2.close()
            sA.close()
            P.emit(top)
            return nc


        print('ops before A3', getattr(P, 'total', 0))
        s3 = ExitStack()
        colp = sb(s3, 'colp', [128, 3, 8])
        rowp = sb(s3, 'rowp', [128, 3, 1024])
        bl = sb(s3, 'bl', [128, 2, 1024])
        cml = sb(s3, 'cml', [128, 2, 1024])
        dcol = sb(s3, 'dcol', [128, 2])
        gwf = sb(s3, 'gwf', [128, 4, 128])
        gwb = sb(s3, 'gwb', [128, 4, 128], BF16)
        hpi = sb(s3, 'hpi', [128, 1])
        Bre = sb(s3, 'Bre', [128, 1024], BF16)
        Bim = sb(s3, 'Bim', [128, 1024], BF16)
        pwr = sb(s3, 'pwr', [128, 8, 12])
        pwi = sb(s3, 'pwi', [128, 8, 12])
        npwi = sb(s3, 'npwi', [128, 8, 12])
        yacc = sb(s3, 'yacc', [128, NT])
        gy = sb(s3, 'gy', [128, NT], BF16)
        sgb = [sb(s3, f'sgb{i}', [128, 512]) for i in range(2)]
        yso = [sb(s3, f'yso{i}', [128, 512], BF16) for i in range(2)]
        pB = [ps(s3, f'pB{i}', [128, 512]) for i in range(4)]
        pY = [ps(s3, f'pY{i}', [128, 512]) for i in range(2)]
        pGa = ps(s3, 'pGa', [128, 512])
        pGb = ps(s3, 'pGb', [128, 512])
        Ur = sb(s3, 'Ur', [128, 8, 64])
        Ui = sb(s3, 'Ui', [128, 8, 64])
        Vr = sb(s3, 'Vr', [128, 8, 64])
        Vi = sb(s3, 'Vi', [128, 8, 64])
        t1s = sb(s3, 't1s', [128, 8, 64])
        t2s = sb(s3, 't2s', [128, 8, 64])
        lamz = sb(s3, 'lamz', [128, 8, 4])
        cS = [sb(s3, f'cS{i}', [128, 2, 64]) for i in range(2)]
        cZ = sb(s3, 'cZ', [128, 2, 65])
        s3t = ExitStack()
        rt = [sb(s3t, f'rt{i}', [128, 1024]) for i in range(7)]
        ct = [sb(s3t, f'ct{i}', [128, 8]) for i in range(8)]

        P.dma('sp', lambda e: e.dma_start(out=colp[:, :, :], in_=s5c[:, :, :]), 'c', writes=['colp'])
        P.dma('sp', lambda e: e.dma_start(out=rowp[:, :, :], in_=s5r[:, :, :]), 'c', writes=['rowp'])
        P.dma('sp', lambda e: e.dma_start(out=bl[:, :, :], in_=s5b[:, :, :]), 'c', writes=['bl'])
        P.dma('sp', lambda e: e.dma_start(out=cml[:, :, :], in_=s5cm[:, :, :]), 'c', writes=['cml'])
        P.dma('sp', lambda e: e.dma_start(out=dcol[:, :], in_=s5d[:, :]), 'c', writes=['dcol'])
        P.dma('sp', lambda e: e.dma_start(out=gwf[:, :, :], in_=s5gw[:, :, :]), 'c', writes=['gwf'])
        P.op('dve', lambda e: e.tensor_copy(out=gwb[:, :, :], in_=gwf[:, :, :]), reads=['gwf'], writes=['gwb'])
        P.op('dve', lambda e: e.memset(hpi[:, :], float(np.pi / 2)), writes=['hpi'])

        def lam_bar(src, T, n, tag, srckey):
            dt_, lrd, th, sn, cs, t1, t2 = T[:7]
            k = [tag]
            P.op('act', lambda e: e.activation(out=dt_[:, 0:n], in_=src[:, 2, :], func=AF.Exp), reads=k + [srckey], writes=k)
            P.op('dve', lambda e: e.tensor_tensor(out=lrd[:, 0:n], in0=src[:, 0, :], in1=dt_[:, 0:n], op=ALU.mult), reads=k + [srckey], writes=k)
            P.op('dve', lambda e: e.tensor_tensor(out=th[:, 0:n], in0=src[:, 1, :], in1=dt_[:, 0:n], op=ALU.mult), reads=k + [srckey], writes=k)
            P.op('act', lambda e: e.activation(out=sn[:, 0:n], in_=th[:, 0:n], func=AF.Sin, scale=1.0 / 16.0), reads=k, writes=k)
            P.op('act', lambda e: e.activation(out=cs[:, 0:n], in_=th[:, 0:n], func=AF.Sin, scale=1.0 / 16.0, bias=hpi[:, 0:1]), reads=k + ['hpi'], writes=k)
            for _ in range(4):
                P.op('dve', lambda e: e.tensor_tensor(out=t1[:, 0:n], in0=cs[:, 0:n], in1=cs[:, 0:n], op=ALU.mult), reads=k, writes=k)
                P.op('dve', lambda e: e.tensor_tensor(out=t2[:, 0:n], in0=sn[:, 0:n], in1=sn[:, 0:n], op=ALU.mult), reads=k, writes=k)
                P.op('dve', lambda e: e.scalar_tensor_tensor(out=sn[:, 0:n], in0=sn[:, 0:n], scalar=2.0, in1=cs[:, 0:n], op0=ALU.mult, op1=ALU.mult), reads=k, writes=k)
                P.op('dve', lambda e: e.tensor_tensor(out=cs[:, 0:n], in0=t1[:, 0:n], in1=t2[:, 0:n], op=ALU.subtract), reads=k, writes=k)
            P.op('act', lambda e: e.activation(out=t1[:, 0:n], in_=lrd[:, 0:n], func=AF.Exp), reads=k, writes=k)
            P.op('dve', lambda e: e.tensor_tensor(out=cs[:, 0:n], in0=cs[:, 0:n], in1=t1[:, 0:n], op=ALU.mult), reads=k, writes=k)
            P.op('dve', lambda e: e.tensor_tensor(out=sn[:, 0:n], in0=sn[:, 0:n], in1=t1[:, 0:n], op=ALU.mult), reads=k, writes=k)
            return cs, sn

        car, cai = lam_bar(colp, ct, 8, 'c', 'colp')
        P.op('dve', lambda e: e.tensor_copy(out=pwr[:, :, 0], in_=car[:, 0:8]), reads=['c'], writes=['pw'])
        P.op('dve', lambda e: e.tensor_copy(out=pwi[:, :, 0], in_=cai[:, 0:8]), reads=['c'], writes=['pw'])
        for k_ in range(1, 12):
            P.op('dve', lambda e, k_=k_: e.tensor_tensor(out=ct[0][:, :], in0=pwr[:, :, k_ - 1], in1=pwr[:, :, k_ - 1], op=ALU.mult), reads=['pw', 'c'], writes=['c'])
            P.op('dve', lambda e, k_=k_: e.tensor_tensor(out=ct[1][:, :], in0=pwi[:, :, k_ - 1], in1=pwi[:, :, k_ - 1], op=ALU.mult), reads=['pw', 'c'], writes=['c'])
            P.op('dve', lambda e, k_=k_: e.tensor_tensor(out=pwr[:, :, k_], in0=ct[0][:, :], in1=ct[1][:, :], op=ALU.subtract), reads=['c', 'pw'], writes=['pw'])
            P.op('dve', lambda e, k_=k_: e.scalar_tensor_tensor(out=pwi[:, :, k_], in0=pwr[:, :, k_ - 1], scalar=2.0, in1=pwi[:, :, k_ - 1], op0=ALU.mult, op1=ALU.mult), reads=['pw'], writes=['pw'])
        P.op('dve', lambda e: e.tensor_scalar(out=npwi[:, :, :], in0=pwi[:, :, :], scalar1=-1.0, scalar2=None, op0=ALU.mult), reads=['pw'], writes=['npw'])

        rar, rai = lam_bar(rowp, rt, 1024, 'r', 'rowp')
        R = ['r']
        P.op('dve', lambda e: e.tensor_scalar(out=rar[:, :], in0=rar[:, :], scalar1=-1.0, scalar2=None, op0=ALU.add), reads=R, writes=R)
        P.op('dve', lambda e: e.tensor_tensor(out=rt[0][:, :], in0=rowp[:, 0, :], in1=rowp[:, 0, :], op=ALU.mult), reads=R + ['rowp'], writes=R)
        P.op('dve', lambda e: e.tensor_tensor(out=rt[1][:, :], in0=rowp[:, 1, :], in1=rowp[:, 1, :], op=ALU.mult), reads=R + ['rowp'], writes=R)
        P.op('dve', lambda e: e.tensor_tensor(out=rt[0][:, :], in0=rt[0][:, :], in1=rt[1][:, :], op=ALU.add), reads=R, writes=R)
        P.op('dve', lambda e: e.reciprocal(out=rt[0][:, :], in_=rt[0][:, :]), reads=R, writes=R)
        P.op('dve', lambda e: e.tensor_tensor(out=rt[1][:, :], in0=rar[:, :], in1=rowp[:, 0, :], op=ALU.mult), reads=R + ['rowp'], writes=R)
        P.op('dve', lambda e: e.tensor_tensor(out=rt[2][:, :], in0=rai[:, :], in1=rowp[:, 1, :], op=ALU.mult), reads=R + ['rowp'], writes=R)
        P.op('dve', lambda e: e.tensor_tensor(out=rt[1][:, :], in0=rt[1][:, :], in1=rt[2][:, :], op=ALU.add), reads=R, writes=R)
        P.op('dve', lambda e: e.tensor_tensor(out=rt[1][:, :], in0=rt[1][:, :], in1=rt[0][:, :], op=ALU.mult), reads=R, writes=R)
        P.op('dve', lambda e: e.tensor_tensor(out=rt[2][:, :], in0=rai[:, :], in1=rowp[:, 0, :], op=ALU.mult), reads=R + ['rowp'], writes=R)
        P.op('dve', lambda e: e.tensor_tensor(out=rt[5][:, :], in0=rar[:, :], in1=rowp[:, 1, :], op=ALU.mult), reads=R + ['rowp'], writes=R)
        P.op('dve', lambda e: e.tensor_tensor(out=rt[2][:, :], in0=rt[2][:, :], in1=rt[5][:, :], op=ALU.subtract), reads=R, writes=R)
        P.op('dve', lambda e: e.tensor_tensor(out=rt[2][:, :], in0=rt[2][:, :], in1=rt[0][:, :], op=ALU.mult), reads=R, writes=R)
        P.op('dve', lambda e: e.tensor_tensor(out=rt[5][:, :], in0=rt[1][:, :], in1=bl[:, 0, :], op=ALU.mult), reads=R + ['bl'], writes=R)
        P.op('dve', lambda e: e.tensor_tensor(out=rt[6][:, :], in0=rt[2][:, :], in1=bl[:, 1, :], op=ALU.mult), reads=R + ['bl'], writes=R)
        P.op('dve', lambda e: e.tensor_tensor(out=Bre[:, :], in0=rt[5][:, :], in1=rt[6][:, :], op=ALU.subtract), reads=R, writes=['Bre'])
        P.op('dve', lambda e: e.tensor_tensor(out=rt[5][:, :], in0=rt[1][:, :], in1=bl[:, 1, :], op=ALU.mult), reads=R + ['bl', 'Bre'], writes=R)
        P.op('dve', lambda e: e.tensor_tensor(out=rt[6][:, :], in0=rt[2][:, :], in1=bl[:, 0, :], op=ALU.mult), reads=R + ['bl'], writes=R)
        P.op('dve', lambda e: e.tensor_tensor(out=Bim[:, :], in0=rt[5][:, :], in1=rt[6][:, :], op=ALU.add), reads=R, writes=['Bim'])
        P.op('dve', lambda e: e.tensor_scalar(out=cml[:, 1, :], in0=cml[:, 1, :], scalar1=-1.0, scalar2=None, op0=ALU.mult), reads=['cml'], writes=['cml'])

        P.op('dve', lambda e: e.memset(Ur[:, :, 0:1], 1.0), writes=['U'])
        P.op('dve', lambda e: e.memset(Ui[:, :, 0:1], 0.0), reads=['U'], writes=['U'])
        for k_ in range(6):
            s_ = 1 << k_
            Lr = lambda s_=s_, k_=k_: pwr[:, :, k_:k_ + 1].to_broadcast([128, 8, s_])
            Li = lambda s_=s_, k_=k_: pwi[:, :, k_:k_ + 1].to_broadcast([128, 8, s_])
            P.op('dve', lambda e, s_=s_, Lr=Lr: e.tensor_tensor(out=t1s[:, :, 0:s_], in0=Ur[:, :, 0:s_], in1=Lr(), op=ALU.mult), reads=['U', 'pw'], writes=['t1s'])
            P.op('dve', lambda e, s_=s_, Li=Li: e.tensor_tensor(out=t2s[:, :, 0:s_], in0=Ui[:, :, 0:s_], in1=Li(), op=ALU.mult), reads=['U', 'pw'], writes=['t2s'])
            P.op('dve', lambda e, s_=s_: e.tensor_tensor(out=Ur[:, :, s_:2 * s_], in0=t1s[:, :, 0:s_], in1=t2s[:, :, 0:s_], op=ALU.subtract), reads=['t1s', 't2s', 'U'], writes=['U2'])
            P.op('dve', lambda e, s_=s_, Li=Li: e.tensor_tensor(out=t1s[:, :, 0:s_], in0=Ur[:, :, 0:s_], in1=Li(), op=ALU.mult), reads=['U', 'U2', 'pw'], writes=['t1s'])
            P.op('dve', lambda e, s_=s_, Lr=Lr: e.tensor_tensor(out=t2s[:, :, 0:s_], in0=Ui[:, :, 0:s_], in1=Lr(), op=ALU.mult), reads=['U', 'U2', 'pw'], writes=['t2s'])
            P.op('dve', lambda e, s_=s_: e.tensor_tensor(out=Ui[:, :, s_:2 * s_], in0=t1s[:, :, 0:s_], in1=t2s[:, :, 0:s_], op=ALU.add), reads=['t1s', 't2s', 'U2'], writes=['U'])
        P.op('dve', lambda e: e.tensor_tensor(out=t1s[:, :, :], in0=Ur[:, :, :], in1=Ur[:, :, :], op=ALU.mult), reads=['U'], writes=['t1s'])
        P.op('dve', lambda e: e.tensor_tensor(out=t2s[:, :, :], in0=Ui[:, :, :], in1=Ui[:, :, :], op=ALU.mult), reads=['U'], writes=['t2s'])
        P.op('dve', lambda e: e.tensor_tensor(out=t1s[:, :, :], in0=t1s[:, :, :], in1=t2s[:, :, :], op=ALU.add), reads=['t1s', 't2s'], writes=['t1s'])
        P.op('dve', lambda e: e.reciprocal(out=t1s[:, :, :], in_=t1s[:, :, :]), reads=['t1s'], writes=['t1s'])
        P.op('dve', lambda e: e.tensor_tensor(out=Vr[:, :, :], in0=Ur[:, :, :], in1=t1s[:, :, :], op=ALU.mult), reads=['U', 't1s'], writes=['V'])
        P.op('dve', lambda e: e.scalar_tensor_tensor(out=Vi[:, :, :], in0=Ui[:, :, :], scalar=-1.0, in1=t1s[:, :, :], op0=ALU.mult, op1=ALU.mult), reads=['U', 't1s', 'V'], writes=['V'])
        P.op('dve', lambda e: e.tensor_copy(out=lamz[:, :, 0:1], in_=Ur[:, :, 63:64]), reads=['U'], writes=['lamz'])
        P.op('dve', lambda e: e.tensor_copy(out=lamz[:, :, 1:2], in_=Ui[:, :, 63:64]), reads=['U', 'lamz'], writes=['lamz'])
        P.op('dve', lambda e: e.tensor_scalar(out=lamz[:, :, 2:3], in0=Ui[:, :, 63:64], scalar1=-1.0, scalar2=None, op0=ALU.mult), reads=['U', 'lamz'], writes=['lamz'])
        P.barrier()
        s3t.close()
        BT = [sb(s3, f'BT{i}', [128, NT]) for i in range(5)]
        msk = sb(s3, 'msk', [128, NT])
        P.op('pool', lambda e: e.memset(msk[:, :], 1.0), writes=['msk'])
        P.op('pool', lambda e: e.memset(msk[:, 0:NT:64], 0.0), reads=['msk'], writes=['msk'])
        P.op('dve', lambda e: e.memset(cZ[:, :, 0:1], 0.0), writes=['cZ0'])

        def v3(t):
            return t[:, :].rearrange("p (c t) -> p c t", t=64)
        for q in range(8):
            cc = q // 4
            ukeys = [('uT', cc, tb) for tb in range(8)]
            for nb in range(8):
                ns = slice(nb * 512, (nb + 1) * 512)
                pr, pi_ = pB[(nb % 2) * 2], pB[(nb % 2) * 2 + 1]
                P.op('pe', lambda e, q=q, cc=cc, ns=ns, pr=pr: e.matmul(pr[:, :], lhsT=Bre[:, q * 128:(q + 1) * 128], rhs=uT[:, cc, ns], start=True, stop=True),
                     reads=ukeys + ['Bre'], writes=[('pB', (nb % 2) * 2)])
                P.op('pe', lambda e, q=q, cc=cc, ns=ns, pi_=pi_: e.matmul(pi_[:, :], lhsT=Bim[:, q * 128:(q + 1) * 128], rhs=uT[:, cc, ns], start=True, stop=True),
                     reads=ukeys + ['Bim'], writes=[('pB', (nb % 2) * 2 + 1)])
                P.op('act', lambda e, ns=ns, pr=pr: e.activation(out=BT[0][:, ns], in_=pr[:, :], func=AF.Copy), reads=[('pB', (nb % 2) * 2)], writes=['BT0'])
                P.op('dve', lambda e, ns=ns, pi_=pi_: e.tensor_copy(out=BT[1][:, ns], in_=pi_[:, :]), reads=[('pB', (nb % 2) * 2 + 1)], writes=['BT1'])
            A_, B_, C_, D_, E_ = BT
            F_ = D_
            tb_ = lambda T, q=q: T[:, q, None, :].to_broadcast([128, 64, 64])
            P.op('pool', lambda e, tb_=tb_: e.tensor_tensor(out=v3(C_), in0=v3(A_), in1=tb_(Vr), op=ALU.mult), reads=['BT0', 'V'], writes=['BT2'])
            P.op('pool', lambda e, tb_=tb_: e.tensor_tensor(out=v3(D_), in0=v3(B_), in1=tb_(Vi), op=ALU.mult), reads=['BT1', 'V'], writes=['BT3'])
            P.op('dve', lambda e: e.tensor_tensor(out=C_[:, :], in0=C_[:, :], in1=D_[:, :], op=ALU.subtract), reads=['BT2', 'BT3'], writes=['BT2'])
            P.op('pool', lambda e, tb_=tb_: e.tensor_tensor(out=v3(D_), in0=v3(A_), in1=tb_(Vi), op=ALU.mult), reads=['BT0', 'V', 'BT2'], writes=['BT3'])
            P.op('pool', lambda e, tb_=tb_: e.tensor_tensor(out=v3(E_), in0=v3(B_), in1=tb_(Vr), op=ALU.mult), reads=['BT1', 'V'], writes=['BT4'])
            P.op('dve', lambda e: e.tensor_tensor(out=D_[:, :], in0=D_[:, :], in1=E_[:, :], op=ALU.add), reads=['BT3', 'BT4'], writes=['BT3'])
            P.op('dve', lambda e: e.tensor_tensor_scan(out=A_[:, :], data0=msk[:, :], data1=C_[:, :], initial=0.0, op0=ALU.mult, op1=ALU.add), reads=['msk', 'BT2'], writes=['BT0'])
            P.op('dve', lambda e: e.tensor_tensor_scan(out=B_[:, :], data0=msk[:, :], data1=D_[:, :], initial=0.0, op0=ALU.mult, op1=ALU.add), reads=['msk', 'BT3'], writes=['BT1'])
            P.op('dve', lambda e, q=q: e.tensor_scalar(out=cS[0][:, 0, :], in0=v3(A_)[:, :, 63], scalar1=lamz[:, q, 0:1], scalar2=None, op0=ALU.mult), reads=['BT0', 'lamz'], writes=['cS0'])
            P.op('dve', lambda e, q=q: e.scalar_tensor_tensor(out=cS[0][:, 0, :], in0=v3(B_)[:, :, 63], scalar=lamz[:, q, 2:3], in1=cS[0][:, 0, :], op0=ALU.mult, op1=ALU.add), reads=['BT1', 'lamz', 'cS0'], writes=['cS0'])
            P.op('dve', lambda e, q=q: e.tensor_scalar(out=cS[0][:, 1, :], in0=v3(B_)[:, :, 63], scalar1=lamz[:, q, 0:1], scalar2=None, op0=ALU.mult), reads=['BT1', 'lamz', 'cS0'], writes=['cS0'])
            P.op('dve', lambda e, q=q: e.scalar_tensor_tensor(out=cS[0][:, 1, :], in0=v3(A_)[:, :, 63], scalar=lamz[:, q, 1:2], in1=cS[0][:, 1, :], op0=ALU.mult, op1=ALU.add), reads=['BT0', 'lamz', 'cS0'], writes=['cS0'])
            cur = 0
            for j_ in range(6):
                sft = 1 << j_
                k_ = 6 + j_
                nxt = 1 - cur
                s_, d_ = cS[cur], cS[nxt]
                ks, kd = f'cS{cur}', f'cS{nxt}'
                P.op('dve', lambda e, s_=s_, d_=d_, sft=sft, q=q, k_=k_: e.scalar_tensor_tensor(out=d_[:, 0, sft:], in0=s_[:, 0, 0:64 - sft], scalar=pwr[:, q, k_:k_ + 1], in1=s_[:, 0, sft:], op0=ALU.mult, op1=ALU.add), reads=[ks, 'pw'], writes=[kd])
                P.op('dve', lambda e, s_=s_, d_=d_, sft=sft, q=q, k_=k_: e.scalar_tensor_tensor(out=d_[:, 0, sft:], in0=s_[:, 1, 0:64 - sft], scalar=npwi[:, q, k_:k_ + 1], in1=d_[:, 0, sft:], op0=ALU.mult, op1=ALU.add), reads=[ks, 'npw', kd], writes=[kd])
                P.op('dve', lambda e, s_=s_, d_=d_, sft=sft, q=q, k_=k_: e.scalar_tensor_tensor(out=d_[:, 1, sft:], in0=s_[:, 1, 0:64 - sft], scalar=pwr[:, q, k_:k_ + 1], in1=s_[:, 1, sft:], op0=ALU.mult, op1=ALU.add), reads=[ks, 'pw', kd], writes=[kd])
                P.op('dve', lambda e, s_=s_, d_=d_, sft=sft, q=q, k_=k_: e.scalar_tensor_tensor(out=d_[:, 1, sft:], in0=s_[:, 0, 0:64 - sft], scalar=pwi[:, q, k_:k_ + 1], in1=d_[:, 1, sft:], op0=ALU.mult, op1=ALU.add), reads=[ks, 'pw', kd], writes=[kd])
                P.op('dve', lambda e, s_=s_, d_=d_, sft=sft: e.tensor_copy(out=d_[:, :, 0:sft], in_=s_[:, :, 0:sft]), reads=[ks, kd], writes=[kd])
                cur = nxt
            Xc = cS[cur]
            kx = f'cS{cur}'
            P.op('dve', lambda e, q=q, Xc=Xc: e.tensor_scalar(out=cZ[:, 0, 1:65], in0=Xc[:, 0, :], scalar1=pwr[:, q, 0:1], scalar2=None, op0=ALU.mult), reads=[kx, 'pw', 'cZ0'], writes=['cZ'])
            P.op('dve', lambda e, q=q, Xc=Xc: e.scalar_tensor_tensor(out=cZ[:, 0, 1:65], in0=Xc[:, 1, :], scalar=npwi[:, q, 0:1], in1=cZ[:, 0, 1:65], op0=ALU.mult, op1=ALU.add), reads=[kx, 'npw', 'cZ'], writes=['cZ'])
            P.op('dve', lambda e, q=q, Xc=Xc: e.tensor_scalar(out=cZ[:, 1, 1:65], in0=Xc[:, 1, :], scalar1=pwr[:, q, 0:1], scalar2=None, op0=ALU.mult), reads=[kx, 'pw', 'cZ'], writes=['cZ'])
            P.op('dve', lambda e, q=q, Xc=Xc: e.scalar_tensor_tensor(out=cZ[:, 1, 1:65], in0=Xc[:, 0, :], scalar=pwi[:, q, 0:1], in1=cZ[:, 1, 1:65], op0=ALU.mult, op1=ALU.add), reads=[kx, 'pw', 'cZ'], writes=['cZ'])
            P.op('dve', lambda e, tb_=tb_: e.tensor_tensor(out=v3(A_), in0=v3(A_), in1=cZ[:, 0, 0:64, None].to_broadcast([128, 64, 64]), op=ALU.add), reads=['BT0', 'cZ'], writes=['BT0'])
            P.op('dve', lambda e, tb_=tb_: e.tensor_tensor(out=v3(B_), in0=v3(B_), in1=cZ[:, 1, 0:64, None].to_broadcast([128, 64, 64]), op=ALU.add), reads=['BT1', 'cZ'], writes=['BT1'])
            P.op('pool', lambda e, tb_=tb_: e.tensor_tensor(out=v3(C_), in0=v3(A_), in1=tb_(Ur), op=ALU.mult), reads=['BT0', 'U'], writes=['BT2'])
            P.op('pool', lambda e, tb_=tb_: e.tensor_tensor(out=v3(D_), in0=v3(B_), in1=tb_(Ui), op=ALU.mult), reads=['BT1', 'U'], writes=['BT3'])
            P.op('dve', lambda e: e.tensor_tensor(out=C_[:, :], in0=C_[:, :], in1=D_[:, :], op=ALU.subtract), reads=['BT2', 'BT3'], writes=['BT2'])
            P.op('dve', lambda e, tb_=tb_: e.tensor_tensor(out=v3(E_), in0=v3(A_), in1=tb_(Ui), op=ALU.mult), reads=['BT0', 'U'], writes=['BT4'])
            P.op('pool', lambda e, tb_=tb_: e.tensor_tensor(out=v3(F_), in0=v3(B_), in1=tb_(Ur), op=ALU.mult), reads=['BT1', 'U'], writes=['BT3'])
            P.op('dve', lambda e: e.tensor_tensor(out=E_[:, :], in0=E_[:, :], in1=F_[:, :], op=ALU.add), reads=['BT4', 'BT3'], writes=['BT4'])
            fr_, fi_ = C_, E_
            for nb in range(8):
                ns = slice(nb * 512, (nb + 1) * 512)
                py = pY[nb % 2]
                P.op('pe', lambda e, q=q, ns=ns, py=py, fr_=fr_: e.matmul(py[:, :], lhsT=cml[:, 0, q * 128:(q + 1) * 128], rhs=fr_[:, ns], start=True, stop=False),
                     reads=['cml', 'BT2'], writes=[('pY', nb % 2)])
                P.op('pe', lambda e, q=q, ns=ns, py=py, fi_=fi_: e.matmul(py[:, :], lhsT=cml[:, 1, q * 128:(q + 1) * 128], rhs=fi_[:, ns], start=False, stop=True),
                     reads=['cml', 'BT4'], writes=[('pY', nb % 2)])
                if q % 4 == 0:
                    P.op('dve', lambda e, cc=cc, ns=ns, py=py: e.scalar_tensor_tensor(out=yacc[:, ns], in0=uT[:, cc, ns], scalar=dcol[:, cc:cc + 1], in1=py[:, :], op0=ALU.mult, op1=ALU.add),
                         reads=ukeys + ['dcol', ('pY', nb % 2)], writes=[('yacc', nb)])
                else:
                    P.op('dve', lambda e, ns=ns, py=py: e.tensor_tensor(out=yacc[:, ns], in0=yacc[:, ns], in1=py[:, :], op=ALU.add),
                         reads=[('yacc', nb), ('pY', nb % 2)], writes=[('yacc', nb)])
            if q % 4 == 3:
                for nb in range(8):
                    ns = slice(nb * 512, (nb + 1) * 512)
                    b = nb % 2
                    P.op('act', lambda e, ns=ns: e.activation(out=gy[:, ns], in_=yacc[:, ns], func=AF.Gelu), reads=[('yacc', nb)], writes=[('gy', nb)])
                    P.op('pe', lambda e, cc=cc, ns=ns: e.matmul(pGa[:, :], lhsT=gwb[:, cc * 2, :], rhs=gy[:, ns], start=True, stop=True), reads=['gwb', ('gy', nb)], writes=['pGa'])
                    P.op('pe', lambda e, cc=cc, ns=ns: e.matmul(pGb[:, :], lhsT=gwb[:, cc * 2 + 1, :], rhs=gy[:, ns], start=True, stop=True), reads=['gwb', ('gy', nb)], writes=['pGb'])
                    P.op('act', lambda e, b=b: e.activation(out=sgb[b][:, :], in_=pGb[:, :], func=AF.Sigmoid), reads=['pGb'], writes=[f'sgb{b}'])
                    P.op('dve', lambda e, b=b: e.tensor_tensor(out=yso[b][:, :], in0=pGa[:, :], in1=sgb[b][:, :], op=ALU.mult), reads=['pGa', f'sgb{b}'], writes=[f'yso{b}'])
                    P.dma('sp', lambda e, cc=cc, nb=nb, b=b: e.dma_start(out=mxi[nb // 2][256 + cc * 128:256 + (cc + 1) * 128, (nb % 2) * 512:(nb % 2 + 1) * 512], in_=yso[b][:, :]), f'ys{b}',
                          reads=[f'yso{b}'], writes=[('mx_in', 'y', cc, nb)])
        P.barrier()
        s3.close()
        if stage == 'A3':
            for q_ in range(4):
                P.dma('sp', lambda e, q_=q_: e.dma_start(out=dbg[:, q_ * 1024:(q_ + 1) * 1024], in_=mxi[q_][:, :]), 'out', writes=['dbg'], force=True)
            sA2.close()
            sA.close()
            P.emit(top)
            return nc

        sA.close()
        print('ops before exchange', getattr(P, 'total', 0))
        NEI = int(os.environ.get('KNE', '128'))
        if stage == 'simB':
            mxsrc = [mx_test[:, q_ * 1024:(q_ + 1) * 1024] for q_ in range(4)]
        else:
            allk = [('mx_in', 'h', c) for c in range(32)] + [('mx_in', 'y', cc, nb) for cc in range(2) for nb in range(8)]
            for q_ in range(4):
                P.dma('pool', lambda e, q_=q_: e.collective_compute("AllGather", ALU.bypass, replica_groups=[[0, 1, 2, 3], [4, 5, 6, 7]],
                                                                   ins=[mxi[q_].ap().opt()], outs=[mxa[q_].ap().opt()]), f'cc{q_}', reads=allk, writes=[('mx_all', q_)], inc=1)
            mxsrc = [mxa[q_].ap() for q_ in range(4)]
        sB = ExitStack()
        h1 = sb(sB, 'h1', [128, 8, DM])
        xn2T = sb(sB, 'xn2T', [128, KC, 1024], BF16)
        em = sb(sB, 'em', [128, 8, 16, 128], BF16)
        sc = sb(sB, 'sc', [128, 8, 8, 2])
        selt = sb(sB, 'selt', [128, 4])
        g2t = sb(sB, 'g2t', [128, KC])
        ssq2 = [sb(sB, f'ssq2{i}', [128, 1]) for i in range(2)]
        rstd2 = [sb(sB, f'rstd2{i}', [128, 1]) for i in range(2)]
        P.dma('sp', lambda e: e.dma_start(out=selt[:, :], in_=selin[:, :]), 'c', writes=['selt'])
        P.dma('sp', lambda e: e.dma_start(out=g2t[:, :], in_=g2c[:, :]), 'c', writes=['g2t'])
        P.dma('sp', lambda e: e.dma_start(out=h1[:, :, :], in_=x_own.rearrange("(tt p) d -> p tt d", p=128)), 'c', writes=['h1'])

        print('ops before B1', getattr(P, 'total', 0))
        sb1 = ExitStack()
        mixo = sb(sb1, 'mixo', [128, KC, 1024], BF16)
        wb = sb(sb1, 'wb', [128, KC, 512], BF16)
        mst = [sb(sb1, f'mst{i}', [128, 1024], BF16) for i in range(2)]
        wst2 = [sb(sb1, f'wst2{i}', [128, 512]) for i in range(2)]
        xs2 = [sb(sb1, f'xs2{i}', [128, DM], BF16) for i in range(2)]
        pM = [ps(sb1, f'pM{i}', [128, 512]) for i in range(2)]
        ptr2 = [ps(sb1, f'ptr2{i}', [128, 8, 128], BF16) for i in range(2)]

        for kc in range(KC):
            for q_ in range(4):
                b = (kc * 4 + q_) % 2
                P.dma('sp', lambda e, kc=kc, q_=q_, b=b: e.dma_start(out=mst[b][:, :], in_=mxsrc[q_][kc * 128:(kc + 1) * 128, :]), f'ms{b}',
                      reads=[('mx_all', q_)], writes=[f'mst{b}'])
                if q_ == 0:
                    P.op('dve', lambda e, kc=kc, b=b: e.tensor_scalar(out=mixo[:, kc, :], in0=mst[b][:, :], scalar1=selt[:, 0:1], scalar2=None, op0=ALU.mult),
                         reads=[f'mst{b}', 'selt'], writes=['mixo'])
                else:
                    P.op('dve', lambda e, kc=kc, b=b, q_=q_: e.scalar_tensor_tensor(out=mixo[:, kc, :], in0=mst[b][:, :], scalar=selt[:, q_:q_ + 1], in1=mixo[:, kc, :], op0=ALU.mult, op1=ALU.add),
                         reads=[f'mst{b}', 'selt', 'mixo'], writes=['mixo'])
        for nblk in range(4):
            for kc in range(KC):
                b = kc % 2
                P.dma('sp', lambda e, kc=kc, b=b, nblk=nblk: e.dma_start(out=wst2[b][:, :], in_=w_o[:, kc, nblk * 512:(nblk + 1) * 512]), f'wo{b}', writes=[f'wst2{b}'])
                P.op('act', lambda e, kc=kc, b=b: e.activation(out=wb[:, kc, :], in_=wst2[b][:, :], func=AF.Copy), reads=[f'wst2{b}'], writes=['wb'])
            for tt in range(8):
                pb_ = tt % 2
                for kc in range(KC):
                    P.op('pe', lambda e, kc=kc, tt=tt, pb_=pb_: e.matmul(pM[pb_][:, :], lhsT=mixo[:, kc, tt * 128:(tt + 1) * 128], rhs=wb[:, kc, :], start=(kc == 0), stop=(kc == KC - 1)),
                         reads=['mixo', 'wb'], writes=[f'pM{pb_}'])
                P.op('dve', lambda e, tt=tt, pb_=pb_, nblk=nblk: e.tensor_tensor(out=h1[:, tt, nblk * 512:(nblk + 1) * 512], in0=h1[:, tt, nblk * 512:(nblk + 1) * 512], in1=pM[pb_][:, :], op=ALU.add),
                     reads=[f'pM{pb_}', 'h1'], writes=['h1'])

        def rms(tt, b, jt, jk):
            P.op('act', lambda e: e.activation(out=jt[:, :], in_=h1[:, tt, :], func=AF.Square, accum_out=ssq2[b][:, :]), reads=['h1'], writes=[jk, f'ssq2{b}'])
            P.op('dve', lambda e: e.tensor_scalar(out=rstd2[b][:, :], in0=ssq2[b][:, :], scalar1=1.0 / DM, scalar2=EPS, op0=ALU.mult, op1=ALU.add), reads=[f'ssq2{b}'], writes=[f'rstd2{b}'])
            P.op('act', lambda e: e.activation(out=rstd2[b][:, :], in_=rstd2[b][:, :], func=AF.Sqrt), reads=[f'rstd2{b}'], writes=[f'rstd2{b}'])
            P.op('dve', lambda e: e.reciprocal(out=rstd2[b][:, :], in_=rstd2[b][:, :]), reads=[f'rstd2{b}'], writes=[f'rstd2{b}'])
        for tt in range(8):
            b = tt % 2
            rms(tt, b, xs2[b], f'xs2{b}')
            P.op('dve', lambda e, tt=tt, b=b: e.tensor_scalar(out=xs2[b][:, :], in0=h1[:, tt, :], scalar1=rstd2[b][:, 0:1], scalar2=None, op0=ALU.mult), reads=['h1', f'rstd2{b}'], writes=[f'xs2{b}'])
            for half in range(2):
                for j in range(8):
                    kc = half * 8 + j
                    P.op('pe', lambda e, b=b, kc=kc, half=half, j=j: e.transpose(out=ptr2[half][:, j, :], in_=xs2[b][:, kc * 128:(kc + 1) * 128], identity=ident_b[:, :]),
                         reads=[f'xs2{b}', 'ident_b'], writes=[f'ptr2{half}'])
                P.op('dve', lambda e, half=half, tt=tt: e.tensor_tensor(out=xn2T[:, half * 8:half * 8 + 8, tt * 128:(tt + 1) * 128], in0=ptr2[half][:, :, :],
                                                                         in1=g2t[:, half * 8:half * 8 + 8, None].to_broadcast([128, 8, 128]), op=ALU.mult),
                     reads=[f'ptr2{half}', 'g2t'], writes=['xn2T'])
        P.barrier()
        sb1.close()

        print('ops before B2', getattr(P, 'total', 0))
        sb2 = ExitStack()
        v16 = sb(sb2, 'v16', [128, 8, 16, 16])
        skf = sb(sb2, 'skf', [128, 2, 128])
        skb = sb(sb2, 'skb', [128, 2, 128], BF16)
        wqst = [sb(sb2, f'wqst{i}', [128, KC, 128]) for i in range(2)]
        wqb = [sb(sb2, f'wqb{i}', [128, KC, 128], BF16) for i in range(2)]
        qj = [sb(sb2, f'qj{i}', [128, 1024], BF16) for i in range(2)]
        ebf = [sb(sb2, f'ebf{i}', [128, 128], BF16) for i in range(2)]
        ef = [sb(sb2, f'ef{i}', [128, 128]) for i in range(2)]
        ef2 = [sb(sb2, f'ef2{i}', [128, 128]) for i in range(2)]
        cand = [sb(sb2, f'cand{i}', [128, 256]) for i in range(2)]
        cand2 = [sb(sb2, f'cand2{i}', [128, 256]) for i in range(2)]
        c16 = [sb(sb2, f'c16{i}', [128, 16]) for i in range(2)]
        pQ = [ps(sb2, f'pQ{i}', [128, 512]) for i in range(2)]
        pS = [ps(sb2, f'pS{i}', [128, 128]) for i in range(2)]
        P.dma('sp', lambda e: e.dma_start(out=skf[:, :, :], in_=skT[:, :, :]), 'c', writes=['skf'])
        P.op('dve', lambda e: e.tensor_copy(out=skb[:, :, :], in_=skf[:, :, :]), reads=['skf'], writes=['skb'])
        for j in range(16):
            b = j % 2
            half = j % 2
            P.dma('sp', lambda e, j=j, b=b: e.dma_start(out=wqst[b][:, :, :], in_=w_q[:, :, j * 128:(j + 1) * 128]), f'wq{b}', writes=[f'wqst{b}'])
            P.op('dve', lambda e, b=b: e.tensor_copy(out=wqb[b][:, :, :], in_=wqst[b][:, :, :]),
                 reads=[f'wqst{b}'], writes=[f'wqb{b}'])
            for th in range(2):
                for kc in range(KC):
                    P.op('pe', lambda e, b=b, kc=kc, th=th: e.matmul(pQ[th][:, :], lhsT=wqb[b][:, kc, :], rhs=xn2T[:, kc, th * 512:(th + 1) * 512], start=(kc == 0), stop=(kc == KC - 1)),
                         reads=[f'wqb{b}', 'xn2T'], writes=[f'pQ{th}'])
                P.op('act', lambda e, b=b, th=th: e.activation(out=qj[b][:, th * 512:(th + 1) * 512], in_=pQ[th][:, :], func=AF.Copy), reads=[f'pQ{th}'], writes=[f'qj{b}'])
            for tt in range(8):
                b2 = tt % 2
                P.op('pe', lambda e, b=b, tt=tt, b2=b2, half=half: e.matmul(pS[b2][:, :], lhsT=qj[b][:, tt * 128:(tt + 1) * 128], rhs=skb[:, half, :], start=True, stop=True),
                     reads=[f'qj{b}', 'skb'], writes=[f'pS{b2}'])
                P.op('act', lambda e, b2=b2: e.activation(out=ebf[b2][:, :], in_=pS[b2][:, :], func=AF.Exp), reads=[f'pS{b2}'], writes=[f'ebf{b2}'])
                P.op('dve', lambda e, b2=b2: e.tensor_copy(out=ef[b2][:, :], in_=ebf[b2][:, :]), reads=[f'ebf{b2}'], writes=[f'ef{b2}'])
                P.op('dve', lambda e, b2=b2, tt=tt, j=j: e.max(out=v16[:, tt, j, 0:8], in_=ef[b2][:, :]), reads=[f'ef{b2}'], writes=['v16'])
                P.op('dve', lambda e, b2=b2, tt=tt, j=j: e.match_replace(out=ef2[b2][:, :], in_to_replace=v16[:, tt, j, 0:8], in_values=ef[b2][:, :], imm_value=-1.0), reads=[f'ef{b2}', 'v16'], writes=[f'ef2{b2}'])
                P.op('dve', lambda e, b2=b2, tt=tt, j=j: e.max(out=v16[:, tt, j, 8:16], in_=ef2[b2][:, :]), reads=[f'ef2{b2}'], writes=['v16'])
                P.op('dve', lambda e, b2=b2, tt=tt, j=j: e.scalar_tensor_tensor(out=em[:, tt, j, :], in0=ef[b2][:, :], scalar=v16[:, tt, j, 15:16], in1=ef[b2][:, :], op0=ALU.is_ge, op1=ALU.mult),
                     reads=[f'ef{b2}', 'v16'], writes=['em'])
        for tt in range(8):
            for hh in range(8):
                b = hh % 2
                P.op('dve', lambda e, tt=tt, hh=hh, b=b: e.tensor_tensor(out=cand[b][:, :].rearrange("p (a c) -> p a c", a=16), in0=v16[:, tt, 2 * hh, :, None].to_broadcast([128, 16, 16]),
                                                                          in1=v16[:, tt, 2 * hh + 1, None, :].to_broadcast([128, 16, 16]), op=ALU.mult), reads=['v16'], writes=[f'cand{b}'])
                P.op('dve', lambda e, b=b: e.max(out=c16[b][:, 0:8], in_=cand[b][:, :]), reads=[f'cand{b}'], writes=[f'c16{b}'])
                P.op('dve', lambda e, b=b: e.match_replace(out=cand2[b][:, :], in_to_replace=c16[b][:, 0:8], in_values=cand[b][:, :], imm_value=-1.0), reads=[f'cand{b}', f'c16{b}'], writes=[f'cand2{b}'])
                P.op('dve', lambda e, b=b: e.max(out=c16[b][:, 8:16], in_=cand2[b][:, :]), reads=[f'cand2{b}'], writes=[f'c16{b}'])
                P.op('dve', lambda e, b=b, tt=tt, hh=hh: e.tensor_scalar(out=sc[:, tt, hh, 0:1], in0=c16[b][:, 15:16], scalar1=0.999996, scalar2=None, op0=ALU.mult), reads=[f'c16{b}'], writes=['sc'])
                P.op('dve', lambda e, b=b, tt=tt, hh=hh: e.reduce_sum(out=sc[:, tt, hh, 1:2], in_=c16[b][:, :], axis=AX.X), reads=[f'c16{b}'], writes=['sc'])
        P.op('dve', lambda e: e.reciprocal(out=sc[:, :, :, 1], in_=sc[:, :, :, 1]), reads=['sc'], writes=['sc'])
        P.barrier()
        sb2.close()

        print('ops before B3', getattr(P, 'total', 0))
        EB = 4
        sb3 = ExitStack()
        Ust = [sb(sb3, f'Ust{i}', [128, 1024]) for i in range(2)]
        Vst = [sb(sb3, f'Vst{i}', [128, 1024]) for i in range(2)]
        Ub = sb(sb3, 'Ub', [128, DM], BF16)
        Vb = [sb(sb3, f'Vb{i}', [128, DM], BF16) for i in range(EB)]
        UT = sb(sb3, 'UT', [128, KC, 128], BF16)
        gelT = [sb(sb3, f'gelT{i}', [128, 1024], BF16) for i in range(2)]
        thw = [sb(sb3, f'thw{i}', [128, 3, 8, 8]) for i in range(2)]
        Yb = [sb(sb3, f'Yb{i}', [128, 8, 128], BF16) for i in range(3)]
        Dg = [sb(sb3, f'Dg{i}', [128, 8, 128], BF16) for i in range(3)]
        Mk = [sb(sb3, f'Mk{i}', [128, 8, 128], BF16) for i in range(3)]
        GAT = sb(sb3, 'GAT', [128, EB, 1024], BF16)
        ptr3 = ps(sb3, 'ptr3', [128, 8, 128], BF16)
        pA2 = [ps(sb3, f'pA2{i}', [128, 512]) for i in range(2)]
        pG = [ps(sb3, f'pG{i}', [128, 128]) for i in range(3)]
        pO = [ps(sb3, f'pO{i}', [128, 512]) for i in range(2)]
        LAG = 2
        pend = []

        def flush_one():
            il_, ib_, tt_, b_ = pend.pop(0)
            P.op('dve', lambda e: e.tensor_tensor(out=GAT[:, il_, tt_ * 128:(tt_ + 1) * 128], in0=pG[b_][:, :], in1=gelT[ib_][:, tt_ * 128:(tt_ + 1) * 128], op=ALU.mult),
                 reads=[('pG', b_), f'gelT{ib_}'], writes=[('GAT', il_, tt_)])
        def prepA_pieces(i):
            ib = i % 2
            tk = f'thw{ib}'
            pieces = []

            def p_load():
                for dh in range(2):
                    ds_ = slice(dh * 1024, (dh + 1) * 1024)
                    P.dma('sp', lambda e, dh=dh, ds_=ds_: e.dma_start(out=Ust[dh][:, :], in_=pu[i * 128:(i + 1) * 128, ds_]), f'pu{dh}', writes=[f'Ust{dh}'])
                    P.op('act', lambda e, dh=dh, ds_=ds_: e.activation(out=Ub[:, ds_], in_=Ust[dh][:, :], func=AF.Copy), reads=[f'Ust{dh}'], writes=[('Ub', dh)])
                P.op('dve', lambda e: e.tensor_scalar(out=thw[ib][:, 0, :, :], in0=em[:, :, 0:16:2, i], scalar1=1e-30, scalar2=None, op0=ALU.max), reads=['em'], writes=[tk])
                P.op('dve', lambda e: e.reciprocal(out=thw[ib][:, 0, :, :], in_=thw[ib][:, 0, :, :]), reads=[tk], writes=[tk])
                P.op('dve', lambda e: e.tensor_tensor(out=thw[ib][:, 1, :, :], in0=thw[ib][:, 0, :, :], in1=sc[:, :, :, 0], op=ALU.mult), reads=[tk, 'sc'], writes=[tk])
                P.op('dve', lambda e: e.tensor_tensor(out=thw[ib][:, 2, :, :], in0=em[:, :, 0:16:2, i], in1=sc[:, :, :, 1], op=ALU.mult), reads=['em', 'sc', tk], writes=[tk])
            pieces.append(p_load)

            def p_tr(half):
                def f():
                    for j in range(8):
                        kc = half * 8 + j
                        P.op('pe', lambda e, kc=kc, j=j: e.transpose(out=ptr3[:, j, :], in_=Ub[:, kc * 128:(kc + 1) * 128], identity=ident_b[:, :]),
                             reads=[('Ub', half), 'ident_b'], writes=['ptr3'])
                    P.op('act', lambda e: e.activation(out=UT[:, half * 8:half * 8 + 8, :], in_=ptr3[:, :, :], func=AF.Copy), reads=['ptr3'], writes=['UT'])
                return f
            pieces.append(p_tr(0))
            pieces.append(p_tr(1))

            def p_mm(th, k0, k1):
                def f():
                    for kc in range(k0, k1):
                        P.op('pe', lambda e, kc=kc: e.matmul(pA2[th][:, :], lhsT=UT[:, kc, :], rhs=xn2T[:, kc, th * 512:(th + 1) * 512], start=(kc == 0), stop=(kc == KC - 1)),
                             reads=['xn2T', 'UT'], writes=[f'pA2{th}'])
                    if k1 == KC:
                        P.op('act', lambda e: e.activation(out=gelT[ib][:, th * 512:(th + 1) * 512], in_=pA2[th][:, :], func=AF.Gelu), reads=[f'pA2{th}'], writes=[f'gelT{ib}'])
                return f
            for th in range(2):
                for k0 in (0, 6, 11):
                    pieces.append(p_mm(th, k0, {0: 6, 6: 11, 11: 16}[k0]))
            return pieces

        for pc in prepA_pieces(0):
            pc()
        for blk in range(NEI // EB):
            for il in range(EB):
                i = blk * EB + il
                ib = i % 2
                tk = f'thw{ib}'
                for dh in range(2):
                    ds_ = slice(dh * 1024, (dh + 1) * 1024)
                    P.dma('sp', lambda e, i=i, dh=dh, ds_=ds_: e.dma_start(out=Vst[dh][:, :], in_=pv_[i * 128:(i + 1) * 128, ds_]), f'pv{dh}', writes=[f'Vst{dh}'])
                    P.op('act', lambda e, dh=dh, ds_=ds_, il=il: e.activation(out=Vb[il][:, ds_], in_=Vst[dh][:, :], func=AF.Copy), reads=[f'Vst{dh}'], writes=[('Vb', il)])
                nxt = prepA_pieces(i + 1) if i + 1 < NEI else []
                if nxt:
                    nxt.pop(0)()
                for tt in range(8):
                    n = il * 8 + tt
                    b = n % 3
                    gb = n % 3
                    mb = n % 3
                    P.op('pool', lambda e, b=b, ib=ib, tt=tt: e.tensor_tensor(out=Dg[b][:, 0:4, :], in0=ident_f[:, None, :].to_broadcast([128, 4, 128]),
                                                                         in1=thw[ib][:, 2, tt, 0:4, None].to_broadcast([128, 4, 128]), op=ALU.mult),
                         reads=['ident_f', tk], writes=[(f'Dg{b}', 0)])
                    for hh in range(4, 8):
                        P.op('act', lambda e, b=b, ib=ib, tt=tt, hh=hh: e.activation(out=Dg[b][:, hh, :], in_=ident_f[:, :], func=AF.Copy, scale=thw[ib][:, 2, tt, hh:hh + 1]),
                             reads=['ident_f', tk], writes=[(f'Dg{b}', hh)])
                    P.op('dve', lambda e, mb=mb, ib=ib, tt=tt: e.tensor_tensor(out=Mk[mb][:, :, :], in0=em[:, tt, 1:16:2, :], in1=thw[ib][:, 1, tt, :, None].to_broadcast([128, 8, 128]), op=ALU.is_ge),
                         reads=['em', tk], writes=[f'Mk{mb}'])
                    P.op('dve', lambda e, b=b, mb=mb, tt=tt: e.tensor_tensor(out=Yb[b][:, :, :], in0=Mk[mb][:, :, :], in1=em[:, tt, 1:16:2, :], op=ALU.mult),
                         reads=['em', f'Mk{mb}'], writes=[f'Yb{b}'])
                    for hh in range(8):
                        P.op('pe', lambda e, b=b, hh=hh, gb=gb: e.matmul(pG[gb][:, :], lhsT=Yb[b][:, hh, :], rhs=Dg[b][:, hh, :], start=(hh == 0), stop=(hh == 7)),
                             reads=[f'Yb{b}', (f'Dg{b}', 0 if hh < 4 else hh)], writes=[('pG', gb)])
                    pend.append((il, ib, tt, gb))
                    if len(pend) > LAG:
                        flush_one()
                    if nxt:
                        nxt.pop(0)()
                while nxt:
                    nxt.pop(0)()
            while pend:
                flush_one()
            for tt in range(8):
                for nblk in range(4):
                    ob = nblk % 2
                    for il in range(EB):
                        P.op('pe', lambda e, il=il, tt=tt, nblk=nblk, ob=ob: e.matmul(pO[ob][:, :], lhsT=GAT[:, il, tt * 128:(tt + 1) * 128], rhs=Vb[il][:, nblk * 512:(nblk + 1) * 512], start=(il == 0), stop=(il == EB - 1)),
                             reads=[('GAT', il, tt), ('Vb', il)], writes=[('pO', ob)])
                    P.op('dve', lambda e, tt=tt, nblk=nblk, ob=ob: e.tensor_tensor(out=h1[:, tt, nblk * 512:(nblk + 1) * 512], in0=h1[:, tt, nblk * 512:(nblk + 1) * 512], in1=pO[ob][:, :], op=ALU.add),
                         reads=[('pO', ob), ('h1', tt, nblk)], writes=[('h1', tt, nblk)])
        P.barrier()
        for nh in range(2):
            P.dma('sp', lambda e, nh=nh: e.dma_start(out=Vst[nh][:, :], in_=gfr[:, nh * 1024:(nh + 1) * 1024]), f'pv{nh}', writes=[f'Vst{nh}'])
        for tt in range(8):
            b = tt % 2
            rms(tt, b, Ub, ('Ub', 0))
            for nh in range(2):
                P.op('dve', lambda e, tt=tt, b=b, nh=nh: e.scalar_tensor_tensor(out=Ust[nh][:, :], in0=h1[:, tt, nh * 1024:(nh + 1) * 1024], scalar=rstd2[b][:, 0:1], in1=Vst[nh][:, :], op0=ALU.mult, op1=ALU.mult),
                     reads=['h1', f'rstd2{b}', f'Vst{nh}'], writes=[f'Ust{nh}'])
                P.dma('sp', lambda e, tt=tt, nh=nh: e.dma_start(out=yout[tt * 128:(tt + 1) * 128, nh * 1024:(nh + 1) * 1024], in_=Ust[nh][:, :]), f'yo{nh}', reads=[f'Ust{nh}'], writes=[('y', tt, nh)])
        P.barrier()
        sb3.close()
        sB.close()
        P.emit(top)
    return nc


def host_inputs(inp, r):
    b, h = r // 4, r % 4
    w_in = inp['w_in'][0]
    cols = np.concatenate([
        np.arange(h * 256, (h + 1) * 256),
        1024 + np.arange(h * 256, (h + 1) * 256),
        4104 + np.arange(h * 256, (h + 1) * 256),
        2048 + np.arange(h * 256, (h + 1) * 256),
        np.array([4096 + h, 4100 + h]),
        3072 + np.arange(h * 256, (h + 1) * 256),
    ])
    w_a = np.ascontiguousarray(w_in[:, cols].reshape(KC, 128, 1282).transpose(1, 0, 2))
    g1 = np.ascontiguousarray(inp['norm1_g'][0].reshape(KC, 128).T)
    bgv = inp['b_gates'][0]
    bg = np.ascontiguousarray(np.broadcast_to(np.array([bgv[h], bgv[4 + h]], np.float32)[None, :], (128, 2)))
    cwf = inp['conv_qk_w'][0]
    chans = np.concatenate([np.arange(h * 256, (h + 1) * 256), 1024 + np.arange(h * 256, (h + 1) * 256)])
    convw = np.ascontiguousarray(cwf[:, chans].T.reshape(4, 128, 4).transpose(1, 0, 2))
    mg = np.ascontiguousarray(np.broadcast_to(inp['mlstm_norm_g'][0][h * 256:(h + 1) * 256][None, :], (128, 256)))

    G0 = 16 * h
    lre = inp['s5_lambda_re'][0][G0:G0 + 16]
    lim = inp['s5_lambda_im'][0][G0:G0 + 16]
    ldt = np.broadcast_to(inp['s5_log_dt'][0][G0:G0 + 16][:, None], (16, 64))
    def colrow(a):
        col = a.reshape(8, 128).T
        row = a.reshape(1024)
        return col, row
    cols_, rows_ = zip(*[colrow(np.asarray(a, np.float32)) for a in (lre, lim, ldt)])
    s5c = np.ascontiguousarray(np.stack(cols_, axis=1))
    s5r = np.ascontiguousarray(np.broadcast_to(np.stack(rows_, axis=0)[None], (128, 3, 1024)))
    s5b = np.zeros((128, 2, 8, 128), np.float32)
    s5cm = np.zeros((128, 2, 8, 128), np.float32)
    for ri, (bsrc, csrc) in enumerate(((inp['s5_b_re'][0], inp['s5_c_re'][0]), (inp['s5_b_im'][0], inp['s5_c_im'][0]))):
        for gl in range(16):
            q, g2 = gl // 2, gl % 2
            r0 = (gl % 8) * 16
            s5b[r0:r0 + 16, ri, q, g2 * 64:(g2 + 1) * 64] = bsrc[G0 + gl].T
            s5cm[g2 * 64:(g2 + 1) * 64, ri, q, r0:r0 + 16] = csrc[G0 + gl].T
    s5d = np.ascontiguousarray(inp['s5_d'][0][G0:G0 + 16].reshape(2, 128).T)
    s5gw = np.zeros((128, 4, 128), np.float32)
    gw = inp['s5_glu_w'][0]
    for gl in range(16):
        cc, r0 = gl // 8, (gl % 8) * 16
        s5gw[r0:r0 + 16, cc * 2, r0:r0 + 16] = gw[G0 + gl][:, :16]
        s5gw[r0:r0 + 16, cc * 2 + 1, r0:r0 + 16] = gw[G0 + gl][:, 16:]

    perm = np.concatenate([np.concatenate([256 * hh + np.arange(256), 1024 + 256 * hh + np.arange(256)]) for hh in range(4)])
    w_o = np.ascontiguousarray(inp['w_out'][0][perm].reshape(KC, 128, DM).transpose(1, 0, 2))
    w_q = np.ascontiguousarray(inp['peer_wq'][0].reshape(KC, 128, DM).transpose(1, 0, 2))
    g2 = inp['norm2_g'][0]
    g2c = np.ascontiguousarray(g2.reshape(KC, 128).T)
    g2r = np.ascontiguousarray(np.broadcast_to(g2[None, :], (128, DM)))
    gfr = np.ascontiguousarray(np.broadcast_to(inp['final_g'][None, :], (128, DM)))
    skT = np.ascontiguousarray(inp['peer_subkeys'][0].transpose(2, 0, 1))
    d = dict(
        x_own=np.ascontiguousarray(inp['x'][b][h * 1024:(h + 1) * 1024]), w_o=w_o, w_q=w_q, g2c=g2c, g2r=g2r, gfr=gfr, skT=skT,
        pu=inp['peer_u'][0], pv=inp['peer_v'][0], sel=np.ascontiguousarray(np.broadcast_to(np.eye(4, dtype=np.float32)[h][None, :], (128, 4))),
        s5c=s5c, s5r=s5r, s5b=s5b.reshape(128, 2, 1024), s5cm=s5cm.reshape(128, 2, 1024), s5d=s5d, s5gw=s5gw,
        x=np.ascontiguousarray(inp['x'][b]),
        w_a=w_a, g1=g1, bg=bg, convw=convw, mg=mg,
        c_ident=_ident(),
        c_triu=np.triu(np.ones((128, 128), np.float32)),
        c_ones=np.ones((128, 128), np.float32),
    )
    return d


def kernel(**inputs):
    inp = {k: np.asarray(v) for k, v in inputs.items()}
    nc = build('full')
    in_maps = [host_inputs(inp, r) for r in range(8)]
    res = run_bass_kernel_spmd(nc, in_maps, core_ids=list(range(8)))
    out = np.zeros((2, NT, DM), np.float32)
    for r in range(8):
        b, h = r // 4, r % 4
        out[b, h * 1024:(h + 1) * 1024] = res.results[r]['y']
    return out
```

```python
import os
import numpy as np
import ml_dtypes
from contextlib import ExitStack
import concourse.bass as bass
import concourse.mybir as mybir
from concourse.bass_utils import run_bass_kernel_spmd

F32 = mybir.dt.float32
BF16 = mybir.dt.bfloat16
ALU = mybir.AluOpType
AF = mybir.ActivationFunctionType
AX = mybir.AxisListType

EPS = 1e-6
NT = 4096
DM = 2048
KC = 16


class Prog:
    ENG = ['pe', 'dve', 'act', 'pool', 'sp']

    def __init__(self, nc):
        self.nc = nc
        self.ops = {e: [] for e in self.ENG}
        self.cnt = {e: 0 for e in self.ENG}
        self.dcnt = {}
        self.lastw = {}
        self.rds = {}
        self.floor = {}

    def _tok_add(self, d, tok):
        s, v, e = tok
        if s not in d or d[s][0] < v:
            d[s] = (v, e)

    def _mk(self, reads, writes):
        deps = dict(self.floor)
        for k in reads:
            if k in self.lastw:
                self._tok_add(deps, self.lastw[k])
        for k in writes:
            if k in self.lastw:
                self._tok_add(deps, self.lastw[k])
            for s, (v, e) in self.rds.get(k, {}).items():
                self._tok_add(deps, (s, v, e))
        return deps

    def _commit(self, tok, reads, writes):
        for k in reads:
            self._tok_add(self.rds.setdefault(k, {}), tok)
        for k in writes:
            self.lastw[k] = tok
            self.rds[k] = {}

    def _skip(self, force):
        self.total = getattr(self, 'total', 0) + 1
        cut = int(os.environ.get('KCUT', '0'))
        return bool(cut) and self.total > cut and not force

    def op(self, eng, fn, reads=(), writes=(), force=False):
        if self._skip(force):
            return
        deps = self._mk(reads, writes)
        self.cnt[eng] += 1
        tok = (eng, self.cnt[eng], eng)
        self.ops[eng].append((deps, fn, eng, 1))
        self._commit(tok, reads, writes)

    def dma(self, queue, fn, stream, reads=(), writes=(), inc=16, force=False):
        if self._skip(force):
            return
        deps = self._mk(reads, writes)
        if stream == 'c':
            self.nuniq = getattr(self, 'nuniq', 0) + 1
            stream = f'c{self.nuniq}'
        s = 'd_' + stream
        self.dcnt[s] = self.dcnt.get(s, 0) + inc
        tok = (s, self.dcnt[s], 'dma')
        self.ops[queue].append((deps, fn, s, inc))
        self._commit(tok, reads, writes)

    def barrier(self):
        for e in self.ENG:
            if self.cnt[e]:
                self.floor[e] = (self.cnt[e], e)
        for s, v in self.dcnt.items():
            self.floor[s] = (v, 'dma')

    def emit(self, stack, final_waits=True):
        nc = self.nc
        sems = {}
        for e in self.ENG:
            sems[e] = stack.enter_context(nc.semaphore('sem_' + e))
        for s in self.dcnt:
            sems[s] = stack.enter_context(nc.semaphore('sem_' + s))
        block = stack.enter_context(nc.Block())
        total = dict((e, (self.cnt[e], e)) for e in self.ENG if self.cnt[e])
        for s, v in self.dcnt.items():
            total[s] = (v, 'dma')

        def run(ename):
            def body(eng):
                seen = {}
                for deps, fn, sname, inc in self.ops[ename]:
                    for s, (v, de) in deps.items():
                        if de == ename and ename == 'pe':
                            continue
                        if seen.get(s, 0) < v:
                            eng.wait_ge(sems[s], v)
                            seen[s] = v
                    ins = fn(eng)
                    ins.then_inc(sems[sname], inc)
                if ename == 'sp':
                    for s, (v, de) in total.items():
                        if seen.get(s, 0) < v:
                            eng.wait_ge(sems[s], v)
            return body
        block.tensor(run('pe'))
        block.vector(run('dve'))
        block.scalar(run('act'))
        block.gpsimd(run('pool'))
        block.sync(run('sp'))


def _ident():
    return np.eye(128, dtype=np.float32)


def build(stage='full'):
    nc = bass.Bass("TRN2", target_bir_lowering=False)
    P = Prog(nc)
    D = {}

    def din(name, shape, dt=F32):
        D[name] = nc.dram_tensor(name, list(shape), dt, kind="ExternalInput").ap()
        return D[name]

    x = din('x', [NT, DM])
    w_a = din('w_a', [128, KC, 1282])
    g1 = din('g1', [128, KC])
    bg = din('bg', [128, 2])
    convw = din('convw', [128, 4, 4])
    mg = din('mg', [128, 256])
    c_ident = din('c_ident', [128, 128])
    c_triu = din('c_triu', [128, 128])
    c_ones = din('c_ones', [128, 128])

    s5c = din('s5c', [128, 3, 8])
    s5r = din('s5r', [128, 3, 1024])
    s5b = din('s5b', [128, 2, 1024])
    s5cm = din('s5cm', [128, 2, 1024])
    s5d = din('s5d', [128, 2])
    s5gw = din('s5gw', [128, 4, 128])
    x_own = din('x_own', [1024, DM])
    w_o = din('w_o', [128, KC, DM])
    w_q = din('w_q', [128, KC, DM])
    g2c = din('g2c', [128, KC])
    g2r = din('g2r', [128, DM])
    gfr = din('gfr', [128, DM])
    skT = din('skT', [128, 2, 128])
    pu = din('pu', [16384, DM])
    pv_ = din('pv', [16384, DM])
    selin = din('sel', [128, 4])
    if stage == 'simB':
        mx_test = din('mx_test', [2048, NT], BF16)
    if stage in ('full', 'simB'):
        yout = nc.dram_tensor('y', [1024, DM], F32, kind="ExternalOutput").ap()
    mxi = [nc.dram_tensor(f'mxi{q}', [512, 1024], BF16) for q in range(4)]
    mxa = [nc.dram_tensor(f'mxa{q}', [2048, 1024], BF16) for q in range(4)]

    if stage in ('A1', 'A2', 'A3'):
        dbg = nc.dram_tensor('dbg', [512, NT], BF16, kind="ExternalOutput").ap()

    top = ExitStack()
    with top:
        def sb(stack, name, shape, dt=F32):
            return stack.enter_context(nc.sbuf_tensor(name, list(shape), dt))

        def ps(stack, name, shape, dt=F32):
            return stack.enter_context(nc.psum_tensor(name, list(shape), dt))

        ident_f = sb(top, 'ident_f', [128, 128])
        ident_b = sb(top, 'ident_b', [128, 128], BF16)
        triu = sb(top, 'triu', [128, 128])
        ones = sb(top, 'ones', [128, 128])
        P.dma('sp', lambda e: e.dma_start(out=ident_f[:, :], in_=c_ident[:, :]), 'c', writes=['ident_f'])
        P.dma('sp', lambda e: e.dma_start(out=triu[:, :], in_=c_triu[:, :]), 'c', writes=['triu'])
        P.dma('sp', lambda e: e.dma_start(out=ones[:, :], in_=c_ones[:, :]), 'c', writes=['ones'])
        P.op('dve', lambda e: e.tensor_copy(out=ident_b[:, :], in_=ident_f[:, :]), reads=['ident_f'], writes=['ident_b'])

        sA = ExitStack()
        uT = sb(sA, 'uT', [128, 2, NT], BF16)
        sA2 = ExitStack()
        qT = sb(sA2, 'qT', [128, 2, NT], BF16)
        kT = sb(sA2, 'kT', [128, 2, NT], BF16)
        v_aug = sb(sA2, 'v_aug', [128, 32, 257], BF16)
        gso = sb(sA2, 'gso', [128, 32, 256], BF16)
        g_tm = sb(sA2, 'g_tm', [128, 32, 2])
        bgt = sb(sA2, 'bgt', [128, 2])
        P.op('pool', lambda e: e.memset(v_aug[:, :, 256:257], 1.0), writes=['v_aug'])

        s1 = ExitStack()
        W = sb(s1, 'W', [128, KC, 1282], BF16)
        wst = [sb(s1, f'wst{i}', [128, 1282]) for i in range(2)]
        g1t = sb(s1, 'g1t', [128, KC])
        cw = sb(s1, 'cw', [128, 4, 4])
        mgt = sb(s1, 'mgt', [128, 256])
        xt = [sb(s1, f'xt{i}', [128, DM]) for i in range(2)]
        xs = [sb(s1, f'xs{i}', [128, DM], BF16) for i in range(2)]
        junk = sb(s1, 'junk', [128, DM], BF16)
        ssq = [sb(s1, f'ssq{i}', [128, 1]) for i in range(2)]
        rstd = [sb(s1, f'rstd{i}', [128, 1]) for i in range(2)]
        xnT = sb(s1, 'xnT', [128, KC, 512], BF16)
        pre = sb(s1, 'pre', [128, 4, 515])
        cacc = [sb(s1, f'cacc{i}', [128, 512]) for i in range(2)]
        sgo = [sb(s1, f'sgo{i}', [128, 256]) for i in range(2)]
        ptr = [ps(s1, f'ptr{i}', [128, 8, 128], BF16) for i in range(2)]
        pf = [ps(s1, f'pf{i}', [128, 512]) for i in range(2)]
        pv = [ps(s1, f'pv{i}', [128, 258]) for i in range(2)]
        po = [ps(s1, f'po{i}', [128, 256]) for i in range(2)]

        P.dma('sp', lambda e: e.dma_start(out=g1t[:, :], in_=g1[:, :]), 'c', writes=['g1t'])
        P.dma('sp', lambda e: e.dma_start(out=bgt[:, :], in_=bg[:, :]), 'c', writes=['bgt'])
        P.dma('sp', lambda e: e.dma_start(out=cw[:, :, :], in_=convw[:, :, :]), 'c', writes=['cw'])
        P.dma('sp', lambda e: e.dma_start(out=mgt[:, :], in_=mg[:, :]), 'c', writes=['mgt'])
        for kc in range(KC):
            b = kc % 2
            P.dma('sp', lambda e, kc=kc, b=b: e.dma_start(out=wst[b][:, :], in_=w_a[:, kc, :]), f'w{b}', writes=[f'wst{b}'])
            P.op('dve' if kc % 2 == 0 else 'pool',
                 lambda e, kc=kc, b=b: e.tensor_scalar(out=W[:, kc, :], in0=wst[b][:, :], scalar1=g1t[:, kc:kc + 1], scalar2=None, op0=ALU.mult),
                 reads=[f'wst{b}', 'g1t'], writes=[('W', kc)])
        Wkeys = [('W', kc) for kc in range(KC)]
        for m in range(4):
            P.op('pool', lambda e, m=m: e.memset(pre[:, m, 0:3], 0.0), writes=[('pre', m)])

        print('ops before token loop', getattr(P, 'total', 0))
        for tb in range(8):
            print('tb', tb, getattr(P, 'total', 0))
            for t4 in range(4):
                c = tb * 4 + t4
                b = c % 2
                P.dma('sp', lambda e, c=c, b=b: e.dma_start(out=xt[b][:, :], in_=x[c * 128:(c + 1) * 128, :]), f'x{b}', writes=[f'xt{b}'])
                P.op('act', lambda e, b=b: e.activation(out=junk[:, :], in_=xt[b][:, :], func=AF.Square, accum_out=ssq[b][:, :]),
                     reads=[f'xt{b}'], writes=['junk', f'ssq{b}'])
                P.op('dve', lambda e, b=b: e.tensor_scalar(out=rstd[b][:, :], in0=ssq[b][:, :], scalar1=1.0 / DM, scalar2=EPS, op0=ALU.mult, op1=ALU.add),
                     reads=[f'ssq{b}'], writes=[f'rstd{b}'])
                P.op('act', lambda e, b=b: e.activation(out=rstd[b][:, :], in_=rstd[b][:, :], func=AF.Sqrt),
                     reads=[f'rstd{b}'], writes=[f'rstd{b}'])
                P.op('dve', lambda e, b=b: e.reciprocal(out=rstd[b][:, :], in_=rstd[b][:, :]),
                     reads=[f'rstd{b}'], writes=[f'rstd{b}'])
                P.op('dve', lambda e, b=b: e.tensor_scalar(out=xs[b][:, :], in0=xt[b][:, :], scalar1=rstd[b][:, 0:1], scalar2=None, op0=ALU.mult),
                     reads=[f'xt{b}', f'rstd{b}'], writes=[f'xs{b}'])
                for half in range(2):
                    for j in range(8):
                        kc = half * 8 + j
                        P.op('pe', lambda e, b=b, kc=kc, half=half, j=j: e.transpose(out=ptr[half][:, j, :], in_=xs[b][:, kc * 128:(kc + 1) * 128], identity=ident_b[:, :]),
                             reads=[f'xs{b}', 'ident_b'], writes=[f'ptr{half}'])
                    P.op('act' if half == 0 else 'dve',
                         (lambda e, half=half, t4=t4: e.activation(out=xnT[:, half * 8:half * 8 + 8, t4 * 128:(t4 + 1) * 128], in_=ptr[half][:, :, :], func=AF.Copy)) if half == 0 else
                         (lambda e, half=half, t4=t4: e.tensor_copy(out=xnT[:, half * 8:half * 8 + 8, t4 * 128:(t4 + 1) * 128], in_=ptr[half][:, :, :])),
                         reads=[f'ptr{half}'], writes=[('xnT', t4)])
            xk = [('xnT', t) for t in range(4)]
            for m in range(6):
                pb = m % 2
                for kc in range(KC):
                    P.op('pe', lambda e, m=m, kc=kc, pb=pb: e.matmul(pf[pb][:, :], lhsT=W[:, kc, m * 128:(m + 1) * 128], rhs=xnT[:, kc, :], start=(kc == 0), stop=(kc == KC - 1)),
                         reads=xk + Wkeys, writes=[f'pf{pb}'])
                if m < 4:
                    P.op('act', lambda e, m=m, pb=pb: e.activation(out=pre[:, m, 3:515], in_=pf[pb][:, :], func=AF.Copy),
                         reads=[f'pf{pb}'], writes=[('pre', m)])
                    cb = m % 2
                    P.op('dve', lambda e, m=m, cb=cb: e.tensor_scalar(out=cacc[cb][:, :], in0=pre[:, m, 0:512], scalar1=cw[:, m, 0:1], scalar2=None, op0=ALU.mult),
                         reads=[('pre', m), 'cw'], writes=[f'cacc{cb}'])
                    for j in range(1, 4):
                        P.op('dve', lambda e, m=m, cb=cb, j=j: e.scalar_tensor_tensor(out=cacc[cb][:, :], in0=pre[:, m, j:j + 512], scalar=cw[:, m, j:j + 1], in1=cacc[cb][:, :], op0=ALU.mult, op1=ALU.add),
                             reads=[('pre', m), 'cw', f'cacc{cb}'], writes=[f'cacc{cb}'])
                    dst = qT if m < 2 else kT
                    P.op('act', lambda e, m=m, cb=cb, dst=dst, tb=tb: e.activation(out=dst[:, m % 2, tb * 512:(tb + 1) * 512], in_=cacc[cb][:, :], func=AF.Silu),
                         reads=[f'cacc{cb}'], writes=[('qk', m, tb)])
                    P.op('pool', lambda e, m=m: e.tensor_copy(out=pre[:, m, 0:3], in_=pre[:, m, 512:515]),
                         reads=[('pre', m)], writes=[('pre', m)])
                else:
                    P.op('dve', lambda e, m=m, pb=pb, tb=tb: e.tensor_copy(out=uT[:, m - 4, tb * 512:(tb + 1) * 512], in_=pf[pb][:, :]),
                         reads=[f'pf{pb}'], writes=[('uT', m - 4, tb)])
            for t4 in range(4):
                c = tb * 4 + t4
                pb = c % 2
                for kc in range(KC):
                    P.op('pe', lambda e, kc=kc, pb=pb, t4=t4: e.matmul(pv[pb][:, :], lhsT=xnT[:, kc, t4 * 128:(t4 + 1) * 128], rhs=W[:, kc, 768:1026], start=(kc == 0), stop=(kc == KC - 1)),
                         reads=xk + Wkeys, writes=[f'pv{pb}'])
                for kc in range(KC):
                    P.op('pe', lambda e, kc=kc, pb=pb, t4=t4: e.matmul(po[pb][:, :], lhsT=xnT[:, kc, t4 * 128:(t4 + 1) * 128], rhs=W[:, kc, 1026:1282], start=(kc == 0), stop=(kc == KC - 1)),
                         reads=xk + Wkeys, writes=[f'po{pb}'])
                P.op('dve', lambda e, c=c, pb=pb: e.tensor_copy(out=v_aug[:, c, 0:256], in_=pv[pb][:, 0:256]),
                     reads=[f'pv{pb}'], writes=[('v', c)])
                P.op('dve', lambda e, c=c, pb=pb: e.tensor_copy(out=g_tm[:, c, :], in_=pv[pb][:, 256:258]),
                     reads=[f'pv{pb}'], writes=[('g_tm', c)])
                P.op('act', lambda e, pb=pb: e.activation(out=sgo[pb][:, :], in_=po[pb][:, :], func=AF.Sigmoid),
                     reads=[f'po{pb}'], writes=[f'sgo{pb}'])
                P.op('pool', lambda e, c=c, pb=pb: e.tensor_tensor(out=gso[:, c, :], in0=sgo[pb][:, :], in1=mgt[:, :], op=ALU.mult),
                     reads=[f'sgo{pb}', 'mgt'], writes=[('gso', c)])
        P.barrier()
        s1.close()
        if stage == 'A1':
            P.dma('sp', lambda e: e.dma_start(out=dbg[0:128, :], in_=qT[:, 0, :]), 'out', writes=['dbg'], force=True)
            P.dma('sp', lambda e: e.dma_start(out=dbg[128:256, :], in_=kT[:, 1, :]), 'out', writes=['dbg'], force=True)
            P.dma('sp', lambda e: e.dma_start(out=dbg[256:384, :], in_=uT[:, 0, :]), 'out', writes=['dbg'], force=True)
            sA2.close()
            sA.close()
            P.emit(top)
            return nc

        print('ops before A2', getattr(P, 'total', 0))
        s2 = ExitStack()
        li = sb(s2, 'li', [128, 32])
        nlf = sb(s2, 'nlf', [128, 32])
        nega = sb(s2, 'nega', [128, 32])
        tmp = sb(s2, 'tmp', [128, 32])
        tmp2 = sb(s2, 'tmp2', [128, 32])
        es = sb(s2, 'es', [128, 32])
        wk = sb(s2, 'wk', [128, 32])
        eA = sb(s2, 'eA', [128, 32])
        nlfrep = sb(s2, 'nlfrep', [128, 32, 128])
        ea_bc = sb(s2, 'ea_bc', [128, NT])
        lnc = sb(s2, 'lnc', [128, 1])
        Cst = sb(s2, 'Cst', [128, 2, 257])
        Cb = sb(s2, 'Cb', [128, 2, 257], BF16)
        Pm = [sb(s2, f'Pm{i}', [128, 128], BF16) for i in range(2)]
        kw = [sb(s2, f'kw{i}', [128, 256], BF16) for i in range(2)]
        hmn = [sb(s2, f'hmn{i}', [128, 256], BF16) for i in range(2)]
        hT = [sb(s2, f'hT{i}', [128, 2, 128], BF16) for i in range(2)]
        sm = [sb(s2, f'sm{i}', [128, 8]) for i in range(2)]
        junk2 = sb(s2, 'junk2', [128, 256], BF16)
        pa = ps(s2, 'pa', [128, 64])
        pbc = ps(s2, 'pbc', [128, 512])
        ps1 = ps(s2, 'ps1', [128, 128])
        ptk = ps(s2, 'ptk', [128, 2, 128], BF16)
        psO = ps(s2, 'psO', [128, 257])
        pth = ps(s2, 'pth', [128, 2, 128], BF16)
        psC = [ps(s2, f'psC{i}', [128, 257]) for i in range(2)]

        gk = [('g_tm', c) for c in range(32)]
        P.op('dve', lambda e: e.tensor_scalar(out=li[:, :], in0=g_tm[:, :, 0], scalar1=bgt[:, 0:1], scalar2=None, op0=ALU.add), reads=gk + ['bgt'], writes=['li'])
        P.op('dve', lambda e: e.tensor_scalar(out=tmp[:, :], in0=g_tm[:, :, 1], scalar1=bgt[:, 1:2], scalar2=None, op0=ALU.add), reads=gk + ['bgt'], writes=['tmp'])
        P.op('act', lambda e: e.activation(out=tmp2[:, :], in_=tmp[:, :], func=AF.Exp, scale=-1.0), reads=['tmp'], writes=['tmp2'])
        P.op('dve', lambda e: e.tensor_scalar(out=tmp2[:, :], in0=tmp2[:, :], scalar1=1.0, scalar2=None, op0=ALU.add), reads=['tmp2'], writes=['tmp2'])
        P.op('act', lambda e: e.activation(out=nlf[:, :], in_=tmp2[:, :], func=AF.Ln), reads=['tmp2'], writes=['nlf'])
        P.op('dve', lambda e: e.memset(lnc[:, :], float(np.log(1.0 / 16.0))), writes=['lnc'])
        P.op('pe', lambda e: e.matmul(pa[:, 0:32], lhsT=triu[:, :], rhs=nlf[:, :], start=True, stop=True), reads=['triu', 'nlf'], writes=['pa'])
        P.op('pe', lambda e: e.matmul(pa[:, 32:64], lhsT=ones[:, :], rhs=nlf[:, :], start=True, stop=True), reads=['ones', 'nlf'], writes=['pa'])
        P.op('dve', lambda e: e.tensor_copy(out=nega[:, :], in_=pa[:, 0:32]), reads=['pa'], writes=['nega'])
        P.op('dve', lambda e: e.tensor_tensor(out=tmp[:, :], in0=li[:, :], in1=nega[:, :], op=ALU.add), reads=['li', 'nega'], writes=['tmp'])
        P.op('act', lambda e: e.activation(out=es[:, :], in_=tmp[:, :], func=AF.Exp), reads=['tmp'], writes=['es'])
        P.op('dve', lambda e: e.tensor_tensor(out=tmp2[:, :], in0=tmp[:, :], in1=pa[:, 32:64], op=ALU.subtract), reads=['tmp', 'pa'], writes=['tmp2'])
        P.op('act', lambda e: e.activation(out=wk[:, :], in_=tmp2[:, :], func=AF.Exp), reads=['tmp2'], writes=['wk'])
        P.op('act', lambda e: e.activation(out=eA[:, :], in_=pa[:, 32:64], func=AF.Exp, scale=-1.0), reads=['pa'], writes=['eA'])
        P.op('dve', lambda e: e.tensor_copy(out=nlfrep[:, :, :], in_=nlf[:, :].to_broadcast([128, 32, 128]) if False else nlf[:, :, None].to_broadcast([128, 32, 128])),
             reads=['nlf'], writes=['nlfrep'])
        for k8 in range(8):
            for j in range(4):
                c = k8 * 4 + j
                P.op('pe', lambda e, c=c, j=j: e.matmul(pbc[:, j * 128:(j + 1) * 128], lhsT=nlfrep[:, c, :], rhs=triu[:, :], start=True, stop=True),
                     reads=['nlfrep', 'triu'], writes=['pbc'])
            P.op('act', lambda e, k8=k8: e.activation(out=ea_bc[:, k8 * 512:(k8 + 1) * 512], in_=pbc[:, :], func=AF.Exp, scale=-1.0, bias=lnc[:, 0:1]),
                 reads=['pbc', 'lnc'], writes=[('ea_bc', k8)])
            for dc in range(2):
                P.op('dve' if dc == 0 else 'pool', lambda e, k8=k8, dc=dc: e.tensor_tensor(out=qT[:, dc, k8 * 512:(k8 + 1) * 512], in0=qT[:, dc, k8 * 512:(k8 + 1) * 512], in1=ea_bc[:, k8 * 512:(k8 + 1) * 512], op=ALU.mult),
                     reads=[('ea_bc', k8), ('qk', dc, k8)], writes=[('qk', dc, k8)])
        P.op('dve', lambda e: e.memset(Cst[:, :, :], 0.0), writes=['Cst'])
        P.op('pool', lambda e: e.memset(Cb[:, :, :], 0.0), writes=['Cb'])

        for c in range(32):
            b = c % 2
            tb = c // 4
            cs = slice(c * 128, (c + 1) * 128)
            qkk = [('qk', m, tb) for m in range(4)]
            for dc in range(2):
                P.op('pe', lambda e, dc=dc, cs=cs: e.matmul(ps1[:, :], lhsT=kT[:, dc, cs], rhs=qT[:, dc, cs], start=(dc == 0), stop=(dc == 1)),
                     reads=qkk, writes=['ps1'])
            P.op('dve', lambda e, b=b, c=c: e.scalar_tensor_tensor(out=Pm[b][:, :], in0=ps1[:, :], scalar=es[:, c:c + 1], in1=triu[:, :], op0=ALU.mult, op1=ALU.mult),
                 reads=['ps1', 'es', 'triu'], writes=[f'Pm{b}'])
            for dc in range(2):
                P.op('pe', lambda e, dc=dc, cs=cs: e.transpose(out=ptk[:, dc, :], in_=kT[:, dc, cs], identity=ident_b[:, :]),
                     reads=qkk + ['ident_b'], writes=['ptk'])
            P.op('act', lambda e, b=b, c=c: e.activation(out=kw[b][:, :], in_=ptk[:, :, :], func=AF.Copy, scale=wk[:, c:c + 1]),
                 reads=['ptk', 'wk'], writes=[f'kw{b}'])
            P.op('pe', lambda e, b=b, c=c: e.matmul(psO[:, :], lhsT=Pm[b][:, :], rhs=v_aug[:, c, :], start=True, stop=False),
                 reads=[f'Pm{b}', ('v', c), 'v_aug'], writes=['psO'])
            for dc in range(2):
                P.op('pe', lambda e, dc=dc, cs=cs: e.matmul(psO[:, :], lhsT=qT[:, dc, cs], rhs=Cb[:, dc, :], start=False, stop=(dc == 1)),
                     reads=qkk + ['Cb'], writes=['psO'])
            P.op('act', lambda e, b=b: e.activation(out=sm[b][:, 0:1], in_=psO[:, 256:257], func=AF.Abs), reads=['psO'], writes=[f'sm{b}'])
            P.op('dve', lambda e, b=b: e.tensor_scalar(out=sm[b][:, 0:1], in0=sm[b][:, 0:1], scalar1=1.0, scalar2=None, op0=ALU.max), reads=[f'sm{b}'], writes=[f'sm{b}'])
            P.op('dve', lambda e, b=b: e.reciprocal(out=sm[b][:, 1:2], in_=sm[b][:, 0:1]), reads=[f'sm{b}'], writes=[f'sm{b}'])
            P.op('act', lambda e, b=b: e.activation(out=junk2[:, :], in_=psO[:, 0:256], func=AF.Square, accum_out=sm[b][:, 2:3]), reads=['psO', f'sm{b}'], writes=['junk2', f'sm{b}'])
            P.op('dve', lambda e, b=b: e.tensor_scalar(out=sm[b][:, 3:4], in0=sm[b][:, 2:3], scalar1=sm[b][:, 1:2], scalar2=sm[b][:, 1:2], op0=ALU.mult, op1=ALU.mult), reads=[f'sm{b}'], writes=[f'sm{b}'])
            P.op('dve', lambda e, b=b: e.tensor_scalar(out=sm[b][:, 4:5], in0=sm[b][:, 3:4], scalar1=1.0 / 256.0, scalar2=EPS, op0=ALU.mult, op1=ALU.add), reads=[f'sm{b}'], writes=[f'sm{b}'])
            P.op('act', lambda e, b=b: e.activation(out=sm[b][:, 6:7], in_=sm[b][:, 4:5], func=AF.Sqrt), reads=[f'sm{b}'], writes=[f'sm{b}'])
            P.op('dve', lambda e, b=b: e.reciprocal(out=sm[b][:, 7:8], in_=sm[b][:, 6:7]), reads=[f'sm{b}'], writes=[f'sm{b}'])
            P.op('dve', lambda e, b=b: e.tensor_tensor(out=sm[b][:, 5:6], in0=sm[b][:, 7:8], in1=sm[b][:, 1:2], op=ALU.mult), reads=[f'sm{b}'], writes=[f'sm{b}'])
            P.op('dve', lambda e, b=b, c=c: e.scalar_tensor_tensor(out=hmn[b][:, :], in0=psO[:, 0:256], scalar=sm[b][:, 5:6], in1=gso[:, c, :], op0=ALU.mult, op1=ALU.mult),
                 reads=['psO', f'sm{b}', ('gso', c)], writes=[f'hmn{b}'])
            for ec in range(2):
                P.op('pe', lambda e, b=b, ec=ec: e.transpose(out=pth[:, ec, :], in_=hmn[b][:, ec * 128:(ec + 1) * 128], identity=ident_b[:, :]),
                     reads=[f'hmn{b}', 'ident_b'], writes=['pth'])
            P.op('act', lambda e, b=b: e.activation(out=hT[b][:, :, :], in_=pth[:, :, :], func=AF.Copy), reads=['pth'], writes=[f'hT{b}'])
            P.dma('sp', lambda e, b=b, c=c: e.dma_start(out=mxi[c // 8][0:256, (c % 8) * 128:(c % 8 + 1) * 128].rearrange("(ec p) j -> p ec j", p=128), in_=hT[b][:, :, :]), f'h{b}',
                  reads=[f'hT{b}'], writes=[('mx_in', 'h', c)])
            for dc in range(2):
                P.op('pe', lambda e, b=b, c=c, dc=dc: e.matmul(psC[dc][:, :], lhsT=kw[b][:, dc * 128:(dc + 1) * 128], rhs=v_aug[:, c, :], start=True, stop=True),
                     reads=[f'kw{b}', ('v', c), 'v_aug'], writes=[f'psC{dc}'])
                P.op('dve', lambda e, c=c, dc=dc: e.scalar_tensor_tensor(out=Cst[:, dc, :], in0=Cst[:, dc, :], scalar=eA[:, c:c + 1], in1=psC[dc][:, :], op0=ALU.mult, op1=ALU.add),
                     reads=['Cst', 'eA', f'psC{dc}'], writes=['Cst'])
            P.op('act', lambda e: e.activation(out=Cb[:, :, :], in_=Cst[:, :, :], func=AF.Copy), reads=['Cst'], writes=['Cb'])
        P.barrier()
        s2.close()
        sA2.close()

        if stage == 'A2':
            hk = [('mx_in', 'h', c) for c in range(32)]
            for q_ in range(4):
                P.dma('sp', lambda e, q_=q_: e.dma_start(out=dbg[0:256, q_ * 1024:(q_ + 1) * 1024], in_=mxi[q_][0:256, :]), 'out', reads=hk, writes=['dbg'])
            sA2.close()
            sA.close()
            P.emit(top)
            return nc


        print('ops before A3', getattr(P, 'total', 0))
        s3 = ExitStack()
        colp = sb(s3, 'colp', [128, 3, 8])
        rowp = sb(s3, 'rowp', [128, 3, 1024])
        bl = sb(s3, 'bl', [128, 2, 1024])
        cml = sb(s3, 'cml', [128, 2, 1024])
        dcol = sb(s3, 'dcol', [128, 2])
        gwf = sb(s3, 'gwf', [128, 4, 128])
        gwb = sb(s3, 'gwb', [128, 4, 128], BF16)
        hpi = sb(s3, 'hpi', [128, 1])
        Bre = sb(s3, 'Bre', [128, 1024], BF16)
        Bim = sb(s3, 'Bim', [128, 1024], BF16)
        pwr = sb(s3, 'pwr', [128, 8, 12])
        pwi = sb(s3, 'pwi', [128, 8, 12])
        npwi = sb(s3, 'npwi', [128, 8, 12])
        yacc = sb(s3, 'yacc', [128, NT])
        gy = sb(s3, 'gy', [128, NT], BF16)
        sgb = [sb(s3, f'sgb{i}', [128, 512]) for i in range(2)]
        yso = [sb(s3, f'yso{i}', [128, 512], BF16) for i in range(2)]
        pB = [ps(s3, f'pB{i}', [128, 512]) for i in range(4)]
        pY = [ps(s3, f'pY{i}', [128, 512]) for i in range(2)]
        pGa = ps(s3, 'pGa', [128, 512])
        pGb = ps(s3, 'pGb', [128, 512])
        Ur = sb(s3, 'Ur', [128, 8, 64])
        Ui = sb(s3, 'Ui', [128, 8, 64])
        Vr = sb(s3, 'Vr', [128, 8, 64])
        Vi = sb(s3, 'Vi', [128, 8, 64])
        t1s = sb(s3, 't1s', [128, 8, 64])
        t2s = sb(s3, 't2s', [128, 8, 64])
        lamz = sb(s3, 'lamz', [128, 8, 4])
        cS = [sb(s3, f'cS{i}', [128, 2, 64]) for i in range(2)]
        cZ = sb(s3, 'cZ', [128, 2, 65])
        s3t = ExitStack()
        rt = [sb(s3t, f'rt{i}', [128, 1024]) for i in range(7)]
        ct = [sb(s3t, f'ct{i}', [128, 8]) for i in range(8)]

        P.dma('sp', lambda e: e.dma_start(out=colp[:, :, :], in_=s5c[:, :, :]), 'c', writes=['colp'])
        P.dma('sp', lambda e: e.dma_start(out=rowp[:, :, :], in_=s5r[:, :, :]), 'c', writes=['rowp'])
        P.dma('sp', lambda e: e.dma_start(out=bl[:, :, :], in_=s5b[:, :, :]), 'c', writes=['bl'])
        P.dma('sp', lambda e: e.dma_start(out=cml[:, :, :], in_=s5cm[:, :, :]), 'c', writes=['cml'])
        P.dma('sp', lambda e: e.dma_start(out=dcol[:, :], in_=s5d[:, :]), 'c', writes=['dcol'])
        P.dma('sp', lambda e: e.dma_start(out=gwf[:, :, :], in_=s5gw[:, :, :]), 'c', writes=['gwf'])
        P.op('dve', lambda e: e.tensor_copy(out=gwb[:, :, :], in_=gwf[:, :, :]), reads=['gwf'], writes=['gwb'])
        P.op('dve', lambda e: e.memset(hpi[:, :], float(np.pi / 2)), writes=['hpi'])

        def lam_bar(src, T, n, tag, srckey):
            dt_, lrd, th, sn, cs, t1, t2 = T[:7]
            k = [tag]
            P.op('act', lambda e: e.activation(out=dt_[:, 0:n], in_=src[:, 2, :], func=AF.Exp), reads=k + [srckey], writes=k)
            P.op('dve', lambda e: e.tensor_tensor(out=lrd[:, 0:n], in0=src[:, 0, :], in1=dt_[:, 0:n], op=ALU.mult), reads=k + [srckey], writes=k)
            P.op('dve', lambda e: e.tensor_tensor(out=th[:, 0:n], in0=src[:, 1, :], in1=dt_[:, 0:n], op=ALU.mult), reads=k + [srckey], writes=k)
            P.op('act', lambda e: e.activation(out=sn[:, 0:n], in_=th[:, 0:n], func=AF.Sin, scale=1.0 / 16.0), reads=k, writes=k)
            P.op('act', lambda e: e.activation(out=cs[:, 0:n], in_=th[:, 0:n], func=AF.Sin, scale=1.0 / 16.0, bias=hpi[:, 0:1]), reads=k + ['hpi'], writes=k)
            for _ in range(4):
                P.op('dve', lambda e: e.tensor_tensor(out=t1[:, 0:n], in0=cs[:, 0:n], in1=cs[:, 0:n], op=ALU.mult), reads=k, writes=k)
                P.op('dve', lambda e: e.tensor_tensor(out=t2[:, 0:n], in0=sn[:, 0:n], in1=sn[:, 0:n], op=ALU.mult), reads=k, writes=k)
                P.op('dve', lambda e: e.scalar_tensor_tensor(out=sn[:, 0:n], in0=sn[:, 0:n], scalar=2.0, in1=cs[:, 0:n], op0=ALU.mult, op1=ALU.mult), reads=k, writes=k)
                P.op('dve', lambda e: e.tensor_tensor(out=cs[:, 0:n], in0=t1[:, 0:n], in1=t2[:, 0:n], op=ALU.subtract), reads=k, writes=k)
            P.op('act', lambda e: e.activation(out=t1[:, 0:n], in_=lrd[:, 0:n], func=AF.Exp), reads=k, writes=k)
            P.op('dve', lambda e: e.tensor_tensor(out=cs[:, 0:n], in0=cs[:, 0:n], in1=t1[:, 0:n], op=ALU.mult), reads=k, writes=k)
            P.op('dve', lambda e: e.tensor_tensor(out=sn[:, 0:n], in0=sn[:, 0:n], in1=t1[:, 0:n], op=ALU.mult), reads=k, writes=k)
            return cs, sn

        car, cai = lam_bar(colp, ct, 8, 'c', 'colp')
        P.op('dve', lambda e: e.tensor_copy(out=pwr[:, :, 0], in_=car[:, 0:8]), reads=['c'], writes=['pw'])
        P.op('dve', lambda e: e.tensor_copy(out=pwi[:, :, 0], in_=cai[:, 0:8]), reads=['c'], writes=['pw'])
        for k_ in range(1, 12):
            P.op('dve', lambda e, k_=k_: e.tensor_tensor(out=ct[0][:, :], in0=pwr[:, :, k_ - 1], in1=pwr[:, :, k_ - 1], op=ALU.mult), reads=['pw', 'c'], writes=['c'])
            P.op('dve', lambda e, k_=k_: e.tensor_tensor(out=ct[1][:, :], in0=pwi[:, :, k_ - 1], in1=pwi[:, :, k_ - 1], op=ALU.mult), reads=['pw', 'c'], writes=['c'])
            P.op('dve', lambda e, k_=k_: e.tensor_tensor(out=pwr[:, :, k_], in0=ct[0][:, :], in1=ct[1][:, :], op=ALU.subtract), reads=['c', 'pw'], writes=['pw'])
            P.op('dve', lambda e, k_=k_: e.scalar_tensor_tensor(out=pwi[:, :, k_], in0=pwr[:, :, k_ - 1], scalar=2.0, in1=pwi[:, :, k_ - 1], op0=ALU.mult, op1=ALU.mult), reads=['pw'], writes=['pw'])
        P.op('dve', lambda e: e.tensor_scalar(out=npwi[:, :, :], in0=pwi[:, :, :], scalar1=-1.0, scalar2=None, op0=ALU.mult), reads=['pw'], writes=['npw'])

        rar, rai = lam_bar(rowp, rt, 1024, 'r', 'rowp')
        R = ['r']
        P.op('dve', lambda e: e.tensor_scalar(out=rar[:, :], in0=rar[:, :], scalar1=-1.0, scalar2=None, op0=ALU.add), reads=R, writes=R)
        P.op('dve', lambda e: e.tensor_tensor(out=rt[0][:, :], in0=rowp[:, 0, :], in1=rowp[:, 0, :], op=ALU.mult), reads=R + ['rowp'], writes=R)
        P.op('dve', lambda e: e.tensor_tensor(out=rt[1][:, :], in0=rowp[:, 1, :], in1=rowp[:, 1, :], op=ALU.mult), reads=R + ['rowp'], writes=R)
        P.op('dve', lambda e: e.tensor_tensor(out=rt[0][:, :], in0=rt[0][:, :], in1=rt[1][:, :], op=ALU.add), reads=R, writes=R)
        P.op('dve', lambda e: e.reciprocal(out=rt[0][:, :], in_=rt[0][:, :]), reads=R, writes=R)
        P.op('dve', lambda e: e.tensor_tensor(out=rt[1][:, :], in0=rar[:, :], in1=rowp[:, 0, :], op=ALU.mult), reads=R + ['rowp'], writes=R)
        P.op('dve', lambda e: e.tensor_tensor(out=rt[2][:, :], in0=rai[:, :], in1=rowp[:, 1, :], op=ALU.mult), reads=R + ['rowp'], writes=R)
        P.op('dve', lambda e: e.tensor_tensor(out=rt[1][:, :], in0=rt[1][:, :], in1=rt[2][:, :], op=ALU.add), reads=R, writes=R)
        P.op('dve', lambda e: e.tensor_tensor(out=rt[1][:, :], in0=rt[1][:, :], in1=rt[0][:, :], op=ALU.mult), reads=R, writes=R)
        P.op('dve', lambda e: e.tensor_tensor(out=rt[2][:, :], in0=rai[:, :], in1=rowp[:, 0, :], op=ALU.mult), reads=R + ['rowp'], writes=R)
        P.op('dve', lambda e: e.tensor_tensor(out=rt[5][:, :], in0=rar[:, :], in1=rowp[:, 1, :], op=ALU.mult), reads=R + ['rowp'], writes=R)
        P.op('dve', lambda e: e.tensor_tensor(out=rt[2][:, :], in0=rt[2][:, :], in1=rt[5][:, :], op=ALU.subtract), reads=R, writes=R)
        P.op('dve', lambda e: e.tensor_tensor(out=rt[2][:, :], in0=rt[2][:, :], in1=rt[0][:, :], op=ALU.mult), reads=R, writes=R)
        P.op('dve', lambda e: e.tensor_tensor(out=rt[5][:, :], in0=rt[1][:, :], in1=bl[:, 0, :], op=ALU.mult), reads=R + ['bl'], writes=R)
        P.op('dve', lambda e: e.tensor_tensor(out=rt[6][:, :], in0=rt[2][:, :], in1=bl[:, 1, :], op=ALU.mult), reads=R + ['bl'], writes=R)
        P.op('dve', lambda e: e.tensor_tensor(out=Bre[:, :], in0=rt[5][:, :], in1=rt[6][:, :], op=ALU.subtract), reads=R, writes=['Bre'])
        P.op('dve', lambda e: e.tensor_tensor(out=rt[5][:, :], in0=rt[1][:, :], in1=bl[:, 1, :], op=ALU.mult), reads=R + ['bl', 'Bre'], writes=R)
        P.op('dve', lambda e: e.tensor_tensor(out=rt[6][:, :], in0=rt[2][:, :], in1=bl[:, 0, :], op=ALU.mult), reads=R + ['bl'], writes=R)
        P.op('dve', lambda e: e.tensor_tensor(out=Bim[:, :], in0=rt[5][:, :], in1=rt[6][:, :], op=ALU.add), reads=R, writes=['Bim'])
        P.op('dve', lambda e: e.tensor_scalar(out=cml[:, 1, :], in0=cml[:, 1, :], scalar1=-1.0, scalar2=None, op0=ALU.mult), reads=['cml'], writes=['cml'])

        P.op('dve', lambda e: e.memset(Ur[:, :, 0:1], 1.0), writes=['U'])
        P.op('dve', lambda e: e.memset(Ui[:, :, 0:1], 0.0), reads=['U'], writes=['U'])
        for k_ in range(6):
            s_ = 1 << k_
            Lr = lambda s_=s_, k_=k_: pwr[:, :, k_:k_ + 1].to_broadcast([128, 8, s_])
            Li = lambda s_=s_, k_=k_: pwi[:, :, k_:k_ + 1].to_broadcast([128, 8, s_])
            P.op('dve', lambda e, s_=s_, Lr=Lr: e.tensor_tensor(out=t1s[:, :, 0:s_], in0=Ur[:, :, 0:s_], in1=Lr(), op=ALU.mult), reads=['U', 'pw'], writes=['t1s'])
            P.op('dve', lambda e, s_=s_, Li=Li: e.tensor_tensor(out=t2s[:, :, 0:s_], in0=Ui[:, :, 0:s_], in1=Li(), op=ALU.mult), reads=['U', 'pw'], writes=['t2s'])
            P.op('dve', lambda e, s_=s_: e.tensor_tensor(out=Ur[:, :, s_:2 * s_], in0=t1s[:, :, 0:s_], in1=t2s[:, :, 0:s_], op=ALU.subtract), reads=['t1s', 't2s', 'U'], writes=['U2'])
            P.op('dve', lambda e, s_=s_, Li=Li: e.tensor_tensor(out=t1s[:, :, 0:s_], in0=Ur[:, :, 0:s_], in1=Li(), op=ALU.mult), reads=['U', 'U2', 'pw'], writes=['t1s'])
            P.op('dve', lambda e, s_=s_, Lr=Lr: e.tensor_tensor(out=t2s[:, :, 0:s_], in0=Ui[:, :, 0:s_], in1=Lr(), op=ALU.mult), reads=['U', 'U2', 'pw'], writes=['t2s'])
            P.op('dve', lambda e, s_=s_: e.tensor_tensor(out=Ui[:, :, s_:2 * s_], in0=t1s[:, :, 0:s_], in1=t2s[:, :, 0:s_], op=ALU.add), reads=['t1s', 't2s', 'U2'], writes=['U'])
        P.op('dve', lambda e: e.tensor_tensor(out=t1s[:, :, :], in0=Ur[:, :, :], in1=Ur[:, :, :], op=ALU.mult), reads=['U'], writes=['t1s'])
        P.op('dve', lambda e: e.tensor_tensor(out=t2s[:, :, :], in0=Ui[:, :, :], in1=Ui[:, :, :], op=ALU.mult), reads=['U'], writes=['t2s'])
        P.op('dve', lambda e: e.tensor_tensor(out=t1s[:, :, :], in0=t1s[:, :, :], in1=t2s[:, :, :], op=ALU.add), reads=['t1s', 't2s'], writes=['t1s'])
        P.op('dve', lambda e: e.reciprocal(out=t1s[:, :, :], in_=t1s[:, :, :]), reads=['t1s'], writes=['t1s'])
        P.op('dve', lambda e: e.tensor_tensor(out=Vr[:, :, :], in0=Ur[:, :, :], in1=t1s[:, :, :], op=ALU.mult), reads=['U', 't1s'], writes=['V'])
        P.op('dve', lambda e: e.scalar_tensor_tensor(out=Vi[:, :, :], in0=Ui[:, :, :], scalar=-1.0, in1=t1s[:, :, :], op0=ALU.mult, op1=ALU.mult), reads=['U', 't1s', 'V'], writes=['V'])
        P.op('dve', lambda e: e.tensor_copy(out=lamz[:, :, 0:1], in_=Ur[:, :, 63:64]), reads=['U'], writes=['lamz'])
        P.op('dve', lambda e: e.tensor_copy(out=lamz[:, :, 1:2], in_=Ui[:, :, 63:64]), reads=['U', 'lamz'], writes=['lamz'])
        P.op('dve', lambda e: e.tensor_scalar(out=lamz[:, :, 2:3], in0=Ui[:, :, 63:64], scalar1=-1.0, scalar2=None, op0=ALU.mult), reads=['U', 'lamz'], writes=['lamz'])
        P.barrier()
        s3t.close()
        BT = [sb(s3, f'BT{i}', [128, NT]) for i in range(5)]
        msk = sb(s3, 'msk', [128, NT])
        P.op('pool', lambda e: e.memset(msk[:, :], 1.0), writes=['msk'])
        P.op('pool', lambda e: e.memset(msk[:, 0:NT:64], 0.0), reads=['msk'], writes=['msk'])
        P.op('dve', lambda e: e.memset(cZ[:, :, 0:1], 0.0), writes=['cZ0'])

        def v3(t):
            return t[:, :].rearrange("p (c t) -> p c t", t=64)
        for q in range(8):
            cc = q // 4
            ukeys = [('uT', cc, tb) for tb in range(8)]
            for nb in range(8):
                ns = slice(nb * 512, (nb + 1) * 512)
                pr, pi_ = pB[(nb % 2) * 2], pB[(nb % 2) * 2 + 1]
                P.op('pe', lambda e, q=q, cc=cc, ns=ns, pr=pr: e.matmul(pr[:, :], lhsT=Bre[:, q * 128:(q + 1) * 128], rhs=uT[:, cc, ns], start=True, stop=True),
                     reads=ukeys + ['Bre'], writes=[('pB', (nb % 2) * 2)])
                P.op('pe', lambda e, q=q, cc=cc, ns=ns, pi_=pi_: e.matmul(pi_[:, :], lhsT=Bim[:, q * 128:(q + 1) * 128], rhs=uT[:, cc, ns], start=True, stop=True),
                     reads=ukeys + ['Bim'], writes=[('pB', (nb % 2) * 2 + 1)])
                P.op('act', lambda e, ns=ns, pr=pr: e.activation(out=BT[0][:, ns], in_=pr[:, :], func=AF.Copy), reads=[('pB', (nb % 2) * 2)], writes=['BT0'])
                P.op('dve', lambda e, ns=ns, pi_=pi_: e.tensor_copy(out=BT[1][:, ns], in_=pi_[:, :]), reads=[('pB', (nb % 2) * 2 + 1)], writes=['BT1'])
            A_, B_, C_, D_, E_ = BT
            F_ = D_
            tb_ = lambda T, q=q: T[:, q, None, :].to_broadcast([128, 64, 64])
            P.op('pool', lambda e, tb_=tb_: e.tensor_tensor(out=v3(C_), in0=v3(A_), in1=tb_(Vr), op=ALU.mult), reads=['BT0', 'V'], writes=['BT2'])
            P.op('pool', lambda e, tb_=tb_: e.tensor_tensor(out=v3(D_), in0=v3(B_), in1=tb_(Vi), op=ALU.mult), reads=['BT1', 'V'], writes=['BT3'])
            P.op('dve', lambda e: e.tensor_tensor(out=C_[:, :], in0=C_[:, :], in1=D_[:, :], op=ALU.subtract), reads=['BT2', 'BT3'], writes=['BT2'])
            P.op('pool', lambda e, tb_=tb_: e.tensor_tensor(out=v3(D_), in0=v3(A_), in1=tb_(Vi), op=ALU.mult), reads=['BT0', 'V', 'BT2'], writes=['BT3'])
            P.op('pool', lambda e, tb_=tb_: e.tensor_tensor(out=v3(E_), in0=v3(B_), in1=tb_(Vr), op=ALU.mult), reads=['BT1', 'V'], writes=['BT4'])
            P.op('dve', lambda e: e.tensor_tensor(out=D_[:, :], in0=D_[:, :], in1=E_[:, :], op=ALU.add), reads=['BT3', 'BT4'], writes=['BT3'])
            P.op('dve', lambda e: e.tensor_tensor_scan(out=A_[:, :], data0=msk[:, :], data1=C_[:, :], initial=0.0, op0=ALU.mult, op1=ALU.add), reads=['msk', 'BT2'], writes=['BT0'])
            P.op('dve', lambda e: e.tensor_tensor_scan(out=B_[:, :], data0=msk[:, :], data1=D_[:, :], initial=0.0, op0=ALU.mult, op1=ALU.add), reads=['msk', 'BT3'], writes=['BT1'])
            P.op('dve', lambda e, q=q: e.tensor_scalar(out=cS[0][:, 0, :], in0=v3(A_)[:, :, 63], scalar1=lamz[:, q, 0:1], scalar2=None, op0=ALU.mult), reads=['BT0', 'lamz'], writes=['cS0'])
            P.op('dve', lambda e, q=q: e.scalar_tensor_tensor(out=cS[0][:, 0, :], in0=v3(B_)[:, :, 63], scalar=lamz[:, q, 2:3], in1=cS[0][:, 0, :], op0=ALU.mult, op1=ALU.add), reads=['BT1', 'lamz', 'cS0'], writes=['cS0'])
            P.op('dve', lambda e, q=q: e.tensor_scalar(out=cS[0][:, 1, :], in0=v3(B_)[:, :, 63], scalar1=lamz[:, q, 0:1], scalar2=None, op0=ALU.mult), reads=['BT1', 'lamz', 'cS0'], writes=['cS0'])
            P.op('dve', lambda e, q=q: e.scalar_tensor_tensor(out=cS[0][:, 1, :], in0=v3(A_)[:, :, 63], scalar=lamz[:, q, 1:2], in1=cS[0][:, 1, :], op0=ALU.mult, op1=ALU.add), reads=['BT0', 'lamz', 'cS0'], writes=['cS0'])
            cur = 0
            for j_ in range(6):
                sft = 1 << j_
                k_ = 6 + j_
                nxt = 1 - cur
                s_, d_ = cS[cur], cS[nxt]
                ks, kd = f'cS{cur}', f'cS{nxt}'
                P.op('dve', lambda e, s_=s_, d_=d_, sft=sft, q=q, k_=k_: e.scalar_tensor_tensor(out=d_[:, 0, sft:], in0=s_[:, 0, 0:64 - sft], scalar=pwr[:, q, k_:k_ + 1], in1=s_[:, 0, sft:], op0=ALU.mult, op1=ALU.add), reads=[ks, 'pw'], writes=[kd])
                P.op('dve', lambda e, s_=s_, d_=d_, sft=sft, q=q, k_=k_: e.scalar_tensor_tensor(out=d_[:, 0, sft:], in0=s_[:, 1, 0:64 - sft], scalar=npwi[:, q, k_:k_ + 1], in1=d_[:, 0, sft:], op0=ALU.mult, op1=ALU.add), reads=[ks, 'npw', kd], writes=[kd])
                P.op('dve', lambda e, s_=s_, d_=d_, sft=sft, q=q, k_=k_: e.scalar_tensor_tensor(out=d_[:, 1, sft:], in0=s_[:, 1, 0:64 - sft], scalar=pwr[:, q, k_:k_ + 1], in1=s_[:, 1, sft:], op0=ALU.mult, op1=ALU.add), reads=[ks, 'pw', kd], writes=[kd])
                P.op('dve', lambda e, s_=s_, d_=d_, sft=sft, q=q, k_=k_: e.scalar_tensor_tensor(out=d_[:, 1, sft:], in0=s_[:, 0, 0:64 - sft], scalar=pwi[:, q, k_:k_ + 1], in1=d_[:, 1, sft:], op0=ALU.mult, op1=ALU.add), reads=[ks, 'pw', kd], writes=[kd])
                P.op('dve', lambda e, s_=s_, d_=d_, sft=sft: e.tensor_copy(out=d_[:, :, 0:sft], in_=s_[:, :, 0:sft]), reads=[ks, kd], writes=[kd])
                cur = nxt
            Xc = cS[cur]
            kx = f'cS{cur}'
            P.op('dve', lambda e, q=q, Xc=Xc: e.tensor_scalar(out=cZ[:, 0, 1:65], in0=Xc[:, 0, :], scalar1=pwr[:, q, 0:1], scalar2=None, op0=ALU.mult), reads=[kx, 'pw', 'cZ0'], writes=['cZ'])
            P.op('dve', lambda e, q=q, Xc=Xc: e.scalar_tensor_tensor(out=cZ[:, 0, 1:65], in0=Xc[:, 1, :], scalar=npwi[:, q, 0:1], in1=cZ[:, 0, 1:65], op0=ALU.mult, op1=ALU.add), reads=[kx, 'npw', 'cZ'], writes=['cZ'])
            P.op('dve', lambda e, q=q, Xc=Xc: e.tensor_scalar(out=cZ[:, 1, 1:65], in0=Xc[:, 1, :], scalar1=pwr[:, q, 0:1], scalar2=None, op0=ALU.mult), reads=[kx, 'pw', 'cZ'], writes=['cZ'])
            P.op('dve', lambda e, q=q, Xc=Xc: e.scalar_tensor_tensor(out=cZ[:, 1, 1:65], in0=Xc[:, 0, :], scalar=pwi[:, q, 0:1], in1=cZ[:, 1, 1:65], op0=ALU.mult, op1=ALU.add), reads=[kx, 'pw', 'cZ'], writes=['cZ'])
            P.op('dve', lambda e, tb_=tb_: e.tensor_tensor(out=v3(A_), in0=v3(A_), in1=cZ[:, 0, 0:64, None].to_broadcast([128, 64, 64]), op=ALU.add), reads=['BT0', 'cZ'], writes=['BT0'])
            P.op('dve', lambda e, tb_=tb_: e.tensor_tensor(out=v3(B_), in0=v3(B_), in1=cZ[:, 1, 0:64, None].to_broadcast([128, 64, 64]), op=ALU.add), reads=['BT1', 'cZ'], writes=['BT1'])
            P.op('pool', lambda e, tb_=tb_: e.tensor_tensor(out=v3(C_), in0=v3(A_), in1=tb_(Ur), op=ALU.mult), reads=['BT0', 'U'], writes=['BT2'])
            P.op('pool', lambda e, tb_=tb_: e.tensor_tensor(out=v3(D_), in0=v3(B_), in1=tb_(Ui), op=ALU.mult), reads=['BT1', 'U'], writes=['BT3'])
            P.op('dve', lambda e: e.tensor_tensor(out=C_[:, :], in0=C_[:, :], in1=D_[:, :], op=ALU.subtract), reads=['BT2', 'BT3'], writes=['BT2'])
            P.op('dve', lambda e, tb_=tb_: e.tensor_tensor(out=v3(E_), in0=v3(A_), in1=tb_(Ui), op=ALU.mult), reads=['BT0', 'U'], writes=['BT4'])
            P.op('pool', lambda e, tb_=tb_: e.tensor_tensor(out=v3(F_), in0=v3(B_), in1=tb_(Ur), op=ALU.mult), reads=['BT1', 'U'], writes=['BT3'])
            P.op('dve', lambda e: e.tensor_tensor(out=E_[:, :], in0=E_[:, :], in1=F_[:, :], op=ALU.add), reads=['BT4', 'BT3'], writes=['BT4'])
            fr_, fi_ = C_, E_
            for nb in range(8):
                ns = slice(nb * 512, (nb + 1) * 512)
                py = pY[nb % 2]
                P.op('pe', lambda e, q=q, ns=ns, py=py, fr_=fr_: e.matmul(py[:, :], lhsT=cml[:, 0, q * 128:(q + 1) * 128], rhs=fr_[:, ns], start=True, stop=False),
                     reads=['cml', 'BT2'], writes=[('pY', nb % 2)])
                P.op('pe', lambda e, q=q, ns=ns, py=py, fi_=fi_: e.matmul(py[:, :], lhsT=cml[:, 1, q * 128:(q + 1) * 128], rhs=fi_[:, ns], start=False, stop=True),
                     reads=['cml', 'BT4'], writes=[('pY', nb % 2)])
                if q % 4 == 0:
                    P.op('dve', lambda e, cc=cc, ns=ns, py=py: e.scalar_tensor_tensor(out=yacc[:, ns], in0=uT[:, cc, ns], scalar=dcol[:, cc:cc + 1], in1=py[:, :], op0=ALU.mult, op1=ALU.add),
                         reads=ukeys + ['dcol', ('pY', nb % 2)], writes=[('yacc', nb)])
                else:
                    P.op('dve', lambda e, ns=ns, py=py: e.tensor_tensor(out=yacc[:, ns], in0=yacc[:, ns], in1=py[:, :], op=ALU.add),
                         reads=[('yacc', nb), ('pY', nb % 2)], writes=[('yacc', nb)])
            if q % 4 == 3:
                for nb in range(8):
                    ns = slice(nb * 512, (nb + 1) * 512)
                    b = nb % 2
                    P.op('act', lambda e, ns=ns: e.activation(out=gy[:, ns], in_=yacc[:, ns], func=AF.Gelu), reads=[('yacc', nb)], writes=[('gy', nb)])
                    P.op('pe', lambda e, cc=cc, ns=ns: e.matmul(pGa[:, :], lhsT=gwb[:, cc * 2, :], rhs=gy[:, ns], start=True, stop=True), reads=['gwb', ('gy', nb)], writes=['pGa'])
                    P.op('pe', lambda e, cc=cc, ns=ns: e.matmul(pGb[:, :], lhsT=gwb[:, cc * 2 + 1, :], rhs=gy[:, ns], start=True, stop=True), reads=['gwb', ('gy', nb)], writes=['pGb'])
                    P.op('act', lambda e, b=b: e.activation(out=sgb[b][:, :], in_=pGb[:, :], func=AF.Sigmoid), reads=['pGb'], writes=[f'sgb{b}'])
                    P.op('dve', lambda e, b=b: e.tensor_tensor(out=yso[b][:, :], in0=pGa[:, :], in1=sgb[b][:, :], op=ALU.mult), reads=['pGa', f'sgb{b}'], writes=[f'yso{b}'])
                    P.dma('sp', lambda e, cc=cc, nb=nb, b=b: e.dma_start(out=mxi[nb // 2][256 + cc * 128:256 + (cc + 1) * 128, (nb % 2) * 512:(nb % 2 + 1) * 512], in_=yso[b][:, :]), f'ys{b}',
                          reads=[f'yso{b}'], writes=[('mx_in', 'y', cc, nb)])
        P.barrier()
        s3.close()
        if stage == 'A3':
            for q_ in range(4):
                P.dma('sp', lambda e, q_=q_: e.dma_start(out=dbg[:, q_ * 1024:(q_ + 1) * 1024], in_=mxi[q_][:, :]), 'out', writes=['dbg'], force=True)
            sA2.close()
            sA.close()
            P.emit(top)
            return nc

        sA.close()
        print('ops before exchange', getattr(P, 'total', 0))
        NEI = int(os.environ.get('KNE', '128'))
        if stage == 'simB':
            mxsrc = [mx_test[:, q_ * 1024:(q_ + 1) * 1024] for q_ in range(4)]
        else:
            allk = [('mx_in', 'h', c) for c in range(32)] + [('mx_in', 'y', cc, nb) for cc in range(2) for nb in range(8)]
            for q_ in range(4):
                P.dma('pool', lambda e, q_=q_: e.collective_compute("AllGather", ALU.bypass, replica_groups=[[0, 1, 2, 3], [4, 5, 6, 7]],
                                                                   ins=[mxi[q_].ap().opt()], outs=[mxa[q_].ap().opt()]), f'cc{q_}', reads=allk, writes=[('mx_all', q_)], inc=1)
            mxsrc = [mxa[q_].ap() for q_ in range(4)]
        sB = ExitStack()
        h1 = sb(sB, 'h1', [128, 8, DM])
        xn2T = sb(sB, 'xn2T', [128, KC, 1024], BF16)
        em = sb(sB, 'em', [128, 8, 16, 128], BF16)
        sc = sb(sB, 'sc', [128, 8, 8, 2])
        selt = sb(sB, 'selt', [128, 4])
        g2t = sb(sB, 'g2t', [128, KC])
        ssq2 = [sb(sB, f'ssq2{i}', [128, 1]) for i in range(2)]
        rstd2 = [sb(sB, f'rstd2{i}', [128, 1]) for i in range(2)]
        P.dma('sp', lambda e: e.dma_start(out=selt[:, :], in_=selin[:, :]), 'c', writes=['selt'])
        P.dma('sp', lambda e: e.dma_start(out=g2t[:, :], in_=g2c[:, :]), 'c', writes=['g2t'])
        P.dma('sp', lambda e: e.dma_start(out=h1[:, :, :], in_=x_own.rearrange("(tt p) d -> p tt d", p=128)), 'c', writes=['h1'])

        print('ops before B1', getattr(P, 'total', 0))
        sb1 = ExitStack()
        mixo = sb(sb1, 'mixo', [128, KC, 1024], BF16)
        wb = sb(sb1, 'wb', [128, KC, 512], BF16)
        mst = [sb(sb1, f'mst{i}', [128, 1024], BF16) for i in range(2)]
        wst2 = [sb(sb1, f'wst2{i}', [128, 512]) for i in range(2)]
        xs2 = [sb(sb1, f'xs2{i}', [128, DM], BF16) for i in range(2)]
        pM = [ps(sb1, f'pM{i}', [128, 512]) for i in range(2)]
        ptr2 = [ps(sb1, f'ptr2{i}', [128, 8, 128], BF16) for i in range(2)]

        for kc in range(KC):
            for q_ in range(4):
                b = (kc * 4 + q_) % 2
                P.dma('sp', lambda e, kc=kc, q_=q_, b=b: e.dma_start(out=mst[b][:, :], in_=mxsrc[q_][kc * 128:(kc + 1) * 128, :]), f'ms{b}',
                      reads=[('mx_all', q_)], writes=[f'mst{b}'])
                if q_ == 0:
                    P.op('dve', lambda e, kc=kc, b=b: e.tensor_scalar(out=mixo[:, kc, :], in0=mst[b][:, :], scalar1=selt[:, 0:1], scalar2=None, op0=ALU.mult),
                         reads=[f'mst{b}', 'selt'], writes=['mixo'])
                else:
                    P.op('dve', lambda e, kc=kc, b=b, q_=q_: e.scalar_tensor_tensor(out=mixo[:, kc, :], in0=mst[b][:, :], scalar=selt[:, q_:q_ + 1], in1=mixo[:, kc, :], op0=ALU.mult, op1=ALU.add),
                         reads=[f'mst{b}', 'selt', 'mixo'], writes=['mixo'])
        for nblk in range(4):
            for kc in range(KC):
                b = kc % 2
                P.dma('sp', lambda e, kc=kc, b=b, nblk=nblk: e.dma_start(out=wst2[b][:, :], in_=w_o[:, kc, nblk * 512:(nblk + 1) * 512]), f'wo{b}', writes=[f'wst2{b}'])
                P.op('act', lambda e, kc=kc, b=b: e.activation(out=wb[:, kc, :], in_=wst2[b][:, :], func=AF.Copy), reads=[f'wst2{b}'], writes=['wb'])
            for tt in range(8):
                pb_ = tt % 2
                for kc in range(KC):
                    P.op('pe', lambda e, kc=kc, tt=tt, pb_=pb_: e.matmul(pM[pb_][:, :], lhsT=mixo[:, kc, tt * 128:(tt + 1) * 128], rhs=wb[:, kc, :], start=(kc == 0), stop=(kc == KC - 1)),
                         reads=['mixo', 'wb'], writes=[f'pM{pb_}'])
                P.op('dve', lambda e, tt=tt, pb_=pb_, nblk=nblk: e.tensor_tensor(out=h1[:, tt, nblk * 512:(nblk + 1) * 512], in0=h1[:, tt, nblk * 512:(nblk + 1) * 512], in1=pM[pb_][:, :], op=ALU.add),
                     reads=[f'pM{pb_}', 'h1'], writes=['h1'])

        def rms(tt, b, jt, jk):
            P.op('act', lambda e: e.activation(out=jt[:, :], in_=h1[:, tt, :], func=AF.Square, accum_out=ssq2[b][:, :]), reads=['h1'], writes=[jk, f'ssq2{b}'])
            P.op('dve', lambda e: e.tensor_scalar(out=rstd2[b][:, :], in0=ssq2[b][:, :], scalar1=1.0 / DM, scalar2=EPS, op0=ALU.mult, op1=ALU.add), reads=[f'ssq2{b}'], writes=[f'rstd2{b}'])
            P.op('act', lambda e: e.activation(out=rstd2[b][:, :], in_=rstd2[b][:, :], func=AF.Sqrt), reads=[f'rstd2{b}'], writes=[f'rstd2{b}'])
            P.op('dve', lambda e: e.reciprocal(out=rstd2[b][:, :], in_=rstd2[b][:, :]), reads=[f'rstd2{b}'], writes=[f'rstd2{b}'])
        for tt in range(8):
            b = tt % 2
            rms(tt, b, xs2[b], f'xs2{b}')
            P.op('dve', lambda e, tt=tt, b=b: e.tensor_scalar(out=xs2[b][:, :], in0=h1[:, tt, :], scalar1=rstd2[b][:, 0:1], scalar2=None, op0=ALU.mult), reads=['h1', f'rstd2{b}'], writes=[f'xs2{b}'])
            for half in range(2):
                for j in range(8):
                    kc = half * 8 + j
                    P.op('pe', lambda e, b=b, kc=kc, half=half, j=j: e.transpose(out=ptr2[half][:, j, :], in_=xs2[b][:, kc * 128:(kc + 1) * 128], identity=ident_b[:, :]),
                         reads=[f'xs2{b}', 'ident_b'], writes=[f'ptr2{half}'])
                P.op('dve', lambda e, half=half, tt=tt: e.tensor_tensor(out=xn2T[:, half * 8:half * 8 + 8, tt * 128:(tt + 1) * 128], in0=ptr2[half][:, :, :],
                                                                         in1=g2t[:, half * 8:half * 8 + 8, None].to_broadcast([128, 8, 128]), op=ALU.mult),
                     reads=[f'ptr2{half}', 'g2t'], writes=['xn2T'])
        P.barrier()
        sb1.close()

        print('ops before B2', getattr(P, 'total', 0))
        sb2 = ExitStack()
        v16 = sb(sb2, 'v16', [128, 8, 16, 16])
        skf = sb(sb2, 'skf', [128, 2, 128])
        skb = sb(sb2, 'skb', [128, 2, 128], BF16)
        wqst = [sb(sb2, f'wqst{i}', [128, KC, 128]) for i in range(2)]
        wqb = [sb(sb2, f'wqb{i}', [128, KC, 128], BF16) for i in range(2)]
        qj = [sb(sb2, f'qj{i}', [128, 1024], BF16) for i in range(2)]
        ebf = [sb(sb2, f'ebf{i}', [128, 128], BF16) for i in range(2)]
        ef = [sb(sb2, f'ef{i}', [128, 128]) for i in range(2)]
        ef2 = [sb(sb2, f'ef2{i}', [128, 128]) for i in range(2)]
        cand = [sb(sb2, f'cand{i}', [128, 256]) for i in range(2)]
        cand2 = [sb(sb2, f'cand2{i}', [128, 256]) for i in range(2)]
        c16 = [sb(sb2, f'c16{i}', [128, 16]) for i in range(2)]
        pQ = [ps(sb2, f'pQ{i}', [128, 512]) for i in range(2)]
        pS = [ps(sb2, f'pS{i}', [128, 128]) for i in range(2)]
        P.dma('sp', lambda e: e.dma_start(out=skf[:, :, :], in_=skT[:, :, :]), 'c', writes=['skf'])
        P.op('dve', lambda e: e.tensor_copy(out=skb[:, :, :], in_=skf[:, :, :]), reads=['skf'], writes=['skb'])
        for j in range(16):
            b = j % 2
            half = j % 2
            P.dma('sp', lambda e, j=j, b=b: e.dma_start(out=wqst[b][:, :, :], in_=w_q[:, :, j * 128:(j + 1) * 128]), f'wq{b}', writes=[f'wqst{b}'])
            P.op('dve', lambda e, b=b: e.tensor_copy(out=wqb[b][:, :, :], in_=wqst[b][:, :, :]),
                 reads=[f'wqst{b}'], writes=[f'wqb{b}'])
            for th in range(2):
                for kc in range(KC):
                    P.op('pe', lambda e, b=b, kc=kc, th=th: e.matmul(pQ[th][:, :], lhsT=wqb[b][:, kc, :], rhs=xn2T[:, kc, th * 512:(th + 1) * 512], start=(kc == 0), stop=(kc == KC - 1)),
                         reads=[f'wqb{b}', 'xn2T'], writes=[f'pQ{th}'])
                P.op('act', lambda e, b=b, th=th: e.activation(out=qj[b][:, th * 512:(th + 1) * 512], in_=pQ[th][:, :], func=AF.Copy), reads=[f'pQ{th}'], writes=[f'qj{b}'])
            for tp_ in range(4):
                tts = (2 * tp_, 2 * tp_ + 1)
                for tt in tts:
                    b2 = tt % 2
                    P.op('pe', lambda e, b=b, tt=tt, b2=b2, half=half: e.matmul(pS[b2][:, :], lhsT=qj[b][:, tt * 128:(tt + 1) * 128], rhs=skb[:, half, :], start=True, stop=True),
                         reads=[f'qj{b}', 'skb'], writes=[f'pS{b2}'])
                for tt in tts:
                    b2 = tt % 2
                    P.op('act', lambda e, b2=b2: e.activation(out=ebf[b2][:, :], in_=pS[b2][:, :], func=AF.Exp), reads=[f'pS{b2}'], writes=[f'ebf{b2}'])
                for tt in tts:
                    b2 = tt % 2
                    P.op('dve', lambda e, b2=b2: e.tensor_copy(out=ef[b2][:, :], in_=ebf[b2][:, :]), reads=[f'ebf{b2}'], writes=[f'ef{b2}'])
                for tt in tts:
                    b2 = tt % 2
                    P.op('dve', lambda e, b2=b2, tt=tt, j=j: e.max(out=v16[:, tt, j, 0:8], in_=ef[b2][:, :]), reads=[f'ef{b2}'], writes=[('v16', tt, j)])
                for tt in tts:
                    b2 = tt % 2
                    P.op('dve', lambda e, b2=b2, tt=tt, j=j: e.match_replace(out=ef2[b2][:, :], in_to_replace=v16[:, tt, j, 0:8], in_values=ef[b2][:, :], imm_value=-1.0), reads=[f'ef{b2}', ('v16', tt, j)], writes=[f'ef2{b2}'])
                for tt in tts:
                    b2 = tt % 2
                    P.op('dve', lambda e, b2=b2, tt=tt, j=j: e.max(out=v16[:, tt, j, 8:16], in_=ef2[b2][:, :]), reads=[f'ef2{b2}'], writes=[('v16', tt, j)])
                for tt in tts:
                    b2 = tt % 2
                    P.op('dve', lambda e, b2=b2, tt=tt, j=j: e.scalar_tensor_tensor(out=em[:, tt, j, :], in0=ef[b2][:, :], scalar=v16[:, tt, j, 15:16], in1=ef[b2][:, :], op0=ALU.is_ge, op1=ALU.mult),
                         reads=[f'ef{b2}', ('v16', tt, j)], writes=[('em', tt, j)])
        P.barrier()
        for tt in range(8):
            for hh in range(8):
                b = hh % 2
                P.op('dve', lambda e, tt=tt, hh=hh, b=b: e.tensor_tensor(out=cand[b][:, :].rearrange("p (a c) -> p a c", a=16), in0=v16[:, tt, 2 * hh, :, None].to_broadcast([128, 16, 16]),
                                                                          in1=v16[:, tt, 2 * hh + 1, None, :].to_broadcast([128, 16, 16]), op=ALU.mult), reads=['v16'], writes=[f'cand{b}'])
                P.op('dve', lambda e, b=b: e.max(out=c16[b][:, 0:8], in_=cand[b][:, :]), reads=[f'cand{b}'], writes=[f'c16{b}'])
                P.op('dve', lambda e, b=b: e.match_replace(out=cand2[b][:, :], in_to_replace=c16[b][:, 0:8], in_values=cand[b][:, :], imm_value=-1.0), reads=[f'cand{b}', f'c16{b}'], writes=[f'cand2{b}'])
                P.op('dve', lambda e, b=b: e.max(out=c16[b][:, 8:16], in_=cand2[b][:, :]), reads=[f'cand2{b}'], writes=[f'c16{b}'])
                P.op('dve', lambda e, b=b, tt=tt, hh=hh: e.tensor_scalar(out=sc[:, tt, hh, 0:1], in0=c16[b][:, 15:16], scalar1=0.999996, scalar2=None, op0=ALU.mult), reads=[f'c16{b}'], writes=['sc'])
                P.op('dve', lambda e, b=b, tt=tt, hh=hh: e.reduce_sum(out=sc[:, tt, hh, 1:2], in_=c16[b][:, :], axis=AX.X), reads=[f'c16{b}'], writes=['sc'])
        P.op('dve', lambda e: e.reciprocal(out=sc[:, :, :, 1], in_=sc[:, :, :, 1]), reads=['sc'], writes=['sc'])
        P.barrier()
        sb2.close()

        print('ops before B3', getattr(P, 'total', 0))
        EB = 4
        sb3 = ExitStack()
        Ust = [sb(sb3, f'Ust{i}', [128, 1024]) for i in range(2)]
        Vst = [sb(sb3, f'Vst{i}', [128, 1024]) for i in range(2)]
        Ub = sb(sb3, 'Ub', [128, DM], BF16)
        Vb = [sb(sb3, f'Vb{i}', [128, DM], BF16) for i in range(EB)]
        UT = sb(sb3, 'UT', [128, KC, 128], BF16)
        gelT = [sb(sb3, f'gelT{i}', [128, 1024], BF16) for i in range(2)]
        thw = [sb(sb3, f'thw{i}', [128, 3, 8, 8]) for i in range(2)]
        Yb = [sb(sb3, f'Yb{i}', [128, 8, 128], BF16) for i in range(3)]
        Dg = [sb(sb3, f'Dg{i}', [128, 8, 128], BF16) for i in range(3)]
        Mk = [sb(sb3, f'Mk{i}', [128, 8, 128], BF16) for i in range(3)]
        GAT = sb(sb3, 'GAT', [128, EB, 1024], BF16)
        ptr3 = ps(sb3, 'ptr3', [128, 8, 128], BF16)
        pA2 = [ps(sb3, f'pA2{i}', [128, 512]) for i in range(2)]
        pG = [ps(sb3, f'pG{i}', [128, 128]) for i in range(3)]
        pO = [ps(sb3, f'pO{i}', [128, 512]) for i in range(2)]
        LAG = 2
        pend = []

        def flush_one():
            il_, ib_, tt_, b_ = pend.pop(0)
            P.op('dve', lambda e: e.tensor_tensor(out=GAT[:, il_, tt_ * 128:(tt_ + 1) * 128], in0=pG[b_][:, :], in1=gelT[ib_][:, tt_ * 128:(tt_ + 1) * 128], op=ALU.mult),
                 reads=[('pG', b_), f'gelT{ib_}'], writes=[('GAT', il_, tt_)])
        def prepA_pieces(i):
            ib = i % 2
            tk = f'thw{ib}'
            pieces = []

            def p_load():
                for dh in range(2):
                    ds_ = slice(dh * 1024, (dh + 1) * 1024)
                    P.dma('sp', lambda e, dh=dh, ds_=ds_: e.dma_start(out=Ust[dh][:, :], in_=pu[i * 128:(i + 1) * 128, ds_]), f'pu{dh}', writes=[f'Ust{dh}'])
                    P.op('act', lambda e, dh=dh, ds_=ds_: e.activation(out=Ub[:, ds_], in_=Ust[dh][:, :], func=AF.Copy), reads=[f'Ust{dh}'], writes=[('Ub', dh)])
                P.op('dve', lambda e: e.tensor_scalar(out=thw[ib][:, 0, :, :], in0=em[:, :, 0:16:2, i], scalar1=1e-30, scalar2=None, op0=ALU.max), reads=['em'], writes=[tk])
                P.op('dve', lambda e: e.reciprocal(out=thw[ib][:, 0, :, :], in_=thw[ib][:, 0, :, :]), reads=[tk], writes=[tk])
                P.op('dve', lambda e: e.tensor_tensor(out=thw[ib][:, 1, :, :], in0=thw[ib][:, 0, :, :], in1=sc[:, :, :, 0], op=ALU.mult), reads=[tk, 'sc'], writes=[tk])
                P.op('dve', lambda e: e.tensor_tensor(out=thw[ib][:, 2, :, :], in0=em[:, :, 0:16:2, i], in1=sc[:, :, :, 1], op=ALU.mult), reads=['em', 'sc', tk], writes=[tk])
            pieces.append(p_load)

            def p_tr(half):
                def f():
                    for j in range(8):
                        kc = half * 8 + j
                        P.op('pe', lambda e, kc=kc, j=j: e.transpose(out=ptr3[:, j, :], in_=Ub[:, kc * 128:(kc + 1) * 128], identity=ident_b[:, :]),
                             reads=[('Ub', half), 'ident_b'], writes=['ptr3'])
                    P.op('act', lambda e: e.activation(out=UT[:, half * 8:half * 8 + 8, :], in_=ptr3[:, :, :], func=AF.Copy), reads=['ptr3'], writes=['UT'])
                return f
            pieces.append(p_tr(0))
            pieces.append(p_tr(1))

            def p_mm(th, k0, k1):
                def f():
                    for kc in range(k0, k1):
                        P.op('pe', lambda e, kc=kc: e.matmul(pA2[th][:, :], lhsT=UT[:, kc, :], rhs=xn2T[:, kc, th * 512:(th + 1) * 512], start=(kc == 0), stop=(kc == KC - 1)),
                             reads=['xn2T', 'UT'], writes=[f'pA2{th}'])
                    if k1 == KC:
                        P.op('act', lambda e: e.activation(out=gelT[ib][:, th * 512:(th + 1) * 512], in_=pA2[th][:, :], func=AF.Gelu), reads=[f'pA2{th}'], writes=[f'gelT{ib}'])
                return f
            for th in range(2):
                for k0 in (0, 6, 11):
                    pieces.append(p_mm(th, k0, {0: 6, 6: 11, 11: 16}[k0]))
            return pieces

        for pc in prepA_pieces(0):
            pc()
        for blk in range(NEI // EB):
            for il in range(EB):
                i = blk * EB + il
                ib = i % 2
                tk = f'thw{ib}'
                for dh in range(2):
                    ds_ = slice(dh * 1024, (dh + 1) * 1024)
                    P.dma('sp', lambda e, i=i, dh=dh, ds_=ds_: e.dma_start(out=Vst[dh][:, :], in_=pv_[i * 128:(i + 1) * 128, ds_]), f'pv{dh}', writes=[f'Vst{dh}'])
                    P.op('act', lambda e, dh=dh, ds_=ds_, il=il: e.activation(out=Vb[il][:, ds_], in_=Vst[dh][:, :], func=AF.Copy), reads=[f'Vst{dh}'], writes=[('Vb', il)])
                nxt = prepA_pieces(i + 1) if i + 1 < NEI else []
                if nxt:
                    nxt.pop(0)()
                for tt in range(8):
                    n = il * 8 + tt
                    b = n % 3
                    gb = n % 3
                    mb = n % 3
                    P.op('pool', lambda e, b=b, ib=ib, tt=tt: e.tensor_tensor(out=Dg[b][:, 0:4, :], in0=ident_f[:, None, :].to_broadcast([128, 4, 128]),
                                                                         in1=thw[ib][:, 2, tt, 0:4, None].to_broadcast([128, 4, 128]), op=ALU.mult),
                         reads=['ident_f', tk], writes=[(f'Dg{b}', 0)])
                    for hh in range(4, 8):
                        P.op('act', lambda e, b=b, ib=ib, tt=tt, hh=hh: e.activation(out=Dg[b][:, hh, :], in_=ident_f[:, :], func=AF.Copy, scale=thw[ib][:, 2, tt, hh:hh + 1]),
                             reads=['ident_f', tk], writes=[(f'Dg{b}', hh)])
                    P.op('dve', lambda e, mb=mb, ib=ib, tt=tt: e.tensor_tensor(out=Mk[mb][:, :, :], in0=em[:, tt, 1:16:2, :], in1=thw[ib][:, 1, tt, :, None].to_broadcast([128, 8, 128]), op=ALU.is_ge),
                         reads=['em', tk], writes=[f'Mk{mb}'])
                    P.op('dve', lambda e, b=b, mb=mb, tt=tt: e.tensor_tensor(out=Yb[b][:, :, :], in0=Mk[mb][:, :, :], in1=em[:, tt, 1:16:2, :], op=ALU.mult),
                         reads=['em', f'Mk{mb}'], writes=[f'Yb{b}'])
                    for hh in range(8):
                        P.op('pe', lambda e, b=b, hh=hh, gb=gb: e.matmul(pG[gb][:, :], lhsT=Yb[b][:, hh, :], rhs=Dg[b][:, hh, :], start=(hh == 0), stop=(hh == 7)),
                             reads=[f'Yb{b}', (f'Dg{b}', 0 if hh < 4 else hh)], writes=[('pG', gb)])
                    pend.append((il, ib, tt, gb))
                    if len(pend) > LAG:
                        flush_one()
                    if nxt:
                        nxt.pop(0)()
                while nxt:
                    nxt.pop(0)()
            while pend:
                flush_one()
            for tt in range(8):
                for nblk in range(4):
                    ob = nblk % 2
                    for il in range(EB):
                        P.op('pe', lambda e, il=il, tt=tt, nblk=nblk, ob=ob: e.matmul(pO[ob][:, :], lhsT=GAT[:, il, tt * 128:(tt + 1) * 128], rhs=Vb[il][:, nblk * 512:(nblk + 1) * 512], start=(il == 0), stop=(il == EB - 1)),
                             reads=[('GAT', il, tt), ('Vb', il)], writes=[('pO', ob)])
                    P.op('dve', lambda e, tt=tt, nblk=nblk, ob=ob: e.tensor_tensor(out=h1[:, tt, nblk * 512:(nblk + 1) * 512], in0=h1[:, tt, nblk * 512:(nblk + 1) * 512], in1=pO[ob][:, :], op=ALU.add),
                         reads=[('pO', ob), ('h1', tt, nblk)], writes=[('h1', tt, nblk)])
        P.barrier()
        for nh in range(2):
            P.dma('sp', lambda e, nh=nh: e.dma_start(out=Vst[nh][:, :], in_=gfr[:, nh * 1024:(nh + 1) * 1024]), f'pv{nh}', writes=[f'Vst{nh}'])
        for tt in range(8):
            b = tt % 2
            rms(tt, b, Ub, ('Ub', 0))
            for nh in range(2):
                P.op('dve', lambda e, tt=tt, b=b, nh=nh: e.scalar_tensor_tensor(out=Ust[nh][:, :], in0=h1[:, tt, nh * 1024:(nh + 1) * 1024], scalar=rstd2[b][:, 0:1], in1=Vst[nh][:, :], op0=ALU.mult, op1=ALU.mult),
                     reads=['h1', f'rstd2{b}', f'Vst{nh}'], writes=[f'Ust{nh}'])
                P.dma('sp', lambda e, tt=tt, nh=nh: e.dma_start(out=yout[tt * 128:(tt + 1) * 128, nh * 1024:(nh + 1) * 1024], in_=Ust[nh][:, :]), f'yo{nh}', reads=[f'Ust{nh}'], writes=[('y', tt, nh)])
        P.barrier()
        sb3.close()
        sB.close()
        P.emit(top)
    return nc


def host_inputs(inp, r):
    b, h = r // 4, r % 4
    w_in = inp['w_in'][0]
    cols = np.concatenate([
        np.arange(h * 256, (h + 1) * 256),
        1024 + np.arange(h * 256, (h + 1) * 256),
        4104 + np.arange(h * 256, (h + 1) * 256),
        2048 + np.arange(h * 256, (h + 1) * 256),
        np.array([4096 + h, 4100 + h]),
        3072 + np.arange(h * 256, (h + 1) * 256),
    ])
    w_a = np.ascontiguousarray(w_in[:, cols].reshape(KC, 128, 1282).transpose(1, 0, 2))
    g1 = np.ascontiguousarray(inp['norm1_g'][0].reshape(KC, 128).T)
    bgv = inp['b_gates'][0]
    bg = np.ascontiguousarray(np.broadcast_to(np.array([bgv[h], bgv[4 + h]], np.float32)[None, :], (128, 2)))
    cwf = inp['conv_qk_w'][0]
    chans = np.concatenate([np.arange(h * 256, (h + 1) * 256), 1024 + np.arange(h * 256, (h + 1) * 256)])
    convw = np.ascontiguousarray(cwf[:, chans].T.reshape(4, 128, 4).transpose(1, 0, 2))
    mg = np.ascontiguousarray(np.broadcast_to(inp['mlstm_norm_g'][0][h * 256:(h + 1) * 256][None, :], (128, 256)))

    G0 = 16 * h
    lre = inp['s5_lambda_re'][0][G0:G0 + 16]
    lim = inp['s5_lambda_im'][0][G0:G0 + 16]
    ldt = np.broadcast_to(inp['s5_log_dt'][0][G0:G0 + 16][:, None], (16, 64))
    def colrow(a):
        col = a.reshape(8, 128).T
        row = a.reshape(1024)
        return col, row
    cols_, rows_ = zip(*[colrow(np.asarray(a, np.float32)) for a in (lre, lim, ldt)])
    s5c = np.ascontiguousarray(np.stack(cols_, axis=1))
    s5r = np.ascontiguousarray(np.broadcast_to(np.stack(rows_, axis=0)[None], (128, 3, 1024)))
    s5b = np.zeros((128, 2, 8, 128), np.float32)
    s5cm = np.zeros((128, 2, 8, 128), np.float32)
    for ri, (bsrc, csrc) in enumerate(((inp['s5_b_re'][0], inp['s5_c_re'][0]), (inp['s5_b_im'][0], inp['s5_c_im'][0]))):
        for gl in range(16):
            q, g2 = gl // 2, gl % 2
            r0 = (gl % 8) * 16
            s5b[r0:r0 + 16, ri, q, g2 * 64:(g2 + 1) * 64] = bsrc[G0 + gl].T
            s5cm[g2 * 64:(g2 + 1) * 64, ri, q, r0:r0 + 16] = csrc[G0 + gl].T
    s5d = np.ascontiguousarray(inp['s5_d'][0][G0:G0 + 16].reshape(2, 128).T)
    s5gw = np.zeros((128, 4, 128), np.float32)
    gw = inp['s5_glu_w'][0]
    for gl in range(16):
        cc, r0 = gl // 8, (gl % 8) * 16
        s5gw[r0:r0 + 16, cc * 2, r0:r0 + 16] = gw[G0 + gl][:, :16]
        s5gw[r0:r0 + 16, cc * 2 + 1, r0:r0 + 16] = gw[G0 + gl][:, 16:]

    perm = np.concatenate([np.concatenate([256 * hh + np.arange(256), 1024 + 256 * hh + np.arange(256)]) for hh in range(4)])
    w_o = np.ascontiguousarray(inp['w_out'][0][perm].reshape(KC, 128, DM).transpose(1, 0, 2))
    w_q = np.ascontiguousarray(inp['peer_wq'][0].reshape(KC, 128, DM).transpose(1, 0, 2))
    g2 = inp['norm2_g'][0]
    g2c = np.ascontiguousarray(g2.reshape(KC, 128).T)
    g2r = np.ascontiguousarray(np.broadcast_to(g2[None, :], (128, DM)))
    gfr = np.ascontiguousarray(np.broadcast_to(inp['final_g'][None, :], (128, DM)))
    skT = np.ascontiguousarray(inp['peer_subkeys'][0].transpose(2, 0, 1))
    d = dict(
        x_own=np.ascontiguousarray(inp['x'][b][h * 1024:(h + 1) * 1024]), w_o=w_o, w_q=w_q, g2c=g2c, g2r=g2r, gfr=gfr, skT=skT,
        pu=inp['peer_u'][0], pv=inp['peer_v'][0], sel=np.ascontiguousarray(np.broadcast_to(np.eye(4, dtype=np.float32)[h][None, :], (128, 4))),
        s5c=s5c, s5r=s5r, s5b=s5b.reshape(128, 2, 1024), s5cm=s5cm.reshape(128, 2, 1024), s5d=s5d, s5gw=s5gw,
        x=np.ascontiguousarray(inp['x'][b]),
        w_a=w_a, g1=g1, bg=bg, convw=convw, mg=mg,
        c_ident=_ident(),
        c_triu=np.triu(np.ones((128, 128), np.float32)),
        c_ones=np.ones((128, 128), np.float32),
    )
    return d


def kernel(**inputs):
    inp = {k: np.asarray(v) for k, v in inputs.items()}
    nc = build('full')
    in_maps = [host_inputs(inp, r) for r in range(8)]
    res = run_bass_kernel_spmd(nc, in_maps, core_ids=list(range(8)))
    out = np.zeros((2, NT, DM), np.float32)
    for r in range(8):
        b, h = r // 4, r % 4
        out[b, h * 1024:(h + 1) * 1024] = res.results[r]['y']
    return out
```

```python
import os
import numpy as np
import ml_dtypes
from contextlib import ExitStack
import concourse.bass as bass
import concourse.mybir as mybir
from concourse.bass_utils import run_bass_kernel_spmd

F32 = mybir.dt.float32
BF16 = mybir.dt.bfloat16
ALU = mybir.AluOpType
AF = mybir.ActivationFunctionType
AX = mybir.AxisListType

EPS = 1e-6
NT = 4096
DM = 2048
KC = 16


class Prog:
    ENG = ['pe', 'dve', 'act', 'pool', 'sp']

    def __init__(self, nc):
        self.nc = nc
        self.ops = {e: [] for e in self.ENG}
        self.cnt = {e: 0 for e in self.ENG}
        self.dcnt = {}
        self.lastw = {}
        self.rds = {}
        self.floor = {}

    def _tok_add(self, d, tok):
        s, v, e = tok
        if s not in d or d[s][0] < v:
            d[s] = (v, e)

    def _mk(self, reads, writes):
        deps = dict(self.floor)
        for k in reads:
            if k in self.lastw:
                self._tok_add(deps, self.lastw[k])
        for k in writes:
            if k in self.lastw:
                self._tok_add(deps, self.lastw[k])
            for s, (v, e) in self.rds.get(k, {}).items():
                self._tok_add(deps, (s, v, e))
        return deps

    def _commit(self, tok, reads, writes):
        for k in reads:
            self._tok_add(self.rds.setdefault(k, {}), tok)
        for k in writes:
            self.lastw[k] = tok
            self.rds[k] = {}

    def _skip(self, force):
        self.total = getattr(self, 'total', 0) + 1
        cut = int(os.environ.get('KCUT', '0'))
        return bool(cut) and self.total > cut and not force

    def op(self, eng, fn, reads=(), writes=(), force=False):
        if self._skip(force):
            return
        deps = self._mk(reads, writes)
        self.cnt[eng] += 1
        tok = (eng, self.cnt[eng], eng)
        self.ops[eng].append((deps, fn, eng, 1))
        self._commit(tok, reads, writes)

    def dma(self, queue, fn, stream, reads=(), writes=(), inc=16, force=False):
        if self._skip(force):
            return
        deps = self._mk(reads, writes)
        if stream == 'c':
            self.nuniq = getattr(self, 'nuniq', 0) + 1
            stream = f'c{self.nuniq}'
        s = 'd_' + stream
        self.dcnt[s] = self.dcnt.get(s, 0) + inc
        tok = (s, self.dcnt[s], 'dma')
        self.ops[queue].append((deps, fn, s, inc))
        self._commit(tok, reads, writes)

    def barrier(self):
        for e in self.ENG:
            if self.cnt[e]:
                self.floor[e] = (self.cnt[e], e)
        for s, v in self.dcnt.items():
            self.floor[s] = (v, 'dma')

    def emit(self, stack, final_waits=True):
        nc = self.nc
        sems = {}
        for e in self.ENG:
            sems[e] = stack.enter_context(nc.semaphore('sem_' + e))
        for s in self.dcnt:
            sems[s] = stack.enter_context(nc.semaphore('sem_' + s))
        block = stack.enter_context(nc.Block())
        total = dict((e, (self.cnt[e], e)) for e in self.ENG if self.cnt[e])
        for s, v in self.dcnt.items():
            total[s] = (v, 'dma')

        def run(ename):
            def body(eng):
                seen = {}
                for deps, fn, sname, inc in self.ops[ename]:
                    for s, (v, de) in deps.items():
                        if de == ename and ename == 'pe':
                            continue
                        if seen.get(s, 0) < v:
                            eng.wait_ge(sems[s], v)
                            seen[s] = v
                    ins = fn(eng)
                    ins.then_inc(sems[sname], inc)
                if ename == 'sp':
                    for s, (v, de) in total.items():
                        if seen.get(s, 0) < v:
                            eng.wait_ge(sems[s], v)
            return body
        block.tensor(run('pe'))
        block.vector(run('dve'))
        block.scalar(run('act'))
        block.gpsimd(run('pool'))
        block.sync(run('sp'))


def _ident():
    return np.eye(128, dtype=np.float32)


def build(stage='full'):
    nc = bass.Bass("TRN2", target_bir_lowering=False)
    P = Prog(nc)
    D = {}

    def din(name, shape, dt=F32):
        D[name] = nc.dram_tensor(name, list(shape), dt, kind="ExternalInput").ap()
        return D[name]

    x = din('x', [NT, DM])
    w_a = din('w_a', [128, KC, 1282])
    g1 = din('g1', [128, KC])
    bg = din('bg', [128, 2])
    convw = din('convw', [128, 4, 4])
    mg = din('mg', [128, 256])
    c_ident = din('c_ident', [128, 128])
    c_triu = din('c_triu', [128, 128])
    c_ones = din('c_ones', [128, 128])

    s5c = din('s5c', [128, 3, 8])
    s5r = din('s5r', [128, 3, 1024])
    s5b = din('s5b', [128, 2, 1024])
    s5cm = din('s5cm', [128, 2, 1024])
    s5d = din('s5d', [128, 2])
    s5gw = din('s5gw', [128, 4, 128])
    x_own = din('x_own', [1024, DM])
    w_o = din('w_o', [128, KC, DM])
    w_q = din('w_q', [128, KC, DM])
    g2c = din('g2c', [128, KC])
    g2r = din('g2r', [128, DM])
    gfr = din('gfr', [128, DM])
    skT = din('skT', [128, 2, 128])
    pu = din('pu', [16384, DM])
    pv_ = din('pv', [16384, DM])
    selin = din('sel', [128, 4])
    if stage == 'simB':
        mx_test = din('mx_test', [2048, NT], BF16)
    if stage in ('full', 'simB'):
        yout = nc.dram_tensor('y', [1024, DM], F32, kind="ExternalOutput").ap()
    mxi = [nc.dram_tensor(f'mxi{q}', [512, 1024], BF16) for q in range(4)]
    mxa = [nc.dram_tensor(f'mxa{q}', [2048, 1024], BF16) for q in range(4)]

    if stage in ('A1', 'A2', 'A3'):
        dbg = nc.dram_tensor('dbg', [512, NT], BF16, kind="ExternalOutput").ap()

    top = ExitStack()
    with top:
        def sb(stack, name, shape, dt=F32):
            return stack.enter_context(nc.sbuf_tensor(name, list(shape), dt))

        def ps(stack, name, shape, dt=F32):
            return stack.enter_context(nc.psum_tensor(name, list(shape), dt))

        ident_f = sb(top, 'ident_f', [128, 128])
        ident_b = sb(top, 'ident_b', [128, 128], BF16)
        triu = sb(top, 'triu', [128, 128])
        ones = sb(top, 'ones', [128, 128])
        P.dma('sp', lambda e: e.dma_start(out=ident_f[:, :], in_=c_ident[:, :]), 'c', writes=['ident_f'])
        P.dma('sp', lambda e: e.dma_start(out=triu[:, :], in_=c_triu[:, :]), 'c', writes=['triu'])
        P.dma('sp', lambda e: e.dma_start(out=ones[:, :], in_=c_ones[:, :]), 'c', writes=['ones'])
        P.op('dve', lambda e: e.tensor_copy(out=ident_b[:, :], in_=ident_f[:, :]), reads=['ident_f'], writes=['ident_b'])

        sA = ExitStack()
        uT = sb(sA, 'uT', [128, 2, NT], BF16)
        sA2 = ExitStack()
        qT = sb(sA2, 'qT', [128, 2, NT], BF16)
        kT = sb(sA2, 'kT', [128, 2, NT], BF16)
        v_aug = sb(sA2, 'v_aug', [128, 32, 257], BF16)
        gso = sb(sA2, 'gso', [128, 32, 256], BF16)
        g_tm = sb(sA2, 'g_tm', [128, 32, 2])
        bgt = sb(sA2, 'bgt', [128, 2])
        P.op('pool', lambda e: e.memset(v_aug[:, :, 256:257], 1.0), writes=['v_aug'])

        s1 = ExitStack()
        W = sb(s1, 'W', [128, KC, 1282], BF16)
        wst = [sb(s1, f'wst{i}', [128, 1282]) for i in range(2)]
        g1t = sb(s1, 'g1t', [128, KC])
        cw = sb(s1, 'cw', [128, 4, 4])
        mgt = sb(s1, 'mgt', [128, 256])
        xt = [sb(s1, f'xt{i}', [128, DM]) for i in range(2)]
        xs = [sb(s1, f'xs{i}', [128, DM], BF16) for i in range(2)]
        junk = sb(s1, 'junk', [128, DM], BF16)
        ssq = [sb(s1, f'ssq{i}', [128, 1]) for i in range(2)]
        rstd = [sb(s1, f'rstd{i}', [128, 1]) for i in range(2)]
        xnT = sb(s1, 'xnT', [128, KC, 512], BF16)
        pre = sb(s1, 'pre', [128, 4, 515])
        cacc = [sb(s1, f'cacc{i}', [128, 512]) for i in range(2)]
        sgo = [sb(s1, f'sgo{i}', [128, 256]) for i in range(2)]
        ptr = [ps(s1, f'ptr{i}', [128, 8, 128], BF16) for i in range(2)]
        pf = [ps(s1, f'pf{i}', [128, 512]) for i in range(2)]
        pv = [ps(s1, f'pv{i}', [128, 258]) for i in range(2)]
        po = [ps(s1, f'po{i}', [128, 256]) for i in range(2)]

        P.dma('sp', lambda e: e.dma_start(out=g1t[:, :], in_=g1[:, :]), 'c', writes=['g1t'])
        P.dma('sp', lambda e: e.dma_start(out=bgt[:, :], in_=bg[:, :]), 'c', writes=['bgt'])
        P.dma('sp', lambda e: e.dma_start(out=cw[:, :, :], in_=convw[:, :, :]), 'c', writes=['cw'])
        P.dma('sp', lambda e: e.dma_start(out=mgt[:, :], in_=mg[:, :]), 'c', writes=['mgt'])
        for kc in range(KC):
            b = kc % 2
            P.dma('sp', lambda e, kc=kc, b=b: e.dma_start(out=wst[b][:, :], in_=w_a[:, kc, :]), f'w{b}', writes=[f'wst{b}'])
            P.op('dve' if kc % 2 == 0 else 'pool',
                 lambda e, kc=kc, b=b: e.tensor_scalar(out=W[:, kc, :], in0=wst[b][:, :], scalar1=g1t[:, kc:kc + 1], scalar2=None, op0=ALU.mult),
                 reads=[f'wst{b}', 'g1t'], writes=[('W', kc)])
        Wkeys = [('W', kc) for kc in range(KC)]
        for m in range(4):
            P.op('pool', lambda e, m=m: e.memset(pre[:, m, 0:3], 0.0), writes=[('pre', m)])

        print('ops before token loop', getattr(P, 'total', 0))
        for tb in range(8):
            print('tb', tb, getattr(P, 'total', 0))
            for t4 in range(4):
                c = tb * 4 + t4
                b = c % 2
                P.dma('sp', lambda e, c=c, b=b: e.dma_start(out=xt[b][:, :], in_=x[c * 128:(c + 1) * 128, :]), f'x{b}', writes=[f'xt{b}'])
                P.op('act', lambda e, b=b: e.activation(out=junk[:, :], in_=xt[b][:, :], func=AF.Square, accum_out=ssq[b][:, :]),
                     reads=[f'xt{b}'], writes=['junk', f'ssq{b}'])
                P.op('dve', lambda e, b=b: e.tensor_scalar(out=rstd[b][:, :], in0=ssq[b][:, :], scalar1=1.0 / DM, scalar2=EPS, op0=ALU.mult, op1=ALU.add),
                     reads=[f'ssq{b}'], writes=[f'rstd{b}'])
                P.op('act', lambda e, b=b: e.activation(out=rstd[b][:, :], in_=rstd[b][:, :], func=AF.Sqrt),
                     reads=[f'rstd{b}'], writes=[f'rstd{b}'])
                P.op('dve', lambda e, b=b: e.reciprocal(out=rstd[b][:, :], in_=rstd[b][:, :]),
                     reads=[f'rstd{b}'], writes=[f'rstd{b}'])
                P.op('dve', lambda e, b=b: e.tensor_scalar(out=xs[b][:, :], in0=xt[b][:, :], scalar1=rstd[b][:, 0:1], scalar2=None, op0=ALU.mult),
                     reads=[f'xt{b}', f'rstd{b}'], writes=[f'xs{b}'])
                for half in range(2):
                    for j in range(8):
                        kc = half * 8 + j
                        P.op('pe', lambda e, b=b, kc=kc, half=half, j=j: e.transpose(out=ptr[half][:, j, :], in_=xs[b][:, kc * 128:(kc + 1) * 128], identity=ident_b[:, :]),
                             reads=[f'xs{b}', 'ident_b'], writes=[f'ptr{half}'])
                    P.op('act' if half == 0 else 'dve',
                         (lambda e, half=half, t4=t4: e.activation(out=xnT[:, half * 8:half * 8 + 8, t4 * 128:(t4 + 1) * 128], in_=ptr[half][:, :, :], func=AF.Copy)) if half == 0 else
                         (lambda e, half=half, t4=t4: e.tensor_copy(out=xnT[:, half * 8:half * 8 + 8, t4 * 128:(t4 + 1) * 128], in_=ptr[half][:, :, :])),
                         reads=[f'ptr{half}'], writes=[('xnT', t4)])
            xk = [('xnT', t) for t in range(4)]
            for m in range(6):
                pb = m % 2
                for kc in range(KC):
                    P.op('pe', lambda e, m=m, kc=kc, pb=pb: e.matmul(pf[pb][:, :], lhsT=W[:, kc, m * 128:(m + 1) * 128], rhs=xnT[:, kc, :], start=(kc == 0), stop=(kc == KC - 1)),
                         reads=xk + Wkeys, writes=[f'pf{pb}'])
                if m < 4:
                    P.op('act', lambda e, m=m, pb=pb: e.activation(out=pre[:, m, 3:515], in_=pf[pb][:, :], func=AF.Copy),
                         reads=[f'pf{pb}'], writes=[('pre', m)])
                    cb = m % 2
                    P.op('dve', lambda e, m=m, cb=cb: e.tensor_scalar(out=cacc[cb][:, :], in0=pre[:, m, 0:512], scalar1=cw[:, m, 0:1], scalar2=None, op0=ALU.mult),
                         reads=[('pre', m), 'cw'], writes=[f'cacc{cb}'])
                    for j in range(1, 4):
                        P.op('dve', lambda e, m=m, cb=cb, j=j: e.scalar_tensor_tensor(out=cacc[cb][:, :], in0=pre[:, m, j:j + 512], scalar=cw[:, m, j:j + 1], in1=cacc[cb][:, :], op0=ALU.mult, op1=ALU.add),
                             reads=[('pre', m), 'cw', f'cacc{cb}'], writes=[f'cacc{cb}'])
                    dst = qT if m < 2 else kT
                    P.op('act', lambda e, m=m, cb=cb, dst=dst, tb=tb: e.activation(out=dst[:, m % 2, tb * 512:(tb + 1) * 512], in_=cacc[cb][:, :], func=AF.Silu),
                         reads=[f'cacc{cb}'], writes=[('qk', m, tb)])
                    P.op('pool', lambda e, m=m: e.tensor_copy(out=pre[:, m, 0:3], in_=pre[:, m, 512:515]),
                         reads=[('pre', m)], writes=[('pre', m)])
                else:
                    P.op('dve', lambda e, m=m, pb=pb, tb=tb: e.tensor_copy(out=uT[:, m - 4, tb * 512:(tb + 1) * 512], in_=pf[pb][:, :]),
                         reads=[f'pf{pb}'], writes=[('uT', m - 4, tb)])
            for t4 in range(4):
                c = tb * 4 + t4
                pb = c % 2
                for kc in range(KC):
                    P.op('pe', lambda e, kc=kc, pb=pb, t4=t4: e.matmul(pv[pb][:, :], lhsT=xnT[:, kc, t4 * 128:(t4 + 1) * 128], rhs=W[:, kc, 768:1026], start=(kc == 0), stop=(kc == KC - 1)),
                         reads=xk + Wkeys, writes=[f'pv{pb}'])
                for kc in range(KC):
                    P.op('pe', lambda e, kc=kc, pb=pb, t4=t4: e.matmul(po[pb][:, :], lhsT=xnT[:, kc, t4 * 128:(t4 + 1) * 128], rhs=W[:, kc, 1026:1282], start=(kc == 0), stop=(kc == KC - 1)),
                         reads=xk + Wkeys, writes=[f'po{pb}'])
                P.op('dve', lambda e, c=c, pb=pb: e.tensor_copy(out=v_aug[:, c, 0:256], in_=pv[pb][:, 0:256]),
                     reads=[f'pv{pb}'], writes=[('v', c)])
                P.op('dve', lambda e, c=c, pb=pb: e.tensor_copy(out=g_tm[:, c, :], in_=pv[pb][:, 256:258]),
                     reads=[f'pv{pb}'], writes=[('g_tm', c)])
                P.op('act', lambda e, pb=pb: e.activation(out=sgo[pb][:, :], in_=po[pb][:, :], func=AF.Sigmoid),
                     reads=[f'po{pb}'], writes=[f'sgo{pb}'])
                P.op('pool', lambda e, c=c, pb=pb: e.tensor_tensor(out=gso[:, c, :], in0=sgo[pb][:, :], in1=mgt[:, :], op=ALU.mult),
                     reads=[f'sgo{pb}', 'mgt'], writes=[('gso', c)])
        P.barrier()
        s1.close()
        if stage == 'A1':
            P.dma('sp', lambda e: e.dma_start(out=dbg[0:128, :], in_=qT[:, 0, :]), 'out', writes=['dbg'], force=True)
            P.dma('sp', lambda e: e.dma_start(out=dbg[128:256, :], in_=kT[:, 1, :]), 'out', writes=['dbg'], force=True)
            P.dma('sp', lambda e: e.dma_start(out=dbg[256:384, :], in_=uT[:, 0, :]), 'out', writes=['dbg'], force=True)
            sA2.close()
            sA.close()
            P.emit(top)
            return nc

        print('ops before A2', getattr(P, 'total', 0))
        s2 = ExitStack()
        li = sb(s2, 'li', [128, 32])
        nlf = sb(s2, 'nlf', [128, 32])
        nega = sb(s2, 'nega', [128, 32])
        tmp = sb(s2, 'tmp', [128, 32])
        tmp2 = sb(s2, 'tmp2', [128, 32])
        es = sb(s2, 'es', [128, 32])
        wk = sb(s2, 'wk', [128, 32])
        eA = sb(s2, 'eA', [128, 32])
        nlfrep = sb(s2, 'nlfrep', [128, 32, 128])
        ea_bc = sb(s2, 'ea_bc', [128, NT])
        lnc = sb(s2, 'lnc', [128, 1])
        Cst = sb(s2, 'Cst', [128, 2, 257])
        Cb = sb(s2, 'Cb', [128, 2, 257], BF16)
        Pm = [sb(s2, f'Pm{i}', [128, 128], BF16) for i in range(2)]
        kw = [sb(s2, f'kw{i}', [128, 256], BF16) for i in range(2)]
        hmn = [sb(s2, f'hmn{i}', [128, 256], BF16) for i in range(2)]
        hT = [sb(s2, f'hT{i}', [128, 2, 128], BF16) for i in range(2)]
        sm = [sb(s2, f'sm{i}', [128, 8]) for i in range(2)]
        junk2 = sb(s2, 'junk2', [128, 256], BF16)
        pa = ps(s2, 'pa', [128, 64])
        pbc = ps(s2, 'pbc', [128, 512])
        ps1 = ps(s2, 'ps1', [128, 128])
        ptk = ps(s2, 'ptk', [128, 2, 128], BF16)
        psO = ps(s2, 'psO', [128, 257])
        pth = ps(s2, 'pth', [128, 2, 128], BF16)
        psC = [ps(s2, f'psC{i}', [128, 257]) for i in range(2)]

        gk = [('g_tm', c) for c in range(32)]
        P.op('dve', lambda e: e.tensor_scalar(out=li[:, :], in0=g_tm[:, :, 0], scalar1=bgt[:, 0:1], scalar2=None, op0=ALU.add), reads=gk + ['bgt'], writes=['li'])
        P.op('dve', lambda e: e.tensor_scalar(out=tmp[:, :], in0=g_tm[:, :, 1], scalar1=bgt[:, 1:2], scalar2=None, op0=ALU.add), reads=gk + ['bgt'], writes=['tmp'])
        P.op('act', lambda e: e.activation(out=tmp2[:, :], in_=tmp[:, :], func=AF.Exp, scale=-1.0), reads=['tmp'], writes=['tmp2'])
        P.op('dve', lambda e: e.tensor_scalar(out=tmp2[:, :], in0=tmp2[:, :], scalar1=1.0, scalar2=None, op0=ALU.add), reads=['tmp2'], writes=['tmp2'])
        P.op('act', lambda e: e.activation(out=nlf[:, :], in_=tmp2[:, :], func=AF.Ln), reads=['tmp2'], writes=['nlf'])
        P.op('dve', lambda e: e.memset(lnc[:, :], float(np.log(1.0 / 16.0))), writes=['lnc'])
        P.op('pe', lambda e: e.matmul(pa[:, 0:32], lhsT=triu[:, :], rhs=nlf[:, :], start=True, stop=True), reads=['triu', 'nlf'], writes=['pa'])
        P.op('pe', lambda e: e.matmul(pa[:, 32:64], lhsT=ones[:, :], rhs=nlf[:, :], start=True, stop=True), reads=['ones', 'nlf'], writes=['pa'])
        P.op('dve', lambda e: e.tensor_copy(out=nega[:, :], in_=pa[:, 0:32]), reads=['pa'], writes=['nega'])
        P.op('dve', lambda e: e.tensor_tensor(out=tmp[:, :], in0=li[:, :], in1=nega[:, :], op=ALU.add), reads=['li', 'nega'], writes=['tmp'])
        P.op('act', lambda e: e.activation(out=es[:, :], in_=tmp[:, :], func=AF.Exp), reads=['tmp'], writes=['es'])
        P.op('dve', lambda e: e.tensor_tensor(out=tmp2[:, :], in0=tmp[:, :], in1=pa[:, 32:64], op=ALU.subtract), reads=['tmp', 'pa'], writes=['tmp2'])
        P.op('act', lambda e: e.activation(out=wk[:, :], in_=tmp2[:, :], func=AF.Exp), reads=['tmp2'], writes=['wk'])
        P.op('act', lambda e: e.activation(out=eA[:, :], in_=pa[:, 32:64], func=AF.Exp, scale=-1.0), reads=['pa'], writes=['eA'])
        P.op('dve', lambda e: e.tensor_copy(out=nlfrep[:, :, :], in_=nlf[:, :].to_broadcast([128, 32, 128]) if False else nlf[:, :, None].to_broadcast([128, 32, 128])),
             reads=['nlf'], writes=['nlfrep'])
        for k8 in range(8):
            for j in range(4):
                c = k8 * 4 + j
                P.op('pe', lambda e, c=c, j=j: e.matmul(pbc[:, j * 128:(j + 1) * 128], lhsT=nlfrep[:, c, :], rhs=triu[:, :], start=True, stop=True),
                     reads=['nlfrep', 'triu'], writes=['pbc'])
            P.op('act', lambda e, k8=k8: e.activation(out=ea_bc[:, k8 * 512:(k8 + 1) * 512], in_=pbc[:, :], func=AF.Exp, scale=-1.0, bias=lnc[:, 0:1]),
                 reads=['pbc', 'lnc'], writes=[('ea_bc', k8)])
            for dc in range(2):
                P.op('dve' if dc == 0 else 'pool', lambda e, k8=k8, dc=dc: e.tensor_tensor(out=qT[:, dc, k8 * 512:(k8 + 1) * 512], in0=qT[:, dc, k8 * 512:(k8 + 1) * 512], in1=ea_bc[:, k8 * 512:(k8 + 1) * 512], op=ALU.mult),
                     reads=[('ea_bc', k8), ('qk', dc, k8)], writes=[('qk', dc, k8)])
        P.op('dve', lambda e: e.memset(Cst[:, :, :], 0.0), writes=['Cst'])
        P.op('pool', lambda e: e.memset(Cb[:, :, :], 0.0), writes=['Cb'])

        for c in range(32):
            b = c % 2
            tb = c // 4
            cs = slice(c * 128, (c + 1) * 128)
            qkk = [('qk', m, tb) for m in range(4)]
            for dc in range(2):
                P.op('pe', lambda e, dc=dc, cs=cs: e.matmul(ps1[:, :], lhsT=kT[:, dc, cs], rhs=qT[:, dc, cs], start=(dc == 0), stop=(dc == 1)),
                     reads=qkk, writes=['ps1'])
            P.op('dve', lambda e, b=b, c=c: e.scalar_tensor_tensor(out=Pm[b][:, :], in0=ps1[:, :], scalar=es[:, c:c + 1], in1=triu[:, :], op0=ALU.mult, op1=ALU.mult),
                 reads=['ps1', 'es', 'triu'], writes=[f'Pm{b}'])
            for dc in range(2):
                P.op('pe', lambda e, dc=dc, cs=cs: e.transpose(out=ptk[:, dc, :], in_=kT[:, dc, cs], identity=ident_b[:, :]),
                     reads=qkk + ['ident_b'], writes=['ptk'])
            P.op('act', lambda e, b=b, c=c: e.activation(out=kw[b][:, :], in_=ptk[:, :, :], func=AF.Copy, scale=wk[:, c:c + 1]),
                 reads=['ptk', 'wk'], writes=[f'kw{b}'])
            P.op('pe', lambda e, b=b, c=c: e.matmul(psO[:, :], lhsT=Pm[b][:, :], rhs=v_aug[:, c, :], start=True, stop=False),
                 reads=[f'Pm{b}', ('v', c), 'v_aug'], writes=['psO'])
            for dc in range(2):
                P.op('pe', lambda e, dc=dc, cs=cs: e.matmul(psO[:, :], lhsT=qT[:, dc, cs], rhs=Cb[:, dc, :], start=False, stop=(dc == 1)),
                     reads=qkk + ['Cb'], writes=['psO'])
            P.op('act', lambda e, b=b: e.activation(out=sm[b][:, 0:1], in_=psO[:, 256:257], func=AF.Abs), reads=['psO'], writes=[f'sm{b}'])
            P.op('dve', lambda e, b=b: e.tensor_scalar(out=sm[b][:, 0:1], in0=sm[b][:, 0:1], scalar1=1.0, scalar2=None, op0=ALU.max), reads=[f'sm{b}'], writes=[f'sm{b}'])
            P.op('dve', lambda e, b=b: e.reciprocal(out=sm[b][:, 1:2], in_=sm[b][:, 0:1]), reads=[f'sm{b}'], writes=[f'sm{b}'])
            P.op('act', lambda e, b=b: e.activation(out=junk2[:, :], in_=psO[:, 0:256], func=AF.Square, accum_out=sm[b][:, 2:3]), reads=['psO', f'sm{b}'], writes=['junk2', f'sm{b}'])
            P.op('dve', lambda e, b=b: e.tensor_scalar(out=sm[b][:, 3:4], in0=sm[b][:, 2:3], scalar1=sm[b][:, 1:2], scalar2=sm[b][:, 1:2], op0=ALU.mult, op1=ALU.mult), reads=[f'sm{b}'], writes=[f'sm{b}'])
            P.op('dve', lambda e, b=b: e.tensor_scalar(out=sm[b][:, 4:5], in0=sm[b][:, 3:4], scalar1=1.0 / 256.0, scalar2=EPS, op0=ALU.mult, op1=ALU.add), reads=[f'sm{b}'], writes=[f'sm{b}'])
            P.op('act', lambda e, b=b: e.activation(out=sm[b][:, 6:7], in_=sm[b][:, 4:5], func=AF.Sqrt), reads=[f'sm{b}'], writes=[f'sm{b}'])
            P.op('dve', lambda e, b=b: e.reciprocal(out=sm[b][:, 7:8], in_=sm[b][:, 6:7]), reads=[f'sm{b}'], writes=[f'sm{b}'])
            P.op('dve', lambda e, b=b: e.tensor_tensor(out=sm[b][:, 5:6], in0=sm[b][:, 7:8], in1=sm[b][:, 1:2], op=ALU.mult), reads=[f'sm{b}'], writes=[f'sm{b}'])
            P.op('dve', lambda e, b=b, c=c: e.scalar_tensor_tensor(out=hmn[b][:, :], in0=psO[:, 0:256], scalar=sm[b][:, 5:6], in1=gso[:, c, :], op0=ALU.mult, op1=ALU.mult),
                 reads=['psO', f'sm{b}', ('gso', c)], writes=[f'hmn{b}'])
            for ec in range(2):
                P.op('pe', lambda e, b=b, ec=ec: e.transpose(out=pth[:, ec, :], in_=hmn[b][:, ec * 128:(ec + 1) * 128], identity=ident_b[:, :]),
                     reads=[f'hmn{b}', 'ident_b'], writes=['pth'])
            P.op('act', lambda e, b=b: e.activation(out=hT[b][:, :, :], in_=pth[:, :, :], func=AF.Copy), reads=['pth'], writes=[f'hT{b}'])
            P.dma('sp', lambda e, b=b, c=c: e.dma_start(out=mxi[c // 8][0:256, (c % 8) * 128:(c % 8 + 1) * 128].rearrange("(ec p) j -> p ec j", p=128), in_=hT[b][:, :, :]), f'h{b}',
                  reads=[f'hT{b}'], writes=[('mx_in', 'h', c)])
            for dc in range(2):
                P.op('pe', lambda e, b=b, c=c, dc=dc: e.matmul(psC[dc][:, :], lhsT=kw[b][:, dc * 128:(dc + 1) * 128], rhs=v_aug[:, c, :], start=True, stop=True),
                     reads=[f'kw{b}', ('v', c), 'v_aug'], writes=[f'psC{dc}'])
                P.op('dve', lambda e, c=c, dc=dc: e.scalar_tensor_tensor(out=Cst[:, dc, :], in0=Cst[:, dc, :], scalar=eA[:, c:c + 1], in1=psC[dc][:, :], op0=ALU.mult, op1=ALU.add),
                     reads=['Cst', 'eA', f'psC{dc}'], writes=['Cst'])
            P.op('act', lambda e: e.activation(out=Cb[:, :, :], in_=Cst[:, :, :], func=AF.Copy), reads=['Cst'], writes=['Cb'])
        P.barrier()
        s2.close()
        sA2.close()

        if stage == 'A2':
            hk = [('mx_in', 'h', c) for c in range(32)]
            for q_ in range(4):
                P.dma('sp', lambda e, q_=q_: e.dma_start(out=dbg[0:256, q_ * 1024:(q_ + 1) * 1024], in_=mxi[q_][0:256, :]), 'out', reads=hk, writes=['dbg'])
            sA2.close()
            sA.close()
            P.emit(top)
            return nc


        print('ops before A3', getattr(P, 'total', 0))
        s3 = ExitStack()
        colp = sb(s3, 'colp', [128, 3, 8])
        rowp = sb(s3, 'rowp', [128, 3, 1024])
        bl = sb(s3, 'bl', [128, 2, 1024])
        cml = sb(s3, 'cml', [128, 2, 1024])
        dcol = sb(s3, 'dcol', [128, 2])
        gwf = sb(s3, 'gwf', [128, 4, 128])
        gwb = sb(s3, 'gwb', [128, 4, 128], BF16)
        hpi = sb(s3, 'hpi', [128, 1])
        Bre = sb(s3, 'Bre', [128, 1024], BF16)
        Bim = sb(s3, 'Bim', [128, 1024], BF16)
        pwr = sb(s3, 'pwr', [128, 8, 12])
        pwi = sb(s3, 'pwi', [128, 8, 12])
        npwi = sb(s3, 'npwi', [128, 8, 12])
        yacc = sb(s3, 'yacc', [128, NT])
        gy = sb(s3, 'gy', [128, NT], BF16)
        sgb = [sb(s3, f'sgb{i}', [128, 512]) for i in range(2)]
        yso = [sb(s3, f'yso{i}', [128, 512], BF16) for i in range(2)]
        pB = [ps(s3, f'pB{i}', [128, 512]) for i in range(4)]
        pY = [ps(s3, f'pY{i}', [128, 512]) for i in range(2)]
        pGa = ps(s3, 'pGa', [128, 512])
        pGb = ps(s3, 'pGb', [128, 512])
        Ur = sb(s3, 'Ur', [128, 8, 64])
        Ui = sb(s3, 'Ui', [128, 8, 64])
        Vr = sb(s3, 'Vr', [128, 8, 64])
        Vi = sb(s3, 'Vi', [128, 8, 64])
        t1s = sb(s3, 't1s', [128, 8, 64])
        t2s = sb(s3, 't2s', [128, 8, 64])
        lamz = sb(s3, 'lamz', [128, 8, 4])
        cS = [sb(s3, f'cS{i}', [128, 2, 64]) for i in range(2)]
        cZ = sb(s3, 'cZ', [128, 2, 65])
        s3t = ExitStack()
        rt = [sb(s3t, f'rt{i}', [128, 1024]) for i in range(7)]
        ct = [sb(s3t, f'ct{i}', [128, 8]) for i in range(8)]

        P.dma('sp', lambda e: e.dma_start(out=colp[:, :, :], in_=s5c[:, :, :]), 'c', writes=['colp'])
        P.dma('sp', lambda e: e.dma_start(out=rowp[:, :, :], in_=s5r[:, :, :]), 'c', writes=['rowp'])
        P.dma('sp', lambda e: e.dma_start(out=bl[:, :, :], in_=s5b[:, :, :]), 'c', writes=['bl'])
        P.dma('sp', lambda e: e.dma_start(out=cml[:, :, :], in_=s5cm[:, :, :]), 'c', writes=['cml'])
        P.dma('sp', lambda e: e.dma_start(out=dcol[:, :], in_=s5d[:, :]), 'c', writes=['dcol'])
        P.dma('sp', lambda e: e.dma_start(out=gwf[:, :, :], in_=s5gw[:, :, :]), 'c', writes=['gwf'])
        P.op('dve', lambda e: e.tensor_copy(out=gwb[:, :, :], in_=gwf[:, :, :]), reads=['gwf'], writes=['gwb'])
        P.op('dve', lambda e: e.memset(hpi[:, :], float(np.pi / 2)), writes=['hpi'])

        def lam_bar(src, T, n, tag, srckey):
            dt_, lrd, th, sn, cs, t1, t2 = T[:7]
            k = [tag]
            P.op('act', lambda e: e.activation(out=dt_[:, 0:n], in_=src[:, 2, :], func=AF.Exp), reads=k + [srckey], writes=k)
            P.op('dve', lambda e: e.tensor_tensor(out=lrd[:, 0:n], in0=src[:, 0, :], in1=dt_[:, 0:n], op=ALU.mult), reads=k + [srckey], writes=k)
            P.op('dve', lambda e: e.tensor_tensor(out=th[:, 0:n], in0=src[:, 1, :], in1=dt_[:, 0:n], op=ALU.mult), reads=k + [srckey], writes=k)
            P.op('act', lambda e: e.activation(out=sn[:, 0:n], in_=th[:, 0:n], func=AF.Sin, scale=1.0 / 16.0), reads=k, writes=k)
            P.op('act', lambda e: e.activation(out=cs[:, 0:n], in_=th[:, 0:n], func=AF.Sin, scale=1.0 / 16.0, bias=hpi[:, 0:1]), reads=k + ['hpi'], writes=k)
            for _ in range(4):
                P.op('dve', lambda e: e.tensor_tensor(out=t1[:, 0:n], in0=cs[:, 0:n], in1=cs[:, 0:n], op=ALU.mult), reads=k, writes=k)
                P.op('dve', lambda e: e.tensor_tensor(out=t2[:, 0:n], in0=sn[:, 0:n], in1=sn[:, 0:n], op=ALU.mult), reads=k, writes=k)
                P.op('dve', lambda e: e.scalar_tensor_tensor(out=sn[:, 0:n], in0=sn[:, 0:n], scalar=2.0, in1=cs[:, 0:n], op0=ALU.mult, op1=ALU.mult), reads=k, writes=k)
                P.op('dve', lambda e: e.tensor_tensor(out=cs[:, 0:n], in0=t1[:, 0:n], in1=t2[:, 0:n], op=ALU.subtract), reads=k, writes=k)
            P.op('act', lambda e: e.activation(out=t1[:, 0:n], in_=lrd[:, 0:n], func=AF.Exp), reads=k, writes=k)
            P.op('dve', lambda e: e.tensor_tensor(out=cs[:, 0:n], in0=cs[:, 0:n], in1=t1[:, 0:n], op=ALU.mult), reads=k, writes=k)
            P.op('dve', lambda e: e.tensor_tensor(out=sn[:, 0:n], in0=sn[:, 0:n], in1=t1[:, 0:n], op=ALU.mult), reads=k, writes=k)
            return cs, sn

        car, cai = lam_bar(colp, ct, 8, 'c', 'colp')
        P.op('dve', lambda e: e.tensor_copy(out=pwr[:, :, 0], in_=car[:, 0:8]), reads=['c'], writes=['pw'])
        P.op('dve', lambda e: e.tensor_copy(out=pwi[:, :, 0], in_=cai[:, 0:8]), reads=['c'], writes=['pw'])
        for k_ in range(1, 12):
            P.op('dve', lambda e, k_=k_: e.tensor_tensor(out=ct[0][:, :], in0=pwr[:, :, k_ - 1], in1=pwr[:, :, k_ - 1], op=ALU.mult), reads=['pw', 'c'], writes=['c'])
            P.op('dve', lambda e, k_=k_: e.tensor_tensor(out=ct[1][:, :], in0=pwi[:, :, k_ - 1], in1=pwi[:, :, k_ - 1], op=ALU.mult), reads=['pw', 'c'], writes=['c'])
            P.op('dve', lambda e, k_=k_: e.tensor_tensor(out=pwr[:, :, k_], in0=ct[0][:, :], in1=ct[1][:, :], op=ALU.subtract), reads=['c', 'pw'], writes=['pw'])
            P.op('dve', lambda e, k_=k_: e.scalar_tensor_tensor(out=pwi[:, :, k_], in0=pwr[:, :, k_ - 1], scalar=2.0, in1=pwi[:, :, k_ - 1], op0=ALU.mult, op1=ALU.mult), reads=['pw'], writes=['pw'])
        P.op('dve', lambda e: e.tensor_scalar(out=npwi[:, :, :], in0=pwi[:, :, :], scalar1=-1.0, scalar2=None, op0=ALU.mult), reads=['pw'], writes=['npw'])

        rar, rai = lam_bar(rowp, rt, 1024, 'r', 'rowp')
        R = ['r']
        P.op('dve', lambda e: e.tensor_scalar(out=rar[:, :], in0=rar[:, :], scalar1=-1.0, scalar2=None, op0=ALU.add), reads=R, writes=R)
        P.op('dve', lambda e: e.tensor_tensor(out=rt[0][:, :], in0=rowp[:, 0, :], in1=rowp[:, 0, :], op=ALU.mult), reads=R + ['rowp'], writes=R)
        P.op('dve', lambda e: e.tensor_tensor(out=rt[1][:, :], in0=rowp[:, 1, :], in1=rowp[:, 1, :], op=ALU.mult), reads=R + ['rowp'], writes=R)
        P.op('dve', lambda e: e.tensor_tensor(out=rt[0][:, :], in0=rt[0][:, :], in1=rt[1][:, :], op=ALU.add), reads=R, writes=R)
        P.op('dve', lambda e: e.reciprocal(out=rt[0][:, :], in_=rt[0][:, :]), reads=R, writes=R)
        P.op('dve', lambda e: e.tensor_tensor(out=rt[1][:, :], in0=rar[:, :], in1=rowp[:, 0, :], op=ALU.mult), reads=R + ['rowp'], writes=R)
        P.op('dve', lambda e: e.tensor_tensor(out=rt[2][:, :], in0=rai[:, :], in1=rowp[:, 1, :], op=ALU.mult), reads=R + ['rowp'], writes=R)
        P.op('dve', lambda e: e.tensor_tensor(out=rt[1][:, :], in0=rt[1][:, :], in1=rt[2][:, :], op=ALU.add), reads=R, writes=R)
        P.op('dve', lambda e: e.tensor_tensor(out=rt[1][:, :], in0=rt[1][:, :], in1=rt[0][:, :], op=ALU.mult), reads=R, writes=R)
        P.op('dve', lambda e: e.tensor_tensor(out=rt[2][:, :], in0=rai[:, :], in1=rowp[:, 0, :], op=ALU.mult), reads=R + ['rowp'], writes=R)
        P.op('dve', lambda e: e.tensor_tensor(out=rt[5][:, :], in0=rar[:, :], in1=rowp[:, 1, :], op=ALU.mult), reads=R + ['rowp'], writes=R)
        P.op('dve', lambda e: e.tensor_tensor(out=rt[2][:, :], in0=rt[2][:, :], in1=rt[5][:, :], op=ALU.subtract), reads=R, writes=R)
        P.op('dve', lambda e: e.tensor_tensor(out=rt[2][:, :], in0=rt[2][:, :], in1=rt[0][:, :], op=ALU.mult), reads=R, writes=R)
        P.op('dve', lambda e: e.tensor_tensor(out=rt[5][:, :], in0=rt[1][:, :], in1=bl[:, 0, :], op=ALU.mult), reads=R + ['bl'], writes=R)
        P.op('dve', lambda e: e.tensor_tensor(out=rt[6][:, :], in0=rt[2][:, :], in1=bl[:, 1, :], op=ALU.mult), reads=R + ['bl'], writes=R)
        P.op('dve', lambda e: e.tensor_tensor(out=Bre[:, :], in0=rt[5][:, :], in1=rt[6][:, :], op=ALU.subtract), reads=R, writes=['Bre'])
        P.op('dve', lambda e: e.tensor_tensor(out=rt[5][:, :], in0=rt[1][:, :], in1=bl[:, 1, :], op=ALU.mult), reads=R + ['bl', 'Bre'], writes=R)
        P.op('dve', lambda e: e.tensor_tensor(out=rt[6][:, :], in0=rt[2][:, :], in1=bl[:, 0, :], op=ALU.mult), reads=R + ['bl'], writes=R)
        P.op('dve', lambda e: e.tensor_tensor(out=Bim[:, :], in0=rt[5][:, :], in1=rt[6][:, :], op=ALU.add), reads=R, writes=['Bim'])
        P.op('dve', lambda e: e.tensor_scalar(out=cml[:, 1, :], in0=cml[:, 1, :], scalar1=-1.0, scalar2=None, op0=ALU.mult), reads=['cml'], writes=['cml'])

        P.op('dve', lambda e: e.memset(Ur[:, :, 0:1], 1.0), writes=['U'])
        P.op('dve', lambda e: e.memset(Ui[:, :, 0:1], 0.0), reads=['U'], writes=['U'])
        for k_ in range(6):
            s_ = 1 << k_
            Lr = lambda s_=s_, k_=k_: pwr[:, :, k_:k_ + 1].to_broadcast([128, 8, s_])
            Li = lambda s_=s_, k_=k_: pwi[:, :, k_:k_ + 1].to_broadcast([128, 8, s_])
            P.op('dve', lambda e, s_=s_, Lr=Lr: e.tensor_tensor(out=t1s[:, :, 0:s_], in0=Ur[:, :, 0:s_], in1=Lr(), op=ALU.mult), reads=['U', 'pw'], writes=['t1s'])
            P.op('dve', lambda e, s_=s_, Li=Li: e.tensor_tensor(out=t2s[:, :, 0:s_], in0=Ui[:, :, 0:s_], in1=Li(), op=ALU.mult), reads=['U', 'pw'], writes=['t2s'])
            P.op('dve', lambda e, s_=s_: e.tensor_tensor(out=Ur[:, :, s_:2 * s_], in0=t1s[:, :, 0:s_], in1=t2s[:, :, 0:s_], op=ALU.subtract), reads=['t1s', 't2s', 'U'], writes=['U2'])
            P.op('dve', lambda e, s_=s_, Li=Li: e.tensor_tensor(out=t1s[:, :, 0:s_], in0=Ur[:, :, 0:s_], in1=Li(), op=ALU.mult), reads=['U', 'U2', 'pw'], writes=['t1s'])
            P.op('dve', lambda e, s_=s_, Lr=Lr: e.tensor_tensor(out=t2s[:, :, 0:s_], in0=Ui[:, :, 0:s_], in1=Lr(), op=ALU.mult), reads=['U', 'U2', 'pw'], writes=['t2s'])
            P.op('dve', lambda e, s_=s_: e.tensor_tensor(out=Ui[:, :, s_:2 * s_], in0=t1s[:, :, 0:s_], in1=t2s[:, :, 0:s_], op=ALU.add), reads=['t1s', 't2s', 'U2'], writes=['U'])
        P.op('dve', lambda e: e.tensor_tensor(out=t1s[:, :, :], in0=Ur[:, :, :], in1=Ur[:, :, :], op=ALU.mult), reads=['U'], writes=['t1s'])
        P.op('dve', lambda e: e.tensor_tensor(out=t2s[:, :, :], in0=Ui[:, :, :], in1=Ui[:, :, :], op=ALU.mult), reads=['U'], writes=['t2s'])
        P.op('dve', lambda e: e.tensor_tensor(out=t1s[:, :, :], in0=t1s[:, :, :], in1=t2s[:, :, :], op=ALU.add), reads=['t1s', 't2s'], writes=['t1s'])
        P.op('dve', lambda e: e.reciprocal(out=t1s[:, :, :], in_=t1s[:, :, :]), reads=['t1s'], writes=['t1s'])
        P.op('dve', lambda e: e.tensor_tensor(out=Vr[:, :, :], in0=Ur[:, :, :], in1=t1s[:, :, :], op=ALU.mult), reads=['U', 't1s'], writes=['V'])
        P.op('dve', lambda e: e.scalar_tensor_tensor(out=Vi[:, :, :], in0=Ui[:, :, :], scalar=-1.0, in1=t1s[:, :, :], op0=ALU.mult, op1=ALU.mult), reads=['U', 't1s', 'V'], writes=['V'])
        P.op('dve', lambda e: e.tensor_copy(out=lamz[:, :, 0:1], in_=Ur[:, :, 63:64]), reads=['U'], writes=['lamz'])
        P.op('dve', lambda e: e.tensor_copy(out=lamz[:, :, 1:2], in_=Ui[:, :, 63:64]), reads=['U', 'lamz'], writes=['lamz'])
        P.op('dve', lambda e: e.tensor_scalar(out=lamz[:, :, 2:3], in0=Ui[:, :, 63:64], scalar1=-1.0, scalar2=None, op0=ALU.mult), reads=['U', 'lamz'], writes=['lamz'])
        P.barrier()
        s3t.close()
        BT = [sb(s3, f'BT{i}', [128, NT]) for i in range(5)]
        msk = sb(s3, 'msk', [128, NT])
        P.op('pool', lambda e: e.memset(msk[:, :], 1.0), writes=['msk'])
        P.op('pool', lambda e: e.memset(msk[:, 0:NT:64], 0.0), reads=['msk'], writes=['msk'])
        P.op('dve', lambda e: e.memset(cZ[:, :, 0:1], 0.0), writes=['cZ0'])

        def v3(t):
            return t[:, :].rearrange("p (c t) -> p c t", t=64)
        for q in range(8):
            cc = q // 4
            ukeys = [('uT', cc, tb) for tb in range(8)]
            for nb in range(8):
                ns = slice(nb * 512, (nb + 1) * 512)
                pr, pi_ = pB[(nb % 2) * 2], pB[(nb % 2) * 2 + 1]
                P.op('pe', lambda e, q=q, cc=cc, ns=ns, pr=pr: e.matmul(pr[:, :], lhsT=Bre[:, q * 128:(q + 1) * 128], rhs=uT[:, cc, ns], start=True, stop=True),
                     reads=ukeys + ['Bre'], writes=[('pB', (nb % 2) * 2)])
                P.op('pe', lambda e, q=q, cc=cc, ns=ns, pi_=pi_: e.matmul(pi_[:, :], lhsT=Bim[:, q * 128:(q + 1) * 128], rhs=uT[:, cc, ns], start=True, stop=True),
                     reads=ukeys + ['Bim'], writes=[('pB', (nb % 2) * 2 + 1)])
                P.op('act', lambda e, ns=ns, pr=pr: e.activation(out=BT[0][:, ns], in_=pr[:, :], func=AF.Copy), reads=[('pB', (nb % 2) * 2)], writes=['BT0'])
                P.op('dve', lambda e, ns=ns, pi_=pi_: e.tensor_copy(out=BT[1][:, ns], in_=pi_[:, :]), reads=[('pB', (nb % 2) * 2 + 1)], writes=['BT1'])
            A_, B_, C_, D_, E_ = BT
            F_ = D_
            tb_ = lambda T, q=q: T[:, q, None, :].to_broadcast([128, 64, 64])
            P.op('pool', lambda e, tb_=tb_: e.tensor_tensor(out=v3(C_), in0=v3(A_), in1=tb_(Vr), op=ALU.mult), reads=['BT0', 'V'], writes=['BT2'])
            P.op('pool', lambda e, tb_=tb_: e.tensor_tensor(out=v3(D_), in0=v3(B_), in1=tb_(Vi), op=ALU.mult), reads=['BT1', 'V'], writes=['BT3'])
            P.op('dve', lambda e: e.tensor_tensor(out=C_[:, :], in0=C_[:, :], in1=D_[:, :], op=ALU.subtract), reads=['BT2', 'BT3'], writes=['BT2'])
            P.op('pool', lambda e, tb_=tb_: e.tensor_tensor(out=v3(D_), in0=v3(A_), in1=tb_(Vi), op=ALU.mult), reads=['BT0', 'V', 'BT2'], writes=['BT3'])
            P.op('pool', lambda e, tb_=tb_: e.tensor_tensor(out=v3(E_), in0=v3(B_), in1=tb_(Vr), op=ALU.mult), reads=['BT1', 'V'], writes=['BT4'])
            P.op('dve', lambda e: e.tensor_tensor(out=D_[:, :], in0=D_[:, :], in1=E_[:, :], op=ALU.add), reads=['BT3', 'BT4'], writes=['BT3'])
            P.op('dve', lambda e: e.tensor_tensor_scan(out=A_[:, :], data0=msk[:, :], data1=C_[:, :], initial=0.0, op0=ALU.mult, op1=ALU.add), reads=['msk', 'BT2'], writes=['BT0'])
            P.op('dve', lambda e: e.tensor_tensor_scan(out=B_[:, :], data0=msk[:, :], data1=D_[:, :], initial=0.0, op0=ALU.mult, op1=ALU.add), reads=['msk', 'BT3'], writes=['BT1'])
            P.op('dve', lambda e, q=q: e.tensor_scalar(out=cS[0][:, 0, :], in0=v3(A_)[:, :, 63], scalar1=lamz[:, q, 0:1], scalar2=None, op0=ALU.mult), reads=['BT0', 'lamz'], writes=['cS0'])
            P.op('dve', lambda e, q=q: e.scalar_tensor_tensor(out=cS[0][:, 0, :], in0=v3(B_)[:, :, 63], scalar=lamz[:, q, 2:3], in1=cS[0][:, 0, :], op0=ALU.mult, op1=ALU.add), reads=['BT1', 'lamz', 'cS0'], writes=['cS0'])
            P.op('dve', lambda e, q=q: e.tensor_scalar(out=cS[0][:, 1, :], in0=v3(B_)[:, :, 63], scalar1=lamz[:, q, 0:1], scalar2=None, op0=ALU.mult), reads=['BT1', 'lamz', 'cS0'], writes=['cS0'])
            P.op('dve', lambda e, q=q: e.scalar_tensor_tensor(out=cS[0][:, 1, :], in0=v3(A_)[:, :, 63], scalar=lamz[:, q, 1:2], in1=cS[0][:, 1, :], op0=ALU.mult, op1=ALU.add), reads=['BT0', 'lamz', 'cS0'], writes=['cS0'])
            cur = 0
            for j_ in range(6):
                sft = 1 << j_
                k_ = 6 + j_
                nxt = 1 - cur
                s_, d_ = cS[cur], cS[nxt]
                ks, kd = f'cS{cur}', f'cS{nxt}'
                P.op('dve', lambda e, s_=s_, d_=d_, sft=sft, q=q, k_=k_: e.scalar_tensor_tensor(out=d_[:, 0, sft:], in0=s_[:, 0, 0:64 - sft], scalar=pwr[:, q, k_:k_ + 1], in1=s_[:, 0, sft:], op0=ALU.mult, op1=ALU.add), reads=[ks, 'pw'], writes=[kd])
                P.op('dve', lambda e, s_=s_, d_=d_, sft=sft, q=q, k_=k_: e.scalar_tensor_tensor(out=d_[:, 0, sft:], in0=s_[:, 1, 0:64 - sft], scalar=npwi[:, q, k_:k_ + 1], in1=d_[:, 0, sft:], op0=ALU.mult, op1=ALU.add), reads=[ks, 'npw', kd], writes=[kd])
                P.op('dve', lambda e, s_=s_, d_=d_, sft=sft, q=q, k_=k_: e.scalar_tensor_tensor(out=d_[:, 1, sft:], in0=s_[:, 1, 0:64 - sft], scalar=pwr[:, q, k_:k_ + 1], in1=s_[:, 1, sft:], op0=ALU.mult, op1=ALU.add), reads=[ks, 'pw', kd], writes=[kd])
                P.op('dve', lambda e, s_=s_, d_=d_, sft=sft, q=q, k_=k_: e.scalar_tensor_tensor(out=d_[:, 1, sft:], in0=s_[:, 0, 0:64 - sft], scalar=pwi[:, q, k_:k_ + 1], in1=d_[:, 1, sft:], op0=ALU.mult, op1=ALU.add), reads=[ks, 'pw', kd], writes=[kd])
                P.op('dve', lambda e, s_=s_, d_=d_, sft=sft: e.tensor_copy(out=d_[:, :, 0:sft], in_=s_[:, :, 0:sft]), reads=[ks, kd], writes=[kd])
                cur = nxt
            Xc = cS[cur]
            kx = f'cS{cur}'
            P.op('dve', lambda e, q=q, Xc=Xc: e.tensor_scalar(out=cZ[:, 0, 1:65], in0=Xc[:, 0, :], scalar1=pwr[:, q, 0:1], scalar2=None, op0=ALU.mult), reads=[kx, 'pw', 'cZ0'], writes=['cZ'])
            P.op('dve', lambda e, q=q, Xc=Xc: e.scalar_tensor_tensor(out=cZ[:, 0, 1:65], in0=Xc[:, 1, :], scalar=npwi[:, q, 0:1], in1=cZ[:, 0, 1:65], op0=ALU.mult, op1=ALU.add), reads=[kx, 'npw', 'cZ'], writes=['cZ'])
            P.op('dve', lambda e, q=q, Xc=Xc: e.tensor_scalar(out=cZ[:, 1, 1:65], in0=Xc[:, 1, :], scalar1=pwr[:, q, 0:1], scalar2=None, op0=ALU.mult), reads=[kx, 'pw', 'cZ'], writes=['cZ'])
            P.op('dve', lambda e, q=q, Xc=Xc: e.scalar_tensor_tensor(out=cZ[:, 1, 1:65], in0=Xc[:, 0, :], scalar=pwi[:, q, 0:1], in1=cZ[:, 1, 1:65], op0=ALU.mult, op1=ALU.add), reads=[kx, 'pw', 'cZ'], writes=['cZ'])
            P.op('dve', lambda e, tb_=tb_: e.tensor_tensor(out=v3(A_), in0=v3(A_), in1=cZ[:, 0, 0:64, None].to_broadcast([128, 64, 64]), op=ALU.add), reads=['BT0', 'cZ'], writes=['BT0'])
            P.op('dve', lambda e, tb_=tb_: e.tensor_tensor(out=v3(B_), in0=v3(B_), in1=cZ[:, 1, 0:64, None].to_broadcast([128, 64, 64]), op=ALU.add), reads=['BT1', 'cZ'], writes=['BT1'])
            P.op('pool', lambda e, tb_=tb_: e.tensor_tensor(out=v3(C_), in0=v3(A_), in1=tb_(Ur), op=ALU.mult), reads=['BT0', 'U'], writes=['BT2'])
            P.op('pool', lambda e, tb_=tb_: e.tensor_tensor(out=v3(D_), in0=v3(B_), in1=tb_(Ui), op=ALU.mult), reads=['BT1', 'U'], writes=['BT3'])
            P.op('dve', lambda e: e.tensor_tensor(out=C_[:, :], in0=C_[:, :], in1=D_[:, :], op=ALU.subtract), reads=['BT2', 'BT3'], writes=['BT2'])
            P.op('dve', lambda e, tb_=tb_: e.tensor_tensor(out=v3(E_), in0=v3(A_), in1=tb_(Ui), op=ALU.mult), reads=['BT0', 'U'], writes=['BT4'])
            P.op('pool', lambda e, tb_=tb_: e.tensor_tensor(out=v3(F_), in0=v3(B_), in1=tb_(Ur), op=ALU.mult), reads=['BT1', 'U'], writes=['BT3'])
            P.op('dve', lambda e: e.tensor_tensor(out=E_[:, :], in0=E_[:, :], in1=F_[:, :], op=ALU.add), reads=['BT4', 'BT3'], writes=['BT4'])
            fr_, fi_ = C_, E_
            for nb in range(8):
                ns = slice(nb * 512, (nb + 1) * 512)
                py = pY[nb % 2]
                P.op('pe', lambda e, q=q, ns=ns, py=py, fr_=fr_: e.matmul(py[:, :], lhsT=cml[:, 0, q * 128:(q + 1) * 128], rhs=fr_[:, ns], start=True, stop=False),
                     reads=['cml', 'BT2'], writes=[('pY', nb % 2)])
                P.op('pe', lambda e, q=q, ns=ns, py=py, fi_=fi_: e.matmul(py[:, :], lhsT=cml[:, 1, q * 128:(q + 1) * 128], rhs=fi_[:, ns], start=False, stop=True),
                     reads=['cml', 'BT4'], writes=[('pY', nb % 2)])
                if q % 4 == 0:
                    P.op('dve', lambda e, cc=cc, ns=ns, py=py: e.scalar_tensor_tensor(out=yacc[:, ns], in0=uT[:, cc, ns], scalar=dcol[:, cc:cc + 1], in1=py[:, :], op0=ALU.mult, op1=ALU.add),
                         reads=ukeys + ['dcol', ('pY', nb % 2)], writes=[('yacc', nb)])
                else:
                    P.op('dve', lambda e, ns=ns, py=py: e.tensor_tensor(out=yacc[:, ns], in0=yacc[:, ns], in1=py[:, :], op=ALU.add),
                         reads=[('yacc', nb), ('pY', nb % 2)], writes=[('yacc', nb)])
            if q % 4 == 3:
                for nb in range(8):
                    ns = slice(nb * 512, (nb + 1) * 512)
                    b = nb % 2
                    P.op('act', lambda e, ns=ns: e.activation(out=gy[:, ns], in_=yacc[:, ns], func=AF.Gelu), reads=[('yacc', nb)], writes=[('gy', nb)])
                    P.op('pe', lambda e, cc=cc, ns=ns: e.matmul(pGa[:, :], lhsT=gwb[:, cc * 2, :], rhs=gy[:, ns], start=True, stop=True), reads=['gwb', ('gy', nb)], writes=['pGa'])
                    P.op('pe', lambda e, cc=cc, ns=ns: e.matmul(pGb[:, :], lhsT=gwb[:, cc * 2 + 1, :], rhs=gy[:, ns], start=True, stop=True), reads=['gwb', ('gy', nb)], writes=['pGb'])
                    P.op('act', lambda e, b=b: e.activation(out=sgb[b][:, :], in_=pGb[:, :], func=AF.Sigmoid), reads=['pGb'], writes=[f'sgb{b}'])
                    P.op('dve', lambda e, b=b: e.tensor_tensor(out=yso[b][:, :], in0=pGa[:, :], in1=sgb[b][:, :], op=ALU.mult), reads=['pGa', f'sgb{b}'], writes=[f'yso{b}'])
                    P.dma('sp', lambda e, cc=cc, nb=nb, b=b: e.dma_start(out=mxi[nb // 2][256 + cc * 128:256 + (cc + 1) * 128, (nb % 2) * 512:(nb % 2 + 1) * 512], in_=yso[b][:, :]), f'ys{b}',
                          reads=[f'yso{b}'], writes=[('mx_in', 'y', cc, nb)])
        P.barrier()
        s3.close()
        if stage == 'A3':
            for q_ in range(4):
                P.dma('sp', lambda e, q_=q_: e.dma_start(out=dbg[:, q_ * 1024:(q_ + 1) * 1024], in_=mxi[q_][:, :]), 'out', writes=['dbg'], force=True)
            sA2.close()
            sA.close()
            P.emit(top)
            return nc

        sA.close()
        print('ops before exchange', getattr(P, 'total', 0))
        NEI = int(os.environ.get('KNE', '128'))
        if stage == 'simB':
            mxsrc = [mx_test[:, q_ * 1024:(q_ + 1) * 1024] for q_ in range(4)]
        else:
            allk = [('mx_in', 'h', c) for c in range(32)] + [('mx_in', 'y', cc, nb) for cc in range(2) for nb in range(8)]
            for q_ in range(4):
                P.dma('pool', lambda e, q_=q_: e.collective_compute("AllGather", ALU.bypass, replica_groups=[[0, 1, 2, 3], [4, 5, 6, 7]],
                                                                   ins=[mxi[q_].ap().opt()], outs=[mxa[q_].ap().opt()]), f'cc{q_}', reads=allk, writes=[('mx_all', q_)], inc=1)
            mxsrc = [mxa[q_].ap() for q_ in range(4)]
        sB = ExitStack()
        h1 = sb(sB, 'h1', [128, 8, DM])
        xn2T = sb(sB, 'xn2T', [128, KC, 1024], BF16)
        em = sb(sB, 'em', [128, 8, 16, 128], BF16)
        sc = sb(sB, 'sc', [128, 8, 8, 2])
        selt = sb(sB, 'selt', [128, 4])
        g2t = sb(sB, 'g2t', [128, KC])
        ssq2 = [sb(sB, f'ssq2{i}', [128, 1]) for i in range(2)]
        rstd2 = [sb(sB, f'rstd2{i}', [128, 1]) for i in range(2)]
        P.dma('sp', lambda e: e.dma_start(out=selt[:, :], in_=selin[:, :]), 'c', writes=['selt'])
        P.dma('sp', lambda e: e.dma_start(out=g2t[:, :], in_=g2c[:, :]), 'c', writes=['g2t'])
        P.dma('sp', lambda e: e.dma_start(out=h1[:, :, :], in_=x_own.rearrange("(tt p) d -> p tt d", p=128)), 'c', writes=['h1'])

        print('ops before B1', getattr(P, 'total', 0))
        sb1 = ExitStack()
        mixo = sb(sb1, 'mixo', [128, KC, 1024], BF16)
        wb = sb(sb1, 'wb', [128, KC, 512], BF16)
        mst = [sb(sb1, f'mst{i}', [128, 1024], BF16) for i in range(2)]
        wst2 = [sb(sb1, f'wst2{i}', [128, 512]) for i in range(2)]
        xs2 = [sb(sb1, f'xs2{i}', [128, DM], BF16) for i in range(2)]
        pM = [ps(sb1, f'pM{i}', [128, 512]) for i in range(2)]
        ptr2 = [ps(sb1, f'ptr2{i}', [128, 8, 128], BF16) for i in range(2)]

        for kc in range(KC):
            for q_ in range(4):
                b = (kc * 4 + q_) % 2
                P.dma('sp', lambda e, kc=kc, q_=q_, b=b: e.dma_start(out=mst[b][:, :], in_=mxsrc[q_][kc * 128:(kc + 1) * 128, :]), f'ms{b}',
                      reads=[('mx_all', q_)], writes=[f'mst{b}'])
                if q_ == 0:
                    P.op('dve', lambda e, kc=kc, b=b: e.tensor_scalar(out=mixo[:, kc, :], in0=mst[b][:, :], scalar1=selt[:, 0:1], scalar2=None, op0=ALU.mult),
                         reads=[f'mst{b}', 'selt'], writes=['mixo'])
                else:
                    P.op('dve', lambda e, kc=kc, b=b, q_=q_: e.scalar_tensor_tensor(out=mixo[:, kc, :], in0=mst[b][:, :], scalar=selt[:, q_:q_ + 1], in1=mixo[:, kc, :], op0=ALU.mult, op1=ALU.add),
                         reads=[f'mst{b}', 'selt', 'mixo'], writes=['mixo'])
        for nblk in range(4):
            for kc in range(KC):
                b = kc % 2
                P.dma('sp', lambda e, kc=kc, b=b, nblk=nblk: e.dma_start(out=wst2[b][:, :], in_=w_o[:, kc, nblk * 512:(nblk + 1) * 512]), f'wo{b}', writes=[f'wst2{b}'])
                P.op('act', lambda e, kc=kc, b=b: e.activation(out=wb[:, kc, :], in_=wst2[b][:, :], func=AF.Copy), reads=[f'wst2{b}'], writes=['wb'])
            for tt in range(8):
                pb_ = tt % 2
                for kc in range(KC):
                    P.op('pe', lambda e, kc=kc, tt=tt, pb_=pb_: e.matmul(pM[pb_][:, :], lhsT=mixo[:, kc, tt * 128:(tt + 1) * 128], rhs=wb[:, kc, :], start=(kc == 0), stop=(kc == KC - 1)),
                         reads=['mixo', 'wb'], writes=[f'pM{pb_}'])
                P.op('dve', lambda e, tt=tt, pb_=pb_, nblk=nblk: e.tensor_tensor(out=h1[:, tt, nblk * 512:(nblk + 1) * 512], in0=h1[:, tt, nblk * 512:(nblk + 1) * 512], in1=pM[pb_][:, :], op=ALU.add),
                     reads=[f'pM{pb_}', 'h1'], writes=['h1'])

        def rms(tt, b, jt, jk):
            P.op('act', lambda e: e.activation(out=jt[:, :], in_=h1[:, tt, :], func=AF.Square, accum_out=ssq2[b][:, :]), reads=['h1'], writes=[jk, f'ssq2{b}'])
            P.op('dve', lambda e: e.tensor_scalar(out=rstd2[b][:, :], in0=ssq2[b][:, :], scalar1=1.0 / DM, scalar2=EPS, op0=ALU.mult, op1=ALU.add), reads=[f'ssq2{b}'], writes=[f'rstd2{b}'])
            P.op('act', lambda e: e.activation(out=rstd2[b][:, :], in_=rstd2[b][:, :], func=AF.Sqrt), reads=[f'rstd2{b}'], writes=[f'rstd2{b}'])
            P.op('dve', lambda e: e.reciprocal(out=rstd2[b][:, :], in_=rstd2[b][:, :]), reads=[f'rstd2{b}'], writes=[f'rstd2{b}'])
        for tt in range(8):
            b = tt % 2
            rms(tt, b, xs2[b], f'xs2{b}')
            P.op('dve', lambda e, tt=tt, b=b: e.tensor_scalar(out=xs2[b][:, :], in0=h1[:, tt, :], scalar1=rstd2[b][:, 0:1], scalar2=None, op0=ALU.mult), reads=['h1', f'rstd2{b}'], writes=[f'xs2{b}'])
            for half in range(2):
                for j in range(8):
                    kc = half * 8 + j
                    P.op('pe', lambda e, b=b, kc=kc, half=half, j=j: e.transpose(out=ptr2[half][:, j, :], in_=xs2[b][:, kc * 128:(kc + 1) * 128], identity=ident_b[:, :]),
                         reads=[f'xs2{b}', 'ident_b'], writes=[f'ptr2{half}'])
                P.op('dve', lambda e, half=half, tt=tt: e.tensor_tensor(out=xn2T[:, half * 8:half * 8 + 8, tt * 128:(tt + 1) * 128], in0=ptr2[half][:, :, :],
                                                                         in1=g2t[:, half * 8:half * 8 + 8, None].to_broadcast([128, 8, 128]), op=ALU.mult),
                     reads=[f'ptr2{half}', 'g2t'], writes=['xn2T'])
        P.barrier()
        sb1.close()

        print('ops before B2', getattr(P, 'total', 0))
        sb2 = ExitStack()
        v16 = sb(sb2, 'v16', [128, 8, 16, 16])
        skf = sb(sb2, 'skf', [128, 2, 128])
        skb = sb(sb2, 'skb', [128, 2, 128], BF16)
        wqst = [sb(sb2, f'wqst{i}', [128, KC, 128]) for i in range(2)]
        wqb = [sb(sb2, f'wqb{i}', [128, KC, 128], BF16) for i in range(2)]
        qj = [sb(sb2, f'qj{i}', [128, 1024], BF16) for i in range(2)]
        ebf = [sb(sb2, f'ebf{i}', [128, 128], BF16) for i in range(2)]
        ef = [sb(sb2, f'ef{i}', [128, 128]) for i in range(2)]
        ef2 = [sb(sb2, f'ef2{i}', [128, 128]) for i in range(2)]
        cand = [sb(sb2, f'cand{i}', [128, 256]) for i in range(2)]
        cand2 = [sb(sb2, f'cand2{i}', [128, 256]) for i in range(2)]
        c16 = [sb(sb2, f'c16{i}', [128, 16]) for i in range(2)]
        pQ = [ps(sb2, f'pQ{i}', [128, 512]) for i in range(2)]
        pS = [ps(sb2, f'pS{i}', [128, 128]) for i in range(2)]
        P.dma('sp', lambda e: e.dma_start(out=skf[:, :, :], in_=skT[:, :, :]), 'c', writes=['skf'])
        P.op('dve', lambda e: e.tensor_copy(out=skb[:, :, :], in_=skf[:, :, :]), reads=['skf'], writes=['skb'])
        for j in range(16):
            b = j % 2
            half = j % 2
            P.dma('sp', lambda e, j=j, b=b: e.dma_start(out=wqst[b][:, :, :], in_=w_q[:, :, j * 128:(j + 1) * 128]), f'wq{b}', writes=[f'wqst{b}'])
            P.op('dve', lambda e, b=b: e.tensor_copy(out=wqb[b][:, :, :], in_=wqst[b][:, :, :]),
                 reads=[f'wqst{b}'], writes=[f'wqb{b}'])
            for th in range(2):
                for kc in range(KC):
                    P.op('pe', lambda e, b=b, kc=kc, th=th: e.matmul(pQ[th][:, :], lhsT=wqb[b][:, kc, :], rhs=xn2T[:, kc, th * 512:(th + 1) * 512], start=(kc == 0), stop=(kc == KC - 1)),
                         reads=[f'wqb{b}', 'xn2T'], writes=[f'pQ{th}'])
                P.op('act', lambda e, b=b, th=th: e.activation(out=qj[b][:, th * 512:(th + 1) * 512], in_=pQ[th][:, :], func=AF.Copy), reads=[f'pQ{th}'], writes=[f'qj{b}'])
            for tp_ in range(4):
                tts = (2 * tp_, 2 * tp_ + 1)
                for tt in tts:
                    b2 = tt % 2
                    P.op('pe', lambda e, b=b, tt=tt, b2=b2, half=half: e.matmul(pS[b2][:, :], lhsT=qj[b][:, tt * 128:(tt + 1) * 128], rhs=skb[:, half, :], start=True, stop=True),
                         reads=[f'qj{b}', 'skb'], writes=[f'pS{b2}'])
                for tt in tts:
                    b2 = tt % 2
                    P.op('act', lambda e, b2=b2: e.activation(out=ebf[b2][:, :], in_=pS[b2][:, :], func=AF.Exp), reads=[f'pS{b2}'], writes=[f'ebf{b2}'])
                for tt in tts:
                    b2 = tt % 2
                    P.op('dve', lambda e, b2=b2: e.tensor_copy(out=ef[b2][:, :], in_=ebf[b2][:, :]), reads=[f'ebf{b2}'], writes=[f'ef{b2}'])
                for tt in tts:
                    b2 = tt % 2
                    P.op('dve', lambda e, b2=b2, tt=tt, j=j: e.max(out=v16[:, tt, j, 0:8], in_=ef[b2][:, :]), reads=[f'ef{b2}'], writes=[('v16', tt, j)])
                for tt in tts:
                    b2 = tt % 2
                    P.op('dve', lambda e, b2=b2, tt=tt, j=j: e.match_replace(out=ef2[b2][:, :], in_to_replace=v16[:, tt, j, 0:8], in_values=ef[b2][:, :], imm_value=-1.0), reads=[f'ef{b2}', ('v16', tt, j)], writes=[f'ef2{b2}'])
                for tt in tts:
                    b2 = tt % 2
                    P.op('dve', lambda e, b2=b2, tt=tt, j=j: e.max(out=v16[:, tt, j, 8:16], in_=ef2[b2][:, :]), reads=[f'ef2{b2}'], writes=[('v16', tt, j)])
                for tt in tts:
                    b2 = tt % 2
                    P.op('dve', lambda e, b2=b2, tt=tt, j=j: e.scalar_tensor_tensor(out=em[:, tt, j, :], in0=ef[b2][:, :], scalar=v16[:, tt, j, 15:16], in1=ef[b2][:, :], op0=ALU.is_ge, op1=ALU.mult),
                         reads=[f'ef{b2}', ('v16', tt, j)], writes=[('em', tt, j)])
        P.barrier()
        for tt in range(8):
            for hp in range(4):
                hs = (2 * hp, 2 * hp + 1)
                for hh in hs:
                    b = hh % 2
                    P.op('dve', lambda e, tt=tt, hh=hh, b=b: e.tensor_tensor(out=cand[b][:, :].rearrange("p (a c) -> p a c", a=16), in0=v16[:, tt, 2 * hh, :, None].to_broadcast([128, 16, 16]),
                                                                              in1=v16[:, tt, 2 * hh + 1, None, :].to_broadcast([128, 16, 16]), op=ALU.mult), reads=['v16'], writes=[f'cand{b}'])
                for hh in hs:
                    b = hh % 2
                    P.op('dve', lambda e, b=b: e.max(out=c16[b][:, 0:8], in_=cand[b][:, :]), reads=[f'cand{b}'], writes=[f'c16{b}'])
                for hh in hs:
                    b = hh % 2
                    P.op('dve', lambda e, b=b: e.match_replace(out=cand2[b][:, :], in_to_replace=c16[b][:, 0:8], in_values=cand[b][:, :], imm_value=-1.0), reads=[f'cand{b}', f'c16{b}'], writes=[f'cand2{b}'])
                for hh in hs:
                    b = hh % 2
                    P.op('dve', lambda e, b=b: e.max(out=c16[b][:, 8:16], in_=cand2[b][:, :]), reads=[f'cand2{b}'], writes=[f'c16{b}'])
                for hh in hs:
                    b = hh % 2
                    P.op('dve', lambda e, b=b, tt=tt, hh=hh: e.tensor_scalar(out=sc[:, tt, hh, 0:1], in0=c16[b][:, 15:16], scalar1=0.999996, scalar2=None, op0=ALU.mult), reads=[f'c16{b}'], writes=[('sc', tt, hh, 0)])
                for hh in hs:
                    b = hh % 2
                    P.op('dve', lambda e, b=b, tt=tt, hh=hh: e.reduce_sum(out=sc[:, tt, hh, 1:2], in_=c16[b][:, :], axis=AX.X), reads=[f'c16{b}'], writes=[('sc', tt, hh, 1)])
        P.barrier()
        P.op('dve', lambda e: e.reciprocal(out=sc[:, :, :, 1], in_=sc[:, :, :, 1]), reads=['sc'], writes=['sc'])
        P.barrier()
        sb2.close()

        print('ops before B3', getattr(P, 'total', 0))
        EB = 4
        sb3 = ExitStack()
        Ust = [sb(sb3, f'Ust{i}', [128, 1024]) for i in range(2)]
        Vst = [sb(sb3, f'Vst{i}', [128, 1024]) for i in range(2)]
        Ub = sb(sb3, 'Ub', [128, DM], BF16)
        Vb = [sb(sb3, f'Vb{i}', [128, DM], BF16) for i in range(EB)]
        UT = sb(sb3, 'UT', [128, KC, 128], BF16)
        gelT = [sb(sb3, f'gelT{i}', [128, 1024], BF16) for i in range(2)]
        thw = [sb(sb3, f'thw{i}', [128, 3, 8, 8]) for i in range(2)]
        Yb = [sb(sb3, f'Yb{i}', [128, 8, 128], BF16) for i in range(3)]
        Dg = [sb(sb3, f'Dg{i}', [128, 8, 128], BF16) for i in range(3)]
        Mk = [sb(sb3, f'Mk{i}', [128, 8, 128], BF16) for i in range(3)]
        GAT = sb(sb3, 'GAT', [128, EB, 1024], BF16)
        ptr3 = ps(sb3, 'ptr3', [128, 8, 128], BF16)
        pA2 = [ps(sb3, f'pA2{i}', [128, 512]) for i in range(2)]
        pG = [ps(sb3, f'pG{i}', [128, 128]) for i in range(3)]
        pO = [ps(sb3, f'pO{i}', [128, 512]) for i in range(2)]
        LAG = 2
        pend = []

        def flush_one():
            il_, ib_, tt_, b_ = pend.pop(0)
            P.op('dve', lambda e: e.tensor_tensor(out=GAT[:, il_, tt_ * 128:(tt_ + 1) * 128], in0=pG[b_][:, :], in1=gelT[ib_][:, tt_ * 128:(tt_ + 1) * 128], op=ALU.mult),
                 reads=[('pG', b_), f'gelT{ib_}'], writes=[('GAT', il_, tt_)])
        def prepA_pieces(i):
            ib = i % 2
            tk = f'thw{ib}'
            pieces = []

            def p_load():
                for dh in range(2):
                    ds_ = slice(dh * 1024, (dh + 1) * 1024)
                    P.dma('sp', lambda e, dh=dh, ds_=ds_: e.dma_start(out=Ust[dh][:, :], in_=pu[i * 128:(i + 1) * 128, ds_]), f'pu{dh}', writes=[f'Ust{dh}'])
                    P.op('act', lambda e, dh=dh, ds_=ds_: e.activation(out=Ub[:, ds_], in_=Ust[dh][:, :], func=AF.Copy), reads=[f'Ust{dh}'], writes=[('Ub', dh)])
                P.op('dve', lambda e: e.tensor_scalar(out=thw[ib][:, 0, :, :], in0=em[:, :, 0:16:2, i], scalar1=1e-30, scalar2=None, op0=ALU.max), reads=['em'], writes=[tk])
                P.op('dve', lambda e: e.reciprocal(out=thw[ib][:, 0, :, :], in_=thw[ib][:, 0, :, :]), reads=[tk], writes=[tk])
                P.op('dve', lambda e: e.tensor_tensor(out=thw[ib][:, 1, :, :], in0=thw[ib][:, 0, :, :], in1=sc[:, :, :, 0], op=ALU.mult), reads=[tk, 'sc'], writes=[tk])
                P.op('dve', lambda e: e.tensor_tensor(out=thw[ib][:, 2, :, :], in0=em[:, :, 0:16:2, i], in1=sc[:, :, :, 1], op=ALU.mult), reads=['em', 'sc', tk], writes=[tk])
            pieces.append(p_load)

            def p_tr(half):
                def f():
                    for j in range(8):
                        kc = half * 8 + j
                        P.op('pe', lambda e, kc=kc, j=j: e.transpose(out=ptr3[:, j, :], in_=Ub[:, kc * 128:(kc + 1) * 128], identity=ident_b[:, :]),
                             reads=[('Ub', half), 'ident_b'], writes=['ptr3'])
                    P.op('act', lambda e: e.activation(out=UT[:, half * 8:half * 8 + 8, :], in_=ptr3[:, :, :], func=AF.Copy), reads=['ptr3'], writes=['UT'])
                return f
            pieces.append(p_tr(0))
            pieces.append(p_tr(1))

            def p_mm(th, k0, k1):
                def f():
                    for kc in range(k0, k1):
                        P.op('pe', lambda e, kc=kc: e.matmul(pA2[th][:, :], lhsT=UT[:, kc, :], rhs=xn2T[:, kc, th * 512:(th + 1) * 512], start=(kc == 0), stop=(kc == KC - 1)),
                             reads=['xn2T', 'UT'], writes=[f'pA2{th}'])
                    if k1 == KC:
                        P.op('act', lambda e: e.activation(out=gelT[ib][:, th * 512:(th + 1) * 512], in_=pA2[th][:, :], func=AF.Gelu), reads=[f'pA2{th}'], writes=[f'gelT{ib}'])
                return f
            for th in range(2):
                for k0 in (0, 6, 11):
                    pieces.append(p_mm(th, k0, {0: 6, 6: 11, 11: 16}[k0]))
            return pieces

        for pc in prepA_pieces(0):
            pc()
        for blk in range(NEI // EB):
            for il in range(EB):
                i = blk * EB + il
                ib = i % 2
                tk = f'thw{ib}'
                for dh in range(2):
                    ds_ = slice(dh * 1024, (dh + 1) * 1024)
                    P.dma('sp', lambda e, i=i, dh=dh, ds_=ds_: e.dma_start(out=Vst[dh][:, :], in_=pv_[i * 128:(i + 1) * 128, ds_]), f'pv{dh}', writes=[f'Vst{dh}'])
                    P.op('act', lambda e, dh=dh, ds_=ds_, il=il: e.activation(out=Vb[il][:, ds_], in_=Vst[dh][:, :], func=AF.Copy), reads=[f'Vst{dh}'], writes=[('Vb', il)])
                nxt = prepA_pieces(i + 1) if i + 1 < NEI else []
                if nxt:
                    nxt.pop(0)()
                for tt in range(8):
                    n = il * 8 + tt
                    b = n % 3
                    gb = n % 3
                    mb = n % 3
                    P.op('pool', lambda e, b=b, ib=ib, tt=tt: e.tensor_tensor(out=Dg[b][:, 0:4, :], in0=ident_f[:, None, :].to_broadcast([128, 4, 128]),
                                                                         in1=thw[ib][:, 2, tt, 0:4, None].to_broadcast([128, 4, 128]), op=ALU.mult),
                         reads=['ident_f', tk], writes=[(f'Dg{b}', 0)])
                    for hh in range(4, 8):
                        P.op('act', lambda e, b=b, ib=ib, tt=tt, hh=hh: e.activation(out=Dg[b][:, hh, :], in_=ident_f[:, :], func=AF.Copy, scale=thw[ib][:, 2, tt, hh:hh + 1]),
                             reads=['ident_f', tk], writes=[(f'Dg{b}', hh)])
                    P.op('dve', lambda e, mb=mb, ib=ib, tt=tt: e.tensor_tensor(out=Mk[mb][:, :, :], in0=em[:, tt, 1:16:2, :], in1=thw[ib][:, 1, tt, :, None].to_broadcast([128, 8, 128]), op=ALU.is_ge),
                         reads=['em', tk], writes=[f'Mk{mb}'])
                    P.op('dve', lambda e, b=b, mb=mb, tt=tt: e.tensor_tensor(out=Yb[b][:, :, :], in0=Mk[mb][:, :, :], in1=em[:, tt, 1:16:2, :], op=ALU.mult),
                         reads=['em', f'Mk{mb}'], writes=[f'Yb{b}'])
                    for hh in range(8):
                        P.op('pe', lambda e, b=b, hh=hh, gb=gb: e.matmul(pG[gb][:, :], lhsT=Yb[b][:, hh, :], rhs=Dg[b][:, hh, :], start=(hh == 0), stop=(hh == 7)),
                             reads=[f'Yb{b}', (f'Dg{b}', 0 if hh < 4 else hh)], writes=[('pG', gb)])
                    pend.append((il, ib, tt, gb))
                    if len(pend) > LAG:
                        flush_one()
                    if nxt:
                        nxt.pop(0)()
                while nxt:
                    nxt.pop(0)()
            while pend:
                flush_one()
            for tt in range(8):
                for nblk in range(4):
                    ob = nblk % 2
                    for il in range(EB):
                        P.op('pe', lambda e, il=il, tt=tt, nblk=nblk, ob=ob: e.matmul(pO[ob][:, :], lhsT=GAT[:, il, tt * 128:(tt + 1) * 128], rhs=Vb[il][:, nblk * 512:(nblk + 1) * 512], start=(il == 0), stop=(il == EB - 1)),
                             reads=[('GAT', il, tt), ('Vb', il)], writes=[('pO', ob)])
                    P.op('dve', lambda e, tt=tt, nblk=nblk, ob=ob: e.tensor_tensor(out=h1[:, tt, nblk * 512:(nblk + 1) * 512], in0=h1[:, tt, nblk * 512:(nblk + 1) * 512], in1=pO[ob][:, :], op=ALU.add),
                         reads=[('pO', ob), ('h1', tt, nblk)], writes=[('h1', tt, nblk)])
        P.barrier()
        for nh in range(2):
            P.dma('sp', lambda e, nh=nh: e.dma_start(out=Vst[nh][:, :], in_=gfr[:, nh * 1024:(nh + 1) * 1024]), f'pv{nh}', writes=[f'Vst{nh}'])
        for tt in range(8):
            b = tt % 2
            rms(tt, b, Ub, ('Ub', 0))
            for nh in range(2):
                P.op('dve', lambda e, tt=tt, b=b, nh=nh: e.scalar_tensor_tensor(out=Ust[nh][:, :], in0=h1[:, tt, nh * 1024:(nh + 1) * 1024], scalar=rstd2[b][:, 0:1], in1=Vst[nh][:, :], op0=ALU.mult, op1=ALU.mult),
                     reads=['h1', f'rstd2{b}', f'Vst{nh}'], writes=[f'Ust{nh}'])
                P.dma('sp', lambda e, tt=tt, nh=nh: e.dma_start(out=yout[tt * 128:(tt + 1) * 128, nh * 1024:(nh + 1) * 1024], in_=Ust[nh][:, :]), f'yo{nh}', reads=[f'Ust{nh}'], writes=[('y', tt, nh)])
        P.barrier()
        sb3.close()
        sB.close()
        P.emit(top)
    return nc


def host_inputs(inp, r):
    b, h = r // 4, r % 4
    w_in = inp['w_in'][0]
    cols = np.concatenate([
        np.arange(h * 256, (h + 1) * 256),
        1024 + np.arange(h * 256, (h + 1) * 256),
        4104 + np.arange(h * 256, (h + 1) * 256),
        2048 + np.arange(h * 256, (h + 1) * 256),
        np.array([4096 + h, 4100 + h]),
        3072 + np.arange(h * 256, (h + 1) * 256),
    ])
    w_a = np.ascontiguousarray(w_in[:, cols].reshape(KC, 128, 1282).transpose(1, 0, 2))
    g1 = np.ascontiguousarray(inp['norm1_g'][0].reshape(KC, 128).T)
    bgv = inp['b_gates'][0]
    bg = np.ascontiguousarray(np.broadcast_to(np.array([bgv[h], bgv[4 + h]], np.float32)[None, :], (128, 2)))
    cwf = inp['conv_qk_w'][0]
    chans = np.concatenate([np.arange(h * 256, (h + 1) * 256), 1024 + np.arange(h * 256, (h + 1) * 256)])
    convw = np.ascontiguousarray(cwf[:, chans].T.reshape(4, 128, 4).transpose(1, 0, 2))
    mg = np.ascontiguousarray(np.broadcast_to(inp['mlstm_norm_g'][0][h * 256:(h + 1) * 256][None, :], (128, 256)))

    G0 = 16 * h
    lre = inp['s5_lambda_re'][0][G0:G0 + 16]
    lim = inp['s5_lambda_im'][0][G0:G0 + 16]
    ldt = np.broadcast_to(inp['s5_log_dt'][0][G0:G0 + 16][:, None], (16, 64))
    def colrow(a):
        col = a.reshape(8, 128).T
        row = a.reshape(1024)
        return col, row
    cols_, rows_ = zip(*[colrow(np.asarray(a, np.float32)) for a in (lre, lim, ldt)])
    s5c = np.ascontiguousarray(np.stack(cols_, axis=1))
    s5r = np.ascontiguousarray(np.broadcast_to(np.stack(rows_, axis=0)[None], (128, 3, 1024)))
    s5b = np.zeros((128, 2, 8, 128), np.float32)
    s5cm = np.zeros((128, 2, 8, 128), np.float32)
    for ri, (bsrc, csrc) in enumerate(((inp['s5_b_re'][0], inp['s5_c_re'][0]), (inp['s5_b_im'][0], inp['s5_c_im'][0]))):
        for gl in range(16):
            q, g2 = gl // 2, gl % 2
            r0 = (gl % 8) * 16
            s5b[r0:r0 + 16, ri, q, g2 * 64:(g2 + 1) * 64] = bsrc[G0 + gl].T
            s5cm[g2 * 64:(g2 + 1) * 64, ri, q, r0:r0 + 16] = csrc[G0 + gl].T
    s5d = np.ascontiguousarray(inp['s5_d'][0][G0:G0 + 16].reshape(2, 128).T)
    s5gw = np.zeros((128, 4, 128), np.float32)
    gw = inp['s5_glu_w'][0]
    for gl in range(16):
        cc, r0 = gl // 8, (gl % 8) * 16
        s5gw[r0:r0 + 16, cc * 2, r0:r0 + 16] = gw[G0 + gl][:, :16]
        s5gw[r0:r0 + 16, cc * 2 + 1, r0:r0 + 16] = gw[G0 + gl][:, 16:]

    perm = np.concatenate([np.concatenate([256 * hh + np.arange(256), 1024 + 256 * hh + np.arange(256)]) for hh in range(4)])
    w_o = np.ascontiguousarray(inp['w_out'][0][perm].reshape(KC, 128, DM).transpose(1, 0, 2))
    w_q = np.ascontiguousarray(inp['peer_wq'][0].reshape(KC, 128, DM).transpose(1, 0, 2))
    g2 = inp['norm2_g'][0]
    g2c = np.ascontiguousarray(g2.reshape(KC, 128).T)
    g2r = np.ascontiguousarray(np.broadcast_to(g2[None, :], (128, DM)))
    gfr = np.ascontiguousarray(np.broadcast_to(inp['final_g'][None, :], (128, DM)))
    skT = np.ascontiguousarray(inp['peer_subkeys'][0].transpose(2, 0, 1))
    d = dict(
        x_own=np.ascontiguousarray(inp['x'][b][h * 1024:(h + 1) * 1024]), w_o=w_o, w_q=w_q, g2c=g2c, g2r=g2r, gfr=gfr, skT=skT,
        pu=inp['peer_u'][0], pv=inp['peer_v'][0], sel=np.ascontiguousarray(np.broadcast_to(np.eye(4, dtype=np.float32)[h][None, :], (128, 4))),
        s5c=s5c, s5r=s5r, s5b=s5b.reshape(128, 2, 1024), s5cm=s5cm.reshape(128, 2, 1024), s5d=s5d, s5gw=s5gw,
        x=np.ascontiguousarray(inp['x'][b]),
        w_a=w_a, g1=g1, bg=bg, convw=convw, mg=mg,
        c_ident=_ident(),
        c_triu=np.triu(np.ones((128, 128), np.float32)),
        c_ones=np.ones((128, 128), np.float32),
    )
    return d


def kernel(**inputs):
    inp = {k: np.asarray(v) for k, v in inputs.items()}
    nc = build('full')
    in_maps = [host_inputs(inp, r) for r in range(8)]
    res = run_bass_kernel_spmd(nc, in_maps, core_ids=list(range(8)))
    out = np.zeros((2, NT, DM), np.float32)
    for r in range(8):
        b, h = r // 4, r % 4
        out[b, h * 1024:(h + 1) * 1024] = res.results[r]['y']
    return out
```
